# Optimizing a Trainium2 kernel written in Bass

```python
import math
import jax
import jax.numpy as jnp
from jax import lax
import numpy as np

D_MODEL = 1024
BATCH = 2
SEQ = 8192
DEPTH = 1

N_HEADS = 4
HEAD_DIM = 64
V_DIM = 2 * HEAD_DIM
ATTN_WIDTH = N_HEADS * V_DIM
Q_BLOCK = 128
LAMBDA_STD = 0.1
ALIBI_SLOPES = tuple(2.0 ** (-8.0 * (h + 1) / N_HEADS) for h in range(N_HEADS))

LRU_WIDTH = D_MODEL // 2
LRU_BLOCKS = 8
LRU_BLOCK_W = LRU_WIDTH // LRU_BLOCKS
CONV_W = 4
LRU_C = 8.0
A_MIN = 0.9
A_MAX = 0.999

N_GROUPS = 4
EXPERTS_PER_GROUP = 8
N_EXPERTS = N_GROUPS * EXPERTS_PER_GROUP
TOP_K = 2
D_EXPERT = D_MODEL // 2
MOE_BLOCK = 128

NORM_EPS = 1e-6

QK_COLS = N_HEADS * 2 * HEAD_DIM
SPLITS = (QK_COLS, 2 * QK_COLS, 2 * QK_COLS + ATTN_WIDTH,
          2 * QK_COLS + ATTN_WIDTH + LRU_WIDTH, 2 * QK_COLS + ATTN_WIDTH + 2 * LRU_WIDTH)
IN_COLS = SPLITS[-1] + 2 * D_MODEL

kernel_name = 'hybrid_diffattn_rglru_hmoe'


def rms_norm(x, g):
    xf = x.astype(jnp.float32)
    y = xf * lax.rsqrt(jnp.mean(xf * xf, axis=-1, keepdims=True) + NORM_EPS)
    return (y * g.astype(jnp.float32)).astype(x.dtype)


def diff_attention(q, k, v, lam, subln_g, lam_init):
    B, S = q.shape[0], q.shape[1]
    n_blocks = S // Q_BLOCK
    slopes = jnp.asarray(ALIBI_SLOPES, dtype=jnp.float32)
    scale = HEAD_DIM ** -0.5
    k_pos = jnp.arange(S)

    def one_block(i):
        start = i * Q_BLOCK
        qb = lax.dynamic_slice_in_dim(q, start, Q_BLOCK, axis=1)
        s = jnp.einsum('bqhmd,bkhmd->bhmqk', qb, k).astype(jnp.float32) * scale
        dist = (start + jnp.arange(Q_BLOCK))[:, None] - k_pos[None, :]
        alibi = -slopes[:, None, None] * dist.astype(jnp.float32)[None]
        s = jnp.where((dist >= 0)[None, None, None], s + alibi[None, :, None], -jnp.inf)
        p = jax.nn.softmax(s, axis=-1)
        w = p[:, :, 0] - lam * p[:, :, 1]
        return jnp.einsum('bhqk,bkhv->bqhv', w.astype(v.dtype), v)

    o = lax.map(one_block, jnp.arange(n_blocks))
    o = jnp.moveaxis(o, 0, 1).reshape(B, S, N_HEADS, V_DIM)
    o = rms_norm(o, subln_g) * (1.0 - lam_init)
    return o.reshape(B, S, ATTN_WIDTH)


def causal_conv(x, w, b):
    C = x.shape[-1]
    y = lax.conv_general_dilated(x, w[:, None, :].astype(x.dtype), window_strides=(1,),
                                 padding=[(CONV_W - 1, 0)],
                                 dimension_numbers=('NWC', 'WIO', 'NWC'),
                                 feature_group_count=C)
    return y + b.astype(x.dtype)


def rg_lru(x, w_r, b_r, w_i, b_i, lru_lambda):
    B, S, C = x.shape
    xb = x.reshape(B, S, LRU_BLOCKS, LRU_BLOCK_W)
    r = jax.nn.sigmoid(jnp.einsum('bsnc,ncd->bsnd', xb, w_r).reshape(B, S, C) + b_r)
    i = jax.nn.sigmoid(jnp.einsum('bsnc,ncd->bsnd', xb, w_i).reshape(B, S, C) + b_i)
    log_a = -LRU_C * r.astype(jnp.float32) * jax.nn.softplus(-lru_lambda.astype(jnp.float32))
    a = jnp.exp(log_a)
    mult = jnp.sqrt(-jnp.expm1(2.0 * log_a))
    mult = jnp.where(jnp.arange(S)[None, :, None] == 0, 1.0, mult)
    u = (x * i).astype(jnp.float32) * mult

    def combine(c1, c2):
        a1, b1 = c1
        a2, b2 = c2
        return a1 * a2, a2 * b1 + b2

    _, h = lax.associative_scan(combine, (a, u), axis=1)
    return h.astype(x.dtype)


def hier_moe(h, w_group, w_expert_router, w_gate, w_up, w_down):
    B, S, D = h.shape
    N = B * S
    t = h.reshape(N, D)
    g_logits = (t @ w_group).astype(jnp.float32)
    g_prob = jax.nn.softmax(g_logits, axis=-1)
    g_idx = jnp.argmax(g_logits, axis=-1)
    g_w = jnp.take_along_axis(g_prob, g_idx[:, None], axis=1)[:, 0]
    e_logits_all = jnp.einsum('nd,gde->nge', t, w_expert_router)
    e_logits = jnp.take_along_axis(e_logits_all, g_idx[:, None, None], axis=1)[:, 0].astype(jnp.float32)
    e_prob = jax.nn.softmax(e_logits, axis=-1)
    top_p, top_i = lax.top_k(e_prob, TOP_K)
    top_p = top_p / jnp.sum(top_p, axis=-1, keepdims=True)
    weights = g_w[:, None] * top_p
    expert_id = g_idx[:, None] * EXPERTS_PER_GROUP + top_i

    A = N * TOP_K
    flat_e = expert_id.reshape(A).astype(jnp.int32)
    flat_tok = jnp.repeat(jnp.arange(N, dtype=jnp.int32), TOP_K)
    flat_w = weights.reshape(A)
    order = jnp.argsort(flat_e)
    sorted_e = flat_e[order]
    counts = jnp.bincount(flat_e, length=N_EXPERTS)
    starts = jnp.cumsum(counts) - counts
    padded = (counts + MOE_BLOCK - 1) // MOE_BLOCK * MOE_BLOCK
    pad_ends = jnp.cumsum(padded)
    pad_starts = pad_ends - padded
    dest = pad_starts[sorted_e] + (jnp.arange(A) - starts[sorted_e])
    n_blocks = (A + N_EXPERTS * (MOE_BLOCK - 1) + MOE_BLOCK - 1) // MOE_BLOCK
    P = n_blocks * MOE_BLOCK
    row_tok = jnp.full((P,), N, dtype=jnp.int32).at[dest].set(flat_tok[order])
    row_w = jnp.zeros((P,), jnp.float32).at[dest].set(flat_w[order])
    block_e = jnp.minimum(jnp.searchsorted(pad_ends, jnp.arange(n_blocks) * MOE_BLOCK, side='right'),
                          N_EXPERTS - 1)
    t_pad = jnp.concatenate([t, jnp.zeros((1, D), t.dtype)], axis=0)
    xs = t_pad[row_tok].reshape(n_blocks, MOE_BLOCK, D)

    def expert_block(args):
        xb, e = args
        return (jax.nn.silu(xb @ w_gate[e]) * (xb @ w_up[e])) @ w_down[e]

    ys = lax.map(expert_block, (xs, block_e)).reshape(P, D)
    out = jnp.zeros((N + 1, D), jnp.float32).at[row_tok].add(ys.astype(jnp.float32) * row_w[:, None])[:N]
    return out.reshape(B, S, D).astype(h.dtype)


def setup_inputs(seed: int = 0) -> dict:
    key = jax.random.key(seed)
    ks = jax.random.split(key, 24)
    f32 = jnp.float32
    nrm = lambda k, shape, s: jax.random.normal(k, shape, f32) * s
    a0 = jax.random.uniform(ks[11], (DEPTH, LRU_WIDTH), f32, A_MIN, A_MAX)
    sig = a0 ** (1.0 / LRU_C)
    lru_lambda = jnp.log(sig) - jnp.log1p(-sig)
    return {
        'x': jax.random.normal(ks[0], (BATCH, SEQ, D_MODEL), f32),
        'norm_mix_g': 1.0 + nrm(ks[1], (DEPTH, D_MODEL), 0.02),
        'w_in': nrm(ks[2], (DEPTH, D_MODEL, IN_COLS), D_MODEL ** -0.5),
        'lambda_qk': nrm(ks[3], (DEPTH, 2, 2, HEAD_DIM), LAMBDA_STD),
        'subln_g': 1.0 + nrm(ks[4], (DEPTH, V_DIM), 0.02),
        'conv_w': nrm(ks[5], (DEPTH, CONV_W, LRU_WIDTH), CONV_W ** -0.5),
        'conv_b': nrm(ks[6], (DEPTH, LRU_WIDTH), 0.01),
        'w_r': nrm(ks[7], (DEPTH, LRU_BLOCKS, LRU_BLOCK_W, LRU_BLOCK_W), LRU_BLOCK_W ** -0.5),
        'b_r': nrm(ks[8], (DEPTH, LRU_WIDTH), 0.01),
        'w_i': nrm(ks[9], (DEPTH, LRU_BLOCKS, LRU_BLOCK_W, LRU_BLOCK_W), LRU_BLOCK_W ** -0.5),
        'b_i': nrm(ks[10], (DEPTH, LRU_WIDTH), 0.01),
        'lru_lambda': lru_lambda,
        'w_o_attn': nrm(ks[12], (DEPTH, ATTN_WIDTH, D_MODEL), ATTN_WIDTH ** -0.5),
        'w_o_lru': nrm(ks[13], (DEPTH, LRU_WIDTH, D_MODEL), LRU_WIDTH ** -0.5),
        'w_out': nrm(ks[14], (DEPTH, D_MODEL, D_MODEL), D_MODEL ** -0.5),
        'norm_ffn_g': 1.0 + nrm(ks[15], (DEPTH, D_MODEL), 0.02),
        'w_group': nrm(ks[16], (DEPTH, D_MODEL, N_GROUPS), D_MODEL ** -0.5),
        'w_expert_router': nrm(ks[17], (DEPTH, N_GROUPS, D_MODEL, EXPERTS_PER_GROUP), D_MODEL ** -0.5),
        'w_gate': nrm(ks[18], (DEPTH, N_EXPERTS, D_MODEL, D_EXPERT), D_MODEL ** -0.5),
        'w_up': nrm(ks[19], (DEPTH, N_EXPERTS, D_MODEL, D_EXPERT), D_MODEL ** -0.5),
        'w_down': nrm(ks[20], (DEPTH, N_EXPERTS, D_EXPERT, D_MODEL), D_EXPERT ** -0.5),
        'final_norm_g': 1.0 + nrm(ks[21], (D_MODEL,), 0.02),
    }


def reference(x, norm_mix_g, w_in, lambda_qk, subln_g, conv_w, conv_b, w_r, b_r, w_i, b_i,
              lru_lambda, w_o_attn, w_o_lru, w_out, norm_ffn_g, w_group, w_expert_router,
              w_gate, w_up, w_down, final_norm_g):
    B, S, D = x.shape
    for l in range(DEPTH):
        lam_init = 0.8 - 0.6 * math.exp(-0.3 * l)
        h = rms_norm(x, norm_mix_g[l])
        proj = h @ w_in[l]
        q, k, v, xr, yr, gate_logits = jnp.split(proj, SPLITS + (IN_COLS - 2 * D_MODEL + 0,), axis=-1)[:6] if False else jnp.split(proj, SPLITS, axis=-1)
        q = q.reshape(B, S, N_HEADS, 2, HEAD_DIM)
        k = k.reshape(B, S, N_HEADS, 2, HEAD_DIM)
        v = v.reshape(B, S, N_HEADS, V_DIM)
        lp = lambda_qk[l].astype(jnp.float32)
        lam = jnp.exp(jnp.sum(lp[0, 0] * lp[0, 1])) - jnp.exp(jnp.sum(lp[1, 0] * lp[1, 1])) + lam_init
        attn = diff_attention(q, k, v, lam, subln_g[l], lam_init)
        xr = causal_conv(xr, conv_w[l], conv_b[l])
        lru = rg_lru(xr, w_r[l], b_r[l], w_i[l], b_i[l], lru_lambda[l]) * jax.nn.gelu(yr)
        gates = jax.nn.sigmoid(gate_logits)
        g_attn = gates[..., :D_MODEL]
        g_lru = gates[..., D_MODEL:]
        merged = g_attn * (attn @ w_o_attn[l]) + g_lru * (lru @ w_o_lru[l])
        x = x + merged @ w_out[l]
        x = x + hier_moe(rms_norm(x, norm_ffn_g[l]), w_group[l], w_expert_router[l],
                         w_gate[l], w_up[l], w_down[l])
    return rms_norm(x, final_norm_g)
```

```python
import numpy as np
import ml_dtypes
from contextlib import ExitStack
import concourse.bass as bass
import concourse.mybir as mybir
from concourse.bass_utils import run_bass_kernel_spmd

F32 = mybir.dt.float32
BF16 = mybir.dt.bfloat16
I32 = mybir.dt.int32
AF = mybir.ActivationFunctionType
ALU = mybir.AluOpType

D = 1024
S = 8192
NB = 2
NH = 4
HD = 64
TOWN = 2048
NCORES = 8
EPS = 1e-6
EPOCH = 12000
KA = 68
SLOPES = [2.0 ** (-8.0 * (h + 1) / NH) for h in range(NH)]
LAM_INIT = 0.8 - 0.6 * 1.0
GELU_K = 0.7978845608028654


class Res:
    __slots__ = ("name", "w", "r")

    def __init__(self, name):
        self.name = name
        self.w = None
        self.r = {}


class EngState:
    def __init__(self, key, eng):
        self.key = key
        self.eng = eng
        self.count = 0
        self.sems = []
        self.known = {}


class FW:
    def __init__(self, nc, es):
        self.nc = nc
        self.es = es
        self.engs = {}
        for key, eng in (("pe", nc.tensor), ("act", nc.scalar), ("dve", nc.vector),
                         ("pool", nc.gpsimd), ("sp", nc.sync)):
            self.engs[key] = EngState(key, eng)
        self.NPOOL = 12
        self.dma_pool = {q: [es.enter_context(nc.semaphore("dq_%s%d" % (q, i))) for i in range(self.NPOOL)]
                         for q in ("sp", "pool")}
        self.dma_pool_uses = {q: [0] * self.NPOOL for q in ("sp", "pool")}
        self.dma_next = {"sp": 0, "pool": 0}
        self.dma_tokens = {}
        self.n_dma = 0

    def _sem_for(self, st, idx):
        e = idx // EPOCH
        while len(st.sems) <= e:
            st.sems.append(self.es.enter_context(
                self.nc.semaphore("e_%s_%d" % (st.key, len(st.sems)))))
        return st.sems[e], idx % EPOCH + 1

    def _wait(self, st, dep):
        key, idx = dep
        if key == "dma":
            if st.known.get(dep, False):
                return
            sem, val = self.dma_tokens[idx]
            st.eng.wait_ge(sem, val)
            st.known[dep] = True
            return
        if key == st.key and key == "pe":
            return
        if st.known.get(key, -1) >= idx:
            return
        sem, val = self._sem_for(self.engs[key], idx)
        st.eng.wait_ge(sem, val)
        st.known[key] = idx

    def _collect(self, st, reads, writes):
        deps = {}
        dma_deps = []

        def add(d):
            if d is None:
                return
            if d[0] == "dma":
                dma_deps.append(d)
            elif deps.get(d[0], -1) < d[1]:
                deps[d[0]] = d[1]
        for r in reads:
            add(r.w)
        for w in writes:
            add(w.w)
            for k, i in w.r.items():
                if k == "dma":
                    for tok in i:
                        add(("dma", tok))
                else:
                    add((k, i))
        for d in dma_deps:
            self._wait(st, d)
        for k, i in deps.items():
            self._wait(st, (k, i))

    def op(self, engkey, fn, reads=(), writes=(), inc=True):
        st = self.engs[engkey]
        self._collect(st, reads, writes)
        ins = fn(st.eng)
        idx = st.count
        if inc:
            sem, _ = self._sem_for(st, idx)
            ins.then_inc(sem, 1)
            st.count += 1
        for r in reads:
            r.r[engkey] = idx
        for w in writes:
            w.w = (engkey, idx)
            w.r = {}
        return ins

    def dma(self, engkey, out, in_, reads=(), writes=(), **kw):
        st = self.engs[engkey]
        self._collect(st, reads, writes)
        slot = self.dma_next[engkey]
        self.dma_next[engkey] = (slot + 1) % self.NPOOL
        sem = self.dma_pool[engkey][slot]
        prev = self.dma_pool_uses[engkey][slot]
        if prev > 0:
            st.eng.wait_ge(sem, 16 * prev)
        ins = st.eng.dma_start(out=out, in_=in_, **kw)
        ins.then_inc(sem, 16)
        self.dma_pool_uses[engkey][slot] = prev + 1
        tok = self.n_dma
        self.n_dma += 1
        self.dma_tokens[tok] = (sem, 16 * (prev + 1))
        for r in reads:
            r.r.setdefault("dma", []).append(tok)
        for w in writes:
            w.w = ("dma", tok)
            w.r = {}
        return tok

    def barrier(self):
        for st in self.engs.values():
            for k2, st2 in self.engs.items():
                if st2.count > 0 and k2 != st.key:
                    self._wait(st, (k2, st2.count - 1))
            for q in ("sp", "pool"):
                for slot in range(self.NPOOL):
                    u = self.dma_pool_uses[q][slot]
                    if u > 0:
                        st.eng.wait_ge(self.dma_pool[q][slot], 16 * u)

    def final_wait(self, engkey="sp"):
        st = self.engs[engkey]
        for q in ("sp", "pool"):
            for slot in range(self.NPOOL):
                u = self.dma_pool_uses[q][slot]
                if u > 0:
                    st.eng.wait_ge(self.dma_pool[q][slot], 16 * u)


def build(stage=99, debug=False):
    nc = bass.Bass("TRN2", target_bir_lowering=False)
    es = ExitStack()

    def din(name, shape, dt=F32):
        return nc.dram_tensor(name, list(shape), dt, kind="ExternalInput").ap()

    def dout(name, shape, dt=F32):
        return nc.dram_tensor(name, list(shape), dt, kind="ExternalOutput").ap()

    def dscr(name, shape, dt):
        return nc.dram_tensor(name, list(shape), dt, kind="Internal").ap()

    xn = din("xn", [D, S])
    xo = din("xo", [D, TOWN])
    g_mix = din("g_mix", [128, 8])
    w_in = din("w_in", [D, 4608])
    kaug = din("kaug", [4, S], BF16)
    qaug = din("qaug", [NH, 4, TOWN], BF16)
    cmask = din("cmask", [128, 4, 128], BF16)
    ident = din("ident", [128, 128], BF16)
    selj = din("selj", [128, 4])
    conv_w = din("conv_w", [128, 4, 4])
    conv_b = din("conv_b", [128, 4])
    wr_bd = din("wr_bd", [128, 4, 128])
    wi_bd = din("wi_bd", [128, 4, 128])
    b_r = din("b_r", [128, 4])
    b_i = din("b_i", [128, 4])
    lru_lam = din("lru_lam", [128, 4])
    xot = din("xot", [TOWN, D])
    lam_qk = din("lam_qk", [64, 4])
    subln = din("subln", [128, 1])
    w_o_attn = din("w_o_attn", [512, D])
    w_o_lru = din("w_o_lru", [512, D])
    w_out = din("w_out", [D, D])
    g_ffn = din("g_ffn", [128, 8])
    w_router = din("w_router", [D, 36])
    w_gate = din("w_gate", [32, D, 512])
    w_up = din("w_up", [32, D, 512])
    w_down = din("w_down", [32, 512, D])
    g_fin = din("g_fin", [128, D])
    g_ffn_rep = din("g_ffn_rep", [128, D])
    iota_s_d = din("iota_s", [128, 256])
    iota_t_d = din("iota_t", [128, TOWN])
    ustrict_d = din("ustrict", [128, 128], BF16)
    tvals_d = din("tvals", [128, 16, 8], BF16)

    dbg = {}
    if debug:
        dbg["kt"] = dout("dbg_kt", [8, 64, S], BF16)
        dbg["v"] = dout("dbg_v", [S, 512], BF16)
        dbg["lru"] = dout("dbg_lru", [128, 4, TOWN])
    out_d = dout("out", [TOWN, D])

    kt_scr = dbg["kt"] if debug else dscr("kt_scr", [8, 64, S], BF16)
    v_scr = dbg["v"] if debug else dscr("v_scr", [S, 512], BF16)

    with es:
        fw = FW(nc, es)

        KB = 1024
        BASE = 17 * KB
        DTB = {F32: 4, BF16: 2, I32: 4}
        cur = {"iv": [[0, 8 * KB]], "es": es}

        def set_free(intervals):
            cur["iv"] = [list(x) for x in intervals]

        def sb(name, shape, dt=F32):
            n = DTB[dt]
            for d_ in shape[1:]:
                n *= d_
            n = (n + 63) // 64 * 64
            for iv in cur["iv"]:
                if iv[1] - iv[0] >= n:
                    off = iv[0]
                    iv[0] += n
                    return nc.alloc_sbuf_tensor_at(name, list(shape), dt, offset=off + BASE)
            raise RuntimeError("SBUF arena full for %s (%d bytes) free=%s" % (name, n, cur["iv"]))

        def sb_at(name, shape, dt, off):
            return nc.alloc_sbuf_tensor_at(name, list(shape), dt, offset=off + BASE)

        def ps(name, shape, dt=F32):
            return cur["es"].enter_context(nc.psum_tensor(name, list(shape), dt))

        ones_bf = sb("ones_bf", [128, 128], BF16)
        r_ones = Res("ones")
        fw.op("pool", lambda e: e.memset(ones_bf[:], 1.0), writes=[r_ones])
        eps_sb = sb("eps_sb", [128, 1])
        one_sb = sb("one_sb", [128, 1])
        fw.op("pool", lambda e: e.memset(eps_sb[:], EPS), writes=[r_ones])
        fw.op("pool", lambda e: e.memset(one_sb[:], 1.0), writes=[r_ones])
        g_sb = sb("g_sb", [128, 8])
        r_g = Res("g")
        fw.dma("sp", g_sb[:], g_mix[:, :], writes=[r_g])
        Cw = sb("Cw", [128, 16, 32]); r_C = Res("C")
        selj_sb = sb("selj_sb", [128, 4])
        cw_sb = sb("cw_sb", [128, 4, 4])
        cb_sb = sb("cb_sb", [128, 4])
        br_sb = sb("br_sb", [128, 4])
        bi_sb = sb("bi_sb", [128, 4])
        lam_sb = sb("lam_sb", [128, 4])
        r_small = Res("small")
        for t_sb, t_d in ((selj_sb, selj), (cb_sb, conv_b), (br_sb, b_r), (bi_sb, b_i),
                          (lam_sb, lru_lam)):
            fw.dma("sp", t_sb[:], t_d[:, :], writes=[r_small])
        fw.dma("sp", cw_sb[:], conv_w[:, :, :], writes=[r_small])
        wr_sb = sb("wr_sb", [128, 4, 128], BF16)
        wi_sb = sb("wi_sb", [128, 4, 128], BF16)
        r_wgate = Res("wgate")
        fw.dma("pool", wr_sb[:], wr_bd[:, :, :], writes=[r_wgate])
        fw.dma("pool", wi_sb[:], wi_bd[:, :, :], writes=[r_wgate])

        ex = sb("ex", [128, 4])
        pl = sb("pl", [128, 4])
        hc = sb("hc", [128, 4])
        cc = sb("cc", [128, 4])
        hbr = sb("hbr", [128, 4])
        hbi = sb("hbi", [128, 4])
        r_const = Res("lruconst")
        fw.op("act", lambda e: e.activation(out=ex[:], in_=lam_sb[:], func=AF.Exp, scale=-1.0),
              reads=[r_small], writes=[r_const])
        fw.op("dve", lambda e: e.tensor_scalar(out=pl[:], in0=ex[:], scalar1=-0.25, scalar2=1.0 / 3.0,
                                               op0=ALU.mult, op1=ALU.add), reads=[r_const], writes=[r_const])
        fw.op("dve", lambda e: e.tensor_tensor(out=pl[:], in0=pl[:], in1=ex[:], op=ALU.mult),
              reads=[r_const], writes=[r_const])
        fw.op("dve", lambda e: e.tensor_scalar(out=pl[:], in0=pl[:], scalar1=-0.5, scalar2=None,
                                               op0=ALU.add), reads=[r_const], writes=[r_const])
        fw.op("dve", lambda e: e.tensor_tensor(out=pl[:], in0=pl[:], in1=ex[:], op=ALU.mult),
              reads=[r_const], writes=[r_const])
        fw.op("dve", lambda e: e.tensor_scalar(out=pl[:], in0=pl[:], scalar1=1.0, scalar2=None,
                                               op0=ALU.add), reads=[r_const], writes=[r_const])
        fw.op("dve", lambda e: e.tensor_tensor(out=pl[:], in0=pl[:], in1=ex[:], op=ALU.mult),
              reads=[r_const], writes=[r_const])
        fw.op("dve", lambda e: e.tensor_scalar(out=cc[:], in0=pl[:], scalar1=-8.0, scalar2=None,
                                               op0=ALU.mult), reads=[r_const], writes=[r_const])
        fw.op("dve", lambda e: e.tensor_scalar(out=hc[:], in0=pl[:], scalar1=-4.0, scalar2=None,
                                               op0=ALU.mult), reads=[r_const], writes=[r_const])
        fw.op("dve", lambda e: e.tensor_scalar(out=hbr[:], in0=br_sb[:], scalar1=0.5, scalar2=None,
                                               op0=ALU.mult), reads=[r_small], writes=[r_const])
        fw.op("dve", lambda e: e.tensor_scalar(out=hbi[:], in0=bi_sb[:], scalar1=0.5, scalar2=None,
                                               op0=ALU.mult), reads=[r_small], writes=[r_const])

        lru_own = sb_at("lru_own", [128, 4, TOWN], F32, 8 * KB)
        r_lru = Res("lru_own")
        qt = sb_at("qt", [128, 8, TOWN], BF16, 40 * KB)
        hn_own = sb_at("hn_own", [128, 8, TOWN], BF16, 72 * KB)
        lruA = sb_at("lruA", [128, 4, TOWN], BF16, 104 * KB)
        attnT = sb_at("attnT", [128, 4, TOWN], BF16, 120 * KB)
        merged = sb_at("merged", [128, 8, TOWN], BF16, 136 * KB)
        x1 = sb_at("x1", [128, 16, D], F32, 8 * KB)
        hn2k = sb_at("hn2k", [128, 16, D], BF16, 72 * KB)
        Mf = sb_at("Mf", [128, 16, 32], F32, 203 * KB)
        Mb = sb_at("Mb", [128, 16, 32], BF16, 205 * KB)
        r_M = Res("M")
        TOP = 207 * KB
        set_free([[40 * KB, TOP]])
        es1 = ExitStack()
        cur["es"] = es1
        wk_sb = sb("wk_sb", [128, 8, 512], BF16)
        wv_sb = sb("wv_sb", [128, 8, 512], BF16)
        wx_sb = sb("wx_sb", [128, 8, 512], BF16)
        r_w1 = Res("w1")
        w_in_v = w_in.rearrange("(c p) n -> p c n", p=128)
        for wsb, c0 in ((wk_sb, 512), (wv_sb, 1024), (wx_sb, 1536)):
            for c in range(8):
                fw.dma("pool", wsb[:, c, :], w_in_v[:, c, c0:c0 + 512], writes=[r_w1])

        TT = 512
        NT = S // TT
        xin = [sb("xin%d" % i, [128, 8, TT]) for i in range(2)]
        r_xin = [Res("xin%d" % i) for i in range(2)]
        xsq = sb("xsq", [128, 8, TT], BF16)
        r_xsq = Res("xsq")
        rb = sb("rb", [128, TT])
        r_rb = Res("rb")
        hn = [sb("hn%d" % i, [128, 8, TT], BF16) for i in range(2)]
        r_hn = [Res("hn%d" % i) for i in range(2)]
        kst = [sb("kst%d" % i, [64, 8, TT], BF16) for i in range(2)]
        r_kst = [Res("kst%d" % i) for i in range(2)]
        vst = [sb("vst%d" % i, [128, 4, 512], BF16) for i in range(2)]
        r_vst = [Res("vst%d" % i) for i in range(2)]
        xrp = [sb("xrp%d" % i, [128, 4, TT + 3]) for i in range(2)]
        r_xrp = [[Res("xrp%d_%d" % (i, g)) for g in range(4)] for i in range(2)]
        xcL = [sb("xc%d" % i, [128, TT]) for i in range(2)]; r_xcL = [Res("xc%d" % i) for i in range(2)]
        xcbL = [sb("xcb%d" % i, [128, TT], BF16) for i in range(2)]; r_xcbL = [Res("xcb%d" % i) for i in range(2)]
        thrL = [sb("thr%d" % i, [128, TT]) for i in range(2)]; r_thrL = [Res("thr%d" % i) for i in range(2)]
        thiL = [sb("thi%d" % i, [128, TT]) for i in range(2)]; r_thiL = [Res("thi%d" % i) for i in range(2)]
        a_tL = [sb("a_t%d" % i, [128, TT]) for i in range(2)]; r_aL = [Res("a%d" % i) for i in range(2)]
        a2_tL = [sb("a2_t%d" % i, [128, TT]) for i in range(2)]; r_a2L = [Res("a2%d" % i) for i in range(2)]
        u_tL = [sb("u_t%d" % i, [128, TT]) for i in range(2)]; r_uL = [Res("u%d" % i) for i in range(2)]
        h_t = [sb("h_t%d" % g, [128, TT]) for g in range(4)]
        r_h = [Res("h%d" % g) for g in range(4)]
        carry = sb("carry", [128, 4])
        r_carry = [Res("carry%d" % g) for g in range(4)]

        ps_ss = ps("ps_ss", [128, TT]); r_pss = Res("ps_ss")
        ps_k = [ps("ps_k%d" % i, [64, TT]) for i in range(2)]
        r_psk = [Res("ps_k%d" % i) for i in range(2)]
        ps_v = [ps("ps_v%d" % i, [128, 512]) for i in range(2)]
        r_psv = [Res("ps_v%d" % i) for i in range(2)]
        ps_x = ps("ps_x", [128, TT]); r_psx = Res("ps_x")
        ps_r = ps("ps_r", [128, TT]); r_psr = Res("ps_r")
        ps_i = ps("ps_i", [128, TT]); r_psi = Res("ps_i")

        for g in range(4):
            fw.op("pool", lambda e, g=g: e.memset(xrp[0][:, g, 0:3], 0.0), writes=[r_xrp[0][g]])

        xn_v = xn.rearrange("(c p) t -> p c t", p=128)
        cnt = {"k": 0, "v": 0}

        def xload(k):
            t0 = k * TT
            xb = xin[k % 2]; rxb = r_xin[k % 2]
            for c in range(0, 8, 4):
                fw.dma("sp", xb[:, c:c + 4, :], xn_v[:, c:c + 4, t0:t0 + TT], writes=[rxb])

        def front(k):
            xb = xin[k % 2]; rxb = r_xin[k % 2]
            hb = hn[k % 2]; rhb = r_hn[k % 2]
            fw.op("act", lambda e: e.activation(out=xsq[:], in_=xb[:], func=AF.Square),
                  reads=[rxb], writes=[r_xsq])
            for c in range(8):
                fw.op("pe", lambda e, c=c: e.matmul(ps_ss[:], lhsT=ones_bf[:], rhs=xsq[:, c, :],
                                                    start=(c == 0), stop=(c == 7)),
                      inc=(c == 7), reads=[r_ones, r_xsq], writes=[r_pss])
            fw.op("act", lambda e: e.activation(out=rb[:], in_=ps_ss[:], func=AF.Ln, bias=eps_sb[:, 0:1],
                                                scale=1.0 / D), reads=[r_pss, r_ones], writes=[r_rb])
            fw.op("act", lambda e: e.activation(out=rb[:], in_=rb[:], func=AF.Exp, scale=-0.5),
                  reads=[r_rb], writes=[r_rb])
            for c in range(8):
                fw.op("dve", lambda e, c=c: e.scalar_tensor_tensor(
                    out=hb[:, c, :], in0=xb[:, c, :], scalar=g_sb[:, c:c + 1], in1=rb[:],
                    op0=ALU.mult, op1=ALU.mult), reads=[rxb, r_g, r_rb], writes=[rhb])

        def kv_quarter(k, q):
            t0 = k * TT
            hb = hn[k % 2]; rhb = r_hn[k % 2]
            ks = kst[k % 2]; rks = r_kst[k % 2]
            vs = vst[k % 2]; rvs = r_vst[k % 2]
            for hm in (2 * q, 2 * q + 1):
                pk = ps_k[cnt["k"] % 2]; rpk = r_psk[cnt["k"] % 2]
                cnt["k"] += 1
                for c in range(8):
                    fw.op("pe", lambda e, c=c, hm=hm, pk=pk: e.matmul(
                        pk[:], lhsT=wk_sb[:, c, hm * 64:(hm + 1) * 64], rhs=hb[:, c, :],
                        start=(c == 0), stop=(c == 7)), inc=(c == 7), reads=[r_w1, rhb], writes=[rpk])
                fw.op("act", lambda e, hm=hm, pk=pk: e.activation(out=ks[:, hm, :], in_=pk[:], func=AF.Copy),
                      reads=[rpk], writes=[rks])
            tb = q
            pv = ps_v[cnt["v"] % 2]; rpv = r_psv[cnt["v"] % 2]
            cnt["v"] += 1
            for c in range(8):
                fw.op("pe", lambda e, c=c, tb=tb, pv=pv: e.matmul(
                    pv[:], lhsT=hb[:, c, tb * 128:(tb + 1) * 128], rhs=wv_sb[:, c, :],
                    start=(c == 0), stop=(c == 7)), inc=(c == 7), reads=[r_w1, rhb], writes=[rpv])
            fw.op("dve", lambda e, tb=tb, pv=pv: e.tensor_copy(out=vs[:, tb, :], in_=pv[:]),
                  reads=[rpv], writes=[rvs])
            if q == 3:
                fw.dma("pool", kt_scr[:, :, t0:t0 + TT].rearrange("h p t -> p h t"), ks[:, :, :], reads=[rks])
                fw.dma("pool", v_scr[t0:t0 + TT, :].rearrange("(b p) n -> p b n", p=128), vs[:, :, :], reads=[rvs])

        def lru_a(k, g):
            gi = g % 2
            xc = xcL[gi]; r_xc = r_xcL[gi]; xcb = xcbL[gi]; r_xcb = r_xcbL[gi]
            thr = thrL[gi]; r_thr = r_thrL[gi]; thi = thiL[gi]; r_thi = r_thiL[gi]
            a_t = a_tL[gi]; r_a = r_aL[gi]; a2_t = a2_tL[gi]; r_a2 = r_a2L[gi]; u_t = u_tL[gi]; r_u = r_uL[gi]
            hb = hn[k % 2]; rhb = r_hn[k % 2]
            xp = xrp[k % 2]; xp_n = xrp[(k + 1) % 2]
            rxp = r_xrp[k % 2][g]; rxpn = r_xrp[(k + 1) % 2][g]
            for c in range(8):
                fw.op("pe", lambda e, c=c, g=g: e.matmul(
                    ps_x[:], lhsT=wx_sb[:, c, g * 128:(g + 1) * 128], rhs=hb[:, c, :],
                    start=(c == 0), stop=(c == 7)), inc=(c == 7), reads=[r_w1, rhb], writes=[r_psx])
            fw.op("act", lambda e, g=g: e.activation(out=xp[:, g, 3:TT + 3], in_=ps_x[:], func=AF.Copy),
                  reads=[r_psx], writes=[rxp])
            fw.op("pool", lambda e, g=g: e.tensor_copy(out=xp_n[:, g, 0:3], in_=xp[:, g, TT:TT + 3]),
                  reads=[rxp], writes=[rxpn])
            fw.op("dve", lambda e, g=g: e.tensor_scalar(
                out=xc[:], in0=xp[:, g, 0:TT], scalar1=cw_sb[:, g, 0:1], scalar2=cb_sb[:, g:g + 1],
                op0=ALU.mult, op1=ALU.add), reads=[rxp, r_small], writes=[r_xc])
            for j in range(1, 4):
                fw.op("dve", lambda e, g=g, j=j: e.scalar_tensor_tensor(
                    out=xc[:], in0=xp[:, g, j:j + TT], scalar=cw_sb[:, g, j:j + 1], in1=xc[:],
                    op0=ALU.mult, op1=ALU.add), reads=[rxp, r_small, r_xc], writes=[r_xc])
            fw.op("pool", lambda e: e.tensor_copy(out=xcb[:], in_=xc[:]), reads=[r_xc], writes=[r_xcb])

        def lru_b(k, g):
            gi = g % 2
            xc = xcL[gi]; r_xc = r_xcL[gi]; xcb = xcbL[gi]; r_xcb = r_xcbL[gi]
            thr = thrL[gi]; r_thr = r_thrL[gi]; thi = thiL[gi]; r_thi = r_thiL[gi]
            a_t = a_tL[gi]; r_a = r_aL[gi]; a2_t = a2_tL[gi]; r_a2 = r_a2L[gi]; u_t = u_tL[gi]; r_u = r_uL[gi]
            fw.op("pe", lambda e, g=g: e.matmul(ps_r[:], lhsT=wr_sb[:, g, :], rhs=xcb[:], start=True, stop=True),
                  inc=True, reads=[r_wgate, r_xcb], writes=[r_psr])
            fw.op("pe", lambda e, g=g: e.matmul(ps_i[:], lhsT=wi_sb[:, g, :], rhs=xcb[:], start=True, stop=True),
                  inc=True, reads=[r_wgate, r_xcb], writes=[r_psi])
            fw.op("act", lambda e, g=g: e.activation(out=thr[:], in_=ps_r[:], func=AF.Tanh,
                                                     bias=hbr[:, g:g + 1], scale=0.5),
                  reads=[r_psr, r_const], writes=[r_thr])
            fw.op("act", lambda e, g=g: e.activation(out=thi[:], in_=ps_i[:], func=AF.Tanh,
                                                     bias=hbi[:, g:g + 1], scale=0.5),
                  reads=[r_psi, r_const], writes=[r_thi])
            fw.op("act", lambda e, g=g: e.activation(out=a_t[:], in_=thr[:], func=AF.Exp,
                                                     bias=hc[:, g:g + 1], scale=hc[:, g:g + 1]),
                  reads=[r_thr, r_const], writes=[r_a])
            fw.op("act", lambda e, g=g: e.activation(out=a2_t[:], in_=thr[:], func=AF.Exp,
                                                     bias=cc[:, g:g + 1], scale=cc[:, g:g + 1]),
                  reads=[r_thr, r_const], writes=[r_a2])
            fw.op("act", lambda e: e.activation(out=a2_t[:], in_=a2_t[:], func=AF.Ln, bias=one_sb[:, 0:1],
                                                scale=-1.0), reads=[r_a2, r_ones], writes=[r_a2])
            fw.op("act", lambda e: e.activation(out=a2_t[:], in_=a2_t[:], func=AF.Exp, scale=0.5),
                  reads=[r_a2], writes=[r_a2])
            if k == 0:
                fw.op("dve", lambda e: e.memset(a2_t[:, 0:1], 1.0), reads=[r_a2], writes=[r_a2])
            fw.op("dve", lambda e: e.scalar_tensor_tensor(out=u_t[:], in0=thi[:], scalar=1.0, in1=xc[:],
                                                          op0=ALU.add, op1=ALU.mult),
                  reads=[r_thi, r_xc], writes=[r_u])
            fw.op("dve", lambda e: e.scalar_tensor_tensor(out=u_t[:], in0=a2_t[:], scalar=0.5, in1=u_t[:],
                                                          op0=ALU.mult, op1=ALU.mult),
                  reads=[r_a2, r_u], writes=[r_u])
            if k == 0:
                fw.op("dve", lambda e, g=g: e.tensor_tensor_scan(out=h_t[g][:], data0=a_t[:], data1=u_t[:],
                                                                initial=0.0, op0=ALU.mult, op1=ALU.add),
                      reads=[r_a, r_u], writes=[r_h[g]])
            else:
                fw.op("dve", lambda e, g=g: e.tensor_copy(out=carry[:, g:g + 1], in_=h_t[g][:, TT - 1:TT]),
                      reads=[r_h[g]], writes=[r_carry[g]])
                fw.op("dve", lambda e, g=g: e.tensor_tensor_scan(out=h_t[g][:], data0=a_t[:], data1=u_t[:],
                                                                initial=carry[:, g:g + 1], op0=ALU.mult, op1=ALU.add),
                      reads=[r_a, r_u, r_carry[g]], writes=[r_h[g]])
            fw.op("dve", lambda e, g=g, k=k: e.tensor_scalar(
                out=lru_own[:, g, k * 128:(k + 1) * 128], in0=h_t[g][:, 0:128], scalar1=selj_sb[:, 0:1],
                scalar2=None, op0=ALU.mult), reads=[r_h[g], r_small], writes=[r_lru])
            for jj in range(1, 4):
                fw.op("dve", lambda e, g=g, k=k, jj=jj: e.scalar_tensor_tensor(
                    out=lru_own[:, g, k * 128:(k + 1) * 128], in0=h_t[g][:, jj * 128:(jj + 1) * 128],
                    scalar=selj_sb[:, jj:jj + 1], in1=lru_own[:, g, k * 128:(k + 1) * 128],
                    op0=ALU.mult, op1=ALU.add), reads=[r_h[g], r_small, r_lru], writes=[r_lru])

        xload(0)
        xload(1)
        front(0)
        for q in range(4):
            kv_quarter(0, q)
        for k in range(NT):
            lru_a(k, 0)
            if k + 1 < NT:
                front(k + 1)
            for g in range(4):
                if g + 1 < 4:
                    lru_a(k, g + 1)
                if g == 0 and k + 2 < NT:
                    xload(k + 2)
                if k + 1 < NT:
                    kv_quarter(k + 1, g)
                lru_b(k, g)
        fw.barrier()
        es1.close()

        set_free([[120 * KB, TOP]])
        es2 = ExitStack()
        cur["es"] = es2
        wq_sb = sb("wq_sb", [128, 8, 512], BF16)
        wy_sb = sb("wy_sb", [128, 8, 512], BF16)
        r_w2 = Res("w2")
        for wsb, c0 in ((wq_sb, 0), (wy_sb, 2048)):
            for c in range(8):
                fw.dma("pool", wsb[:, c, :], w_in_v[:, c, c0:c0 + 512], writes=[r_w2])
        r_qt = Res("qt")
        for h in range(NH):
            for m_ in range(2):
                fw.dma("sp", qt[64:68, 2 * h + m_, :], qaug[h, :, :], writes=[r_qt])
        xin2 = sb("xin2", [128, 8, TT]); r_xin2 = Res("xin2")
        xsq2 = sb("xsq2", [128, 8, TT], BF16); r_xsq2 = Res("xsq2")
        rb2 = sb("rb2", [128, TT]); r_rb2 = Res("rb2")
        ysb = sb("ysb", [128, TT]); r_ysb = Res("ysb")
        y2 = sb("y2", [128, TT]); r_y2 = Res("y2")
        thy = sb("thy", [128, TT]); r_thy = Res("thy")
        r_hno = Res("hn_own")
        r_lruA = Res("lruA")
        p2_ss = ps("p2_ss", [128, TT]); r_p2ss = Res("p2ss")
        p2_q = [ps("p2_q%d" % i, [64, TT]) for i in range(2)]
        r_p2q = [Res("p2q%d" % i) for i in range(2)]
        p2_y = [ps("p2_y%d" % i, [128, TT]) for i in range(2)]
        r_p2y = [Res("p2y%d" % i) for i in range(2)]
        xo_v = xo.rearrange("(c p) t -> p c t", p=128)
        for m in range(4):
            t0 = m * TT
            for c in range(0, 8, 4):
                fw.dma("sp", xin2[:, c:c + 4, :], xo_v[:, c:c + 4, t0:t0 + TT], writes=[r_xin2])
            fw.op("act", lambda e: e.activation(out=xsq2[:], in_=xin2[:], func=AF.Square),
                  reads=[r_xin2], writes=[r_xsq2])
            for c in range(8):
                fw.op("pe", lambda e, c=c: e.matmul(p2_ss[:], lhsT=ones_bf[:], rhs=xsq2[:, c, :],
                                                    start=(c == 0), stop=(c == 7)),
                      inc=(c == 7), reads=[r_ones, r_xsq2], writes=[r_p2ss])
            fw.op("act", lambda e: e.activation(out=rb2[:], in_=p2_ss[:], func=AF.Ln, bias=eps_sb[:, 0:1],
                                                scale=1.0 / D), reads=[r_p2ss, r_ones], writes=[r_rb2])
            fw.op("act", lambda e: e.activation(out=rb2[:], in_=rb2[:], func=AF.Exp, scale=-0.5),
                  reads=[r_rb2], writes=[r_rb2])
            for c in range(8):
                fw.op("dve", lambda e, c=c: e.scalar_tensor_tensor(
                    out=hn_own[:, c, t0:t0 + TT], in0=xin2[:, c, :], scalar=g_sb[:, c:c + 1], in1=rb2[:],
                    op0=ALU.mult, op1=ALU.mult), reads=[r_xin2, r_g, r_rb2], writes=[r_hno])
            for hm in range(8):
                pq = p2_q[hm % 2]; rpq = r_p2q[hm % 2]
                for c in range(8):
                    fw.op("pe", lambda e, c=c, hm=hm, pq=pq: e.matmul(
                        pq[:], lhsT=wq_sb[:, c, hm * 64:(hm + 1) * 64], rhs=hn_own[:, c, t0:t0 + TT],
                        start=(c == 0), stop=(c == 7)), inc=(c == 7), reads=[r_w2, r_hno], writes=[rpq])
                fw.op("act", lambda e, hm=hm, pq=pq: e.activation(out=qt[0:64, hm, t0:t0 + TT], in_=pq[:], func=AF.Copy),
                      reads=[rpq], writes=[r_qt])
            for g in range(4):
                py = p2_y[g % 2]; rpy = r_p2y[g % 2]
                for c in range(8):
                    fw.op("pe", lambda e, c=c, g=g, py=py: e.matmul(
                        py[:], lhsT=wy_sb[:, c, g * 128:(g + 1) * 128], rhs=hn_own[:, c, t0:t0 + TT],
                        start=(c == 0), stop=(c == 7)), inc=(c == 7), reads=[r_w2, r_hno], writes=[rpy])
                fw.op("act", lambda e, py=py: e.activation(out=ysb[:], in_=py[:], func=AF.Copy),
                      reads=[rpy], writes=[r_ysb])
                fw.op("act", lambda e, py=py: e.activation(out=y2[:], in_=py[:], func=AF.Square),
                      reads=[rpy], writes=[r_y2])
                fw.op("dve", lambda e: e.tensor_scalar(out=y2[:], in0=y2[:], scalar1=0.044715, scalar2=1.0,
                                                       op0=ALU.mult, op1=ALU.add), reads=[r_y2], writes=[r_y2])
                fw.op("dve", lambda e: e.tensor_tensor(out=y2[:], in0=y2[:], in1=ysb[:], op=ALU.mult),
                      reads=[r_y2, r_ysb], writes=[r_y2])
                fw.op("act", lambda e: e.activation(out=thy[:], in_=y2[:], func=AF.Tanh, scale=GELU_K),
                      reads=[r_y2], writes=[r_thy])
                fw.op("dve", lambda e: e.scalar_tensor_tensor(out=thy[:], in0=thy[:], scalar=1.0, in1=ysb[:],
                                                              op0=ALU.add, op1=ALU.mult),
                      reads=[r_thy, r_ysb], writes=[r_thy])
                fw.op("dve", lambda e, g=g: e.scalar_tensor_tensor(
                    out=lruA[:, g, t0:t0 + TT], in0=thy[:], scalar=0.5, in1=lru_own[:, g, t0:t0 + TT],
                    op0=ALU.mult, op1=ALU.mult), reads=[r_thy, r_lru], writes=[r_lruA])
        fw.barrier()
        es2.close()

        set_free([[136 * KB, TOP]])
        es3 = ExitStack()
        cur["es"] = es3
        kt = [sb_at("kt%d" % i, [128, S], BF16, 8 * KB + i * 16 * KB) for i in range(2)]
        r_kt = [Res("kt%d" % i) for i in range(2)]
        vh = sb("vh", [128, 64, 128], BF16); r_vh = Res("vh")
        pt = [sb("pt%d" % i, [128, 2, 512], BF16) for i in range(2)]
        r_pt = [Res("pt%d" % i) for i in range(2)]
        rl = sb("rl", [128, 2, 512]); r_rl = Res("rl")
        dd = sb("dd", [128, 512]); r_dd = Res("dd")
        tmp1 = sb("tmp1", [128, 512]); r_tmp1 = Res("tmp1")
        sq3 = sb("sq3", [128, 512], BF16); r_sq3 = Res("sq3")
        rs3 = sb("rs3", [128, 512]); r_rs3 = Res("rs3")
        cm_sb = sb("cm_sb", [128, 4, 128], BF16); r_cm = Res("cm")
        lp = sb("lp", [64, 4]); lpp = sb("lpp", [64, 2]); ones64 = sb("ones64", [64, 128])
        nlam = sb("nlam", [128, 1]); elam = sb("elam", [128, 2]); gs = sb("gs", [128, 1])
        r_lam = Res("lam")
        fw.dma("sp", cm_sb[:], cmask[:, :, :], writes=[r_cm])
        ident_sb = sb("ident_sb", [128, 128], BF16)
        negm = sb("negm", [128, 4, 128], BF16); r_negm = Res("negm")
        fw.dma("sp", ident_sb[:], ident[:, :], writes=[r_negm])
        fw.op("dve", lambda e: e.tensor_scalar(out=negm[:], in0=cm_sb[:], scalar1=-1.0, scalar2=30000.0,
                                               op0=ALU.add, op1=ALU.mult), reads=[r_cm, r_negm], writes=[r_negm])
        fw.dma("sp", lp[:], lam_qk[:, :], writes=[r_lam])
        fw.dma("sp", gs[:], subln[:, :], writes=[r_lam])
        for i in range(2):
            fw.dma("sp", kt[i][64:68, :], kaug[:, :], writes=[r_kt[i]])
        ps_s = [ps("ps_s%d" % i, [128, 2, 512]) for i in range(2)]
        r_pss3 = [Res("ps_s%d" % i) for i in range(2)]
        po = ps("po", [128, 2, 512]); r_po = Res("po")
        pl_ = ps("pl_", [128, 2, 512]); r_pl = Res("pl")
        fw.op("pool", lambda e: e.memset(ones64[:], 1.0), writes=[r_lam])
        fw.op("dve", lambda e: e.tensor_tensor(out=lpp[:, 0:1], in0=lp[:, 0:1], in1=lp[:, 1:2], op=ALU.mult),
              reads=[r_lam], writes=[r_lam])
        fw.op("dve", lambda e: e.tensor_tensor(out=lpp[:, 1:2], in0=lp[:, 2:3], in1=lp[:, 3:4], op=ALU.mult),
              reads=[r_lam], writes=[r_lam])
        fw.op("pe", lambda e: e.matmul(ps_s[0][:, 0, 0:2], lhsT=ones64[:], rhs=lpp[:], start=True, stop=True),
              inc=True, reads=[r_lam], writes=[r_pss3[0]])
        fw.op("act", lambda e: e.activation(out=elam[:], in_=ps_s[0][:, 0, 0:2], func=AF.Exp),
              reads=[r_pss3[0]], writes=[r_lam])
        fw.op("dve", lambda e: e.tensor_tensor(out=nlam[:], in0=elam[:, 1:2], in1=elam[:, 0:1], op=ALU.subtract),
              reads=[r_lam], writes=[r_lam])
        fw.op("dve", lambda e: e.tensor_scalar(out=nlam[:], in0=nlam[:], scalar1=-LAM_INIT, scalar2=None,
                                               op0=ALU.add), reads=[r_lam], writes=[r_lam])
        fw.op("dve", lambda e: e.tensor_scalar(out=gs[:], in0=gs[:], scalar1=1.0 - LAM_INIT, scalar2=None,
                                               op0=ALU.mult), reads=[r_lam], writes=[r_lam])
        r_attn = Res("attnT")

        def load_k(h):
            for m_ in range(2):
                fw.dma("sp", kt[m_][0:64, :], kt_scr[2 * h + m_, :, :], writes=[r_kt[m_]])

        def load_v(h):
            for q4 in range(4):
                fw.dma("sp", vh[:, q4 * 16:(q4 + 1) * 16, :],
                       v_scr[q4 * 2048:(q4 + 1) * 2048, h * 128:(h + 1) * 128].rearrange("(b p) v -> p b v", p=128),
                       writes=[r_vh])

        steps = []
        for h in range(NH):
            for m in range(4):
                nkb = 16 * m + 16
                for kb in range(nkb):
                    qmin = max(0, (kb - 16 * m) // 4) if kb >= 16 * m else 0
                    steps.append(dict(h=h, m=m, kb=kb, c0=128 * qmin, nkb=nkb, diag=(kb >= 16 * m),
                                      jj=(kb - 16 * m) % 4, i=len(steps)))

        def emit_qk(st):
            h, m, kb, c0 = st["h"], st["m"], st["kb"], st["c0"]
            pss = ps_s[st["i"] % 2]; rps = r_pss3[st["i"] % 2]
            for m_ in range(2):
                fw.op("pe", lambda e, m_=m_: e.matmul(
                    pss[:, m_, c0:512], lhsT=kt[m_][0:KA, kb * 128:(kb + 1) * 128],
                    rhs=qt[0:KA, 2 * h + m_, m * 512 + c0:(m + 1) * 512], start=True, stop=(not st["diag"])),
                    inc=(not st["diag"]), reads=[r_kt[m_], r_qt], writes=[rps])
                if st["diag"]:
                    fw.op("pe", lambda e, m_=m_: e.matmul(
                        pss[:, m_, c0:c0 + 128], lhsT=ident_sb[:], rhs=negm[:, st["jj"], :], start=False, stop=True),
                        inc=True, reads=[r_negm], writes=[rps])

        def emit_exp(st):
            c0 = st["c0"]
            pss = ps_s[st["i"] % 2]; rps = r_pss3[st["i"] % 2]
            ptb = pt[st["i"] % 2]; rptb = r_pt[st["i"] % 2]
            fw.op("act", lambda e: e.activation(out=ptb[:, :, c0:512], in_=pss[:, :, c0:512], func=AF.Exp, scale=0.125),
                  reads=[rps], writes=[rptb])

        def emit_pv(st):
            kb, c0, nkb = st["kb"], st["c0"], st["nkb"]
            ptb = pt[st["i"] % 2]; rptb = r_pt[st["i"] % 2]
            for m_ in range(2):
                fw.op("pe", lambda e, m_=m_: e.matmul(
                    po[:, m_, c0:512], lhsT=vh[:, kb, :], rhs=ptb[:, m_, c0:512],
                    start=(kb == 0), stop=(kb == nkb - 1)), inc=(kb == nkb - 1), reads=[r_vh, rptb], writes=[r_po])
                fw.op("pe", lambda e, m_=m_: e.matmul(
                    pl_[:, m_, c0:512], lhsT=ones_bf[:], rhs=ptb[:, m_, c0:512],
                    start=(kb == 0), stop=(kb == nkb - 1)), inc=(m_ == 1 or kb == nkb - 1), reads=[r_ones, rptb], writes=[r_pl])

        def finalize(h, m, sbuf_i):
            pfin = ps_s[sbuf_i]; rpfin = r_pss3[sbuf_i]
            fw.op("dve", lambda e: e.reciprocal(out=rl[:], in_=pl_[:]), reads=[r_pl], writes=[r_rl])
            fw.op("dve", lambda e: e.tensor_tensor(out=dd[:], in0=po[:, 0, :], in1=rl[:, 0, :], op=ALU.mult),
                  reads=[r_po, r_rl], writes=[r_dd])
            fw.op("dve", lambda e: e.tensor_tensor(out=tmp1[:], in0=po[:, 1, :], in1=rl[:, 1, :], op=ALU.mult),
                  reads=[r_po, r_rl], writes=[r_tmp1])
            fw.op("dve", lambda e: e.scalar_tensor_tensor(out=dd[:], in0=tmp1[:], scalar=nlam[:, 0:1], in1=dd[:],
                                                          op0=ALU.mult, op1=ALU.add),
                  reads=[r_tmp1, r_lam, r_dd], writes=[r_dd])
            fw.op("act", lambda e: e.activation(out=sq3[:], in_=dd[:], func=AF.Square),
                  reads=[r_dd], writes=[r_sq3])
            fw.op("pe", lambda e: e.matmul(pfin[:, 0, :], lhsT=ones_bf[:], rhs=sq3[:], start=True, stop=True),
                  inc=True, reads=[r_ones, r_sq3], writes=[rpfin])
            fw.op("act", lambda e: e.activation(out=rs3[:], in_=pfin[:, 0, :], func=AF.Ln, bias=eps_sb[:, 0:1],
                                                scale=1.0 / 128.0), reads=[rpfin, r_ones], writes=[r_rs3])
            fw.op("act", lambda e: e.activation(out=rs3[:], in_=rs3[:], func=AF.Exp, scale=-0.5),
                  reads=[r_rs3], writes=[r_rs3])
            fw.op("dve", lambda e: e.scalar_tensor_tensor(
                out=attnT[:, h, m * 512:(m + 1) * 512], in0=dd[:], scalar=gs[:, 0:1], in1=rs3[:],
                op0=ALU.mult, op1=ALU.mult), reads=[r_dd, r_lam, r_rs3], writes=[r_attn])

        load_k(0)
        load_v(0)
        emit_qk(steps[0])
        for i, st in enumerate(steps):
            nxt = steps[i + 1] if i + 1 < len(steps) else None
            newh = nxt is not None and nxt["h"] != st["h"]
            if newh:
                load_k(nxt["h"])
            if nxt is not None:
                emit_qk(nxt)
            emit_exp(st)
            emit_pv(st)
            if newh:
                load_v(nxt["h"])
            if st["kb"] == st["nkb"] - 1:
                finalize(st["h"], st["m"], st["i"] % 2)
        fw.barrier()
        es3.close()

        set_free([[8 * KB, 72 * KB], [168 * KB, TOP]])
        es4 = ExitStack()
        cur["es"] = es4
        woa = sb("woa", [128, 4, D], BF16)
        wol = sb("wol", [128, 4, D], BF16)
        r_w4 = Res("w4")
        woa_v = w_o_attn.rearrange("(c p) n -> p c n", p=128)
        wol_v = w_o_lru.rearrange("(c p) n -> p c n", p=128)
        for c in range(4):
            fw.dma("pool", woa[:, c, :], woa_v[:, c, :], writes=[r_w4])
            fw.dma("pool", wol[:, c, :], wol_v[:, c, :], writes=[r_w4])
        wgA = sb("wgA", [128, 8, D], BF16)
        wgL = sb("wgL", [128, 8, D], BF16)
        r_wgA = Res("wgA")
        gstg = [sb("gstg%d" % i, [128, D]) for i in range(3)]
        r_gstg = [Res("gstg%d" % i) for i in range(3)]
        gi = 0
        for c in range(8):
            for dst, c0 in ((wgA, 2560), (wgL, 3584)):
                sg = gstg[gi % 3]; rsg = r_gstg[gi % 3]
                fw.dma("sp", sg[:], w_in_v[:, c, c0:c0 + D], writes=[rsg])
                if gi % 2 == 0:
                    fw.op("act", lambda e, sg=sg, dst=dst, c=c: e.activation(out=dst[:, c, :], in_=sg[:], func=AF.Copy),
                          reads=[rsg], writes=[r_wgA])
                else:
                    fw.op("dve", lambda e, sg=sg, dst=dst, c=c: e.tensor_copy(out=dst[:, c, :], in_=sg[:]),
                          reads=[rsg], writes=[r_wgA])
                gi += 1
        thA = sb("thA", [128, TT]); r_thA = Res("thA")
        thL = sb("thL", [128, TT]); r_thL = Res("thL")
        m1 = sb("m1", [128, TT]); r_m1 = Res("m1")
        m2 = sb("m2", [128, TT]); r_m2 = Res("m2")
        r_mg = Res("merged")
        pA = ps("pA", [128, TT]); r_pA = Res("pA")
        pL = ps("pL", [128, TT]); r_pL = Res("pL")
        pGA = ps("pGA", [128, TT]); r_pGA = Res("pGA")
        pGL = ps("pGL", [128, TT]); r_pGL = Res("pGL")
        for f in range(8):
            for m in range(4):
                t0 = m * TT
                for c in range(4):
                    fw.op("pe", lambda e, c=c, f=f, t0=t0: e.matmul(
                        pA[:], lhsT=woa[:, c, f * 128:(f + 1) * 128], rhs=attnT[:, c, t0:t0 + TT],
                        start=(c == 0), stop=(c == 3)), inc=(c == 3), reads=[r_w4, r_attn], writes=[r_pA])
                for c in range(4):
                    fw.op("pe", lambda e, c=c, f=f, t0=t0: e.matmul(
                        pL[:], lhsT=wol[:, c, f * 128:(f + 1) * 128], rhs=lruA[:, c, t0:t0 + TT],
                        start=(c == 0), stop=(c == 3)), inc=(c == 3), reads=[r_w4, r_lruA], writes=[r_pL])
                for c in range(8):
                    fw.op("pe", lambda e, c=c, f=f, t0=t0: e.matmul(
                        pGA[:], lhsT=wgA[:, c, f * 128:(f + 1) * 128], rhs=hn_own[:, c, t0:t0 + TT],
                        start=(c == 0), stop=(c == 7)), inc=(c == 7), reads=[r_wgA, r_hno], writes=[r_pGA])
                for c in range(8):
                    fw.op("pe", lambda e, c=c, f=f, t0=t0: e.matmul(
                        pGL[:], lhsT=wgL[:, c, f * 128:(f + 1) * 128], rhs=hn_own[:, c, t0:t0 + TT],
                        start=(c == 0), stop=(c == 7)), inc=(c == 7), reads=[r_wgA, r_hno], writes=[r_pGL])
                fw.op("act", lambda e: e.activation(out=thA[:], in_=pGA[:], func=AF.Tanh, scale=0.5),
                      reads=[r_pGA], writes=[r_thA])
                fw.op("act", lambda e: e.activation(out=thL[:], in_=pGL[:], func=AF.Tanh, scale=0.5),
                      reads=[r_pGL], writes=[r_thL])
                fw.op("dve", lambda e: e.scalar_tensor_tensor(out=m1[:], in0=thA[:], scalar=1.0, in1=pA[:],
                                                              op0=ALU.add, op1=ALU.mult),
                      reads=[r_thA, r_pA], writes=[r_m1])
                fw.op("dve", lambda e: e.scalar_tensor_tensor(out=m2[:], in0=thL[:], scalar=1.0, in1=pL[:],
                                                              op0=ALU.add, op1=ALU.mult),
                      reads=[r_thL, r_pL], writes=[r_m2])
                fw.op("dve", lambda e, f=f, t0=t0: e.tensor_tensor(out=merged[:, f, t0:t0 + TT], in0=m1[:], in1=m2[:],
                                                                  op=ALU.add), reads=[r_m1, r_m2], writes=[r_mg])
        fw.barrier()
        es4.close()

        set_free([[104 * KB, 136 * KB], [168 * KB, 203 * KB]])
        es5 = ExitStack()
        cur["es"] = es5
        wout = sb("wout", [128, 8, D], BF16); r_wo = Res("wout")
        wout_v = w_out.rearrange("(c p) n -> p c n", p=128)
        for c in range(8):
            fw.dma("pool", wout[:, c, :], wout_v[:, c, :], writes=[r_wo])
        x1T = sb("x1T", [128, 8, TT]); r_x1T = Res("x1T")
        xtok = [sb("xtok%d" % i, [128, D]) for i in range(2)]
        r_xtok = [Res("xtok%d" % i) for i in range(2)]
        xoc = [sb("xoc%d" % i, [128, TT]) for i in range(2)]
        r_xoc = [Res("xoc%d" % i) for i in range(2)]
        g2rep = sb("g2rep", [128, D]); r_g2rep = Res("g2rep")
        fw.dma("sp", g2rep[:], g_ffn_rep[:, :], writes=[r_g2rep])
        wr_f = sb("wr_f", [128, 8, 36]); r_wr = Res("wr")
        g2_sb = sb("g2_sb", [128, 8])
        lg = sb("lg", [128, 36]); r_lg = Res("lg")
        rt = sb("rt", [128, 64]); r_rt = Res("rt")
        junk = sb("junk", [128, D], BF16); r_junk = Res("junk")
        fw.dma("sp", g2_sb[:], g_ffn[:, :], writes=[r_wr])
        fw.dma("sp", wr_f[:], w_router.rearrange("(c p) n -> p c n", p=128), writes=[r_wr])
        for c in range(8):
            fw.op("dve", lambda e, c=c: e.tensor_scalar(out=wr_f[:, c, :], in0=wr_f[:, c, :], scalar1=g2_sb[:, c:c + 1],
                                                        scalar2=None, op0=ALU.mult), reads=[r_wr], writes=[r_wr])
        r_x1 = Res("x1")
        r_hn2 = Res("hn2k")
        p_o = [ps("p_o%d" % i, [128, 512]) for i in range(2)]
        r_p_o = [Res("p_o%d" % i) for i in range(2)]
        p_t = [ps("p_t%d" % i, [128, 512]) for i in range(2)]
        r_p_t = [Res("p_t%d" % i) for i in range(2)]
        p_lg = ps("p_lg", [128, 4, 64]); r_p_lg = Res("p_lg")
        xot_v = xot.rearrange("(b p) d -> p b d", p=128)
        ocnt = 0
        for m in range(4):
            t0 = m * TT
            for tb in range(4):
                blk = 4 * m + tb
                xt_ = xtok[blk % 2]; rxt = r_xtok[blk % 2]
                fw.dma("sp", xt_[:], xot_v[:, blk, :], writes=[rxt])
                for half in range(2):
                    po_ = p_o[ocnt % 2]; rpo_ = r_p_o[ocnt % 2]
                    ocnt += 1
                    for f in range(8):
                        fw.op("pe", lambda e, f=f, tb=tb, half=half, po_=po_, t0=t0: e.matmul(
                            po_[:], lhsT=merged[:, f, t0 + tb * 128:t0 + (tb + 1) * 128],
                            rhs=wout[:, f, half * 512:(half + 1) * 512], start=(f == 0), stop=(f == 7)),
                            inc=(f == 7), reads=[r_mg, r_wo], writes=[rpo_])
                    fw.op("dve", lambda e, blk=blk, half=half, po_=po_, xt_=xt_: e.scalar_tensor_tensor(
                        out=x1[:, blk, half * 512:(half + 1) * 512], in0=po_[:], scalar=0.5,
                        in1=xt_[:, half * 512:(half + 1) * 512], op0=ALU.mult, op1=ALU.add),
                        reads=[rpo_, rxt], writes=[r_x1])
            for f2 in range(8):
                pt_ = p_t[f2 % 2]; rpt_ = r_p_t[f2 % 2]
                xc_ = xoc[f2 % 2]; rxc_ = r_xoc[f2 % 2]
                fw.dma("sp", xc_[:], xo_v[:, f2, t0:t0 + TT], writes=[rxc_])
                for f in range(8):
                    fw.op("pe", lambda e, f=f, f2=f2, pt_=pt_, t0=t0: e.matmul(
                        pt_[:], lhsT=wout[:, f, f2 * 128:(f2 + 1) * 128], rhs=merged[:, f, t0:t0 + TT],
                        start=(f == 0), stop=(f == 7)), inc=(f == 7), reads=[r_mg, r_wo], writes=[rpt_])
                fw.op("dve", lambda e, f2=f2, pt_=pt_, xc_=xc_: e.scalar_tensor_tensor(
                    out=x1T[:, f2, :], in0=pt_[:], scalar=0.5, in1=xc_[:], op0=ALU.mult, op1=ALU.add),
                    reads=[rpt_, rxc_], writes=[r_x1T])
            for tb in range(4):
                for c in range(8):
                    fw.op("pe", lambda e, c=c, tb=tb: e.matmul(
                        p_lg[:, tb, 0:36], lhsT=x1T[:, c, tb * 128:(tb + 1) * 128], rhs=wr_f[:, c, :],
                        start=(c == 0), stop=(c == 7)), inc=(c == 7), reads=[r_x1T, r_wr], writes=[r_p_lg])
            for tb in range(4):
                blk = 4 * m + tb
                R = lambda a, b: rt[:, a:b]
                fw.op("act", lambda e, blk=blk: e.activation(out=junk[:], in_=x1[:, blk, :], func=AF.Square,
                                                             accum_out=rt[:, 0:1]),
                      reads=[r_x1], writes=[r_junk, r_rt])
                fw.op("act", lambda e: e.activation(out=rt[:, 1:2], in_=rt[:, 0:1], func=AF.Ln, bias=eps_sb[:, 0:1],
                                                    scale=1.0 / D), reads=[r_rt, r_ones], writes=[r_rt])
                fw.op("act", lambda e: e.activation(out=rt[:, 1:2], in_=rt[:, 1:2], func=AF.Exp, scale=-0.5),
                      reads=[r_rt], writes=[r_rt])
                fw.op("dve", lambda e, tb=tb: e.tensor_scalar(out=lg[:], in0=p_lg[:, tb, 0:36], scalar1=rt[:, 1:2],
                                                              scalar2=None, op0=ALU.mult),
                      reads=[r_p_lg, r_rt], writes=[r_lg])
                fw.op("dve", lambda e, blk=blk: e.scalar_tensor_tensor(
                    out=hn2k[:, blk, :], in0=x1[:, blk, :], scalar=rt[:, 1:2], in1=g2rep[:],
                    op0=ALU.mult, op1=ALU.mult), reads=[r_x1, r_rt, r_g2rep], writes=[r_hn2])
                fw.op("dve", lambda e: e.reduce_max(out=rt[:, 2:3], in_=lg[:, 0:4], axis=mybir.AxisListType.X),
                      reads=[r_lg, r_rt], writes=[r_rt])
                fw.op("dve", lambda e: e.tensor_scalar(out=rt[:, 3:4], in0=rt[:, 2:3], scalar1=-1.0, scalar2=None,
                                                       op0=ALU.mult), reads=[r_rt], writes=[r_rt])
                fw.op("act", lambda e: e.activation(out=rt[:, 4:8], in_=lg[:, 0:4], func=AF.Exp, bias=rt[:, 3:4],
                                                    accum_out=rt[:, 8:9]), reads=[r_lg, r_rt], writes=[r_rt])
                fw.op("dve", lambda e: e.reciprocal(out=rt[:, 9:10], in_=rt[:, 8:9]), reads=[r_rt], writes=[r_rt])
                fw.op("dve", lambda e: e.tensor_scalar(out=rt[:, 10:14], in0=lg[:, 0:4], scalar1=rt[:, 2:3], scalar2=None,
                                                       op0=ALU.is_ge), reads=[r_lg, r_rt], writes=[r_rt])
                fw.op("dve", lambda e: e.tensor_scalar(out=rt[:, 16:24], in0=lg[:, 4:12], scalar1=rt[:, 10:11], scalar2=None,
                                                       op0=ALU.mult), reads=[r_lg, r_rt], writes=[r_rt])
                for g in range(1, 4):
                    fw.op("dve", lambda e, g=g: e.scalar_tensor_tensor(
                        out=rt[:, 16:24], in0=lg[:, 4 + 8 * g:12 + 8 * g], scalar=rt[:, 10 + g:11 + g], in1=rt[:, 16:24],
                        op0=ALU.mult, op1=ALU.add), reads=[r_lg, r_rt], writes=[r_rt])
                fw.op("dve", lambda e: e.reduce_max(out=rt[:, 24:25], in_=rt[:, 16:24], axis=mybir.AxisListType.X),
                      reads=[r_rt], writes=[r_rt])
                fw.op("dve", lambda e: e.tensor_scalar(out=rt[:, 32:40], in0=rt[:, 16:24], scalar1=rt[:, 24:25], scalar2=None,
                                                       op0=ALU.is_ge), reads=[r_rt], writes=[r_rt])
                fw.op("dve", lambda e: e.scalar_tensor_tensor(out=rt[:, 40:48], in0=rt[:, 32:40], scalar=-1e30, in1=rt[:, 16:24],
                                                              op0=ALU.mult, op1=ALU.add), reads=[r_rt], writes=[r_rt])
                fw.op("dve", lambda e: e.reduce_max(out=rt[:, 25:26], in_=rt[:, 40:48], axis=mybir.AxisListType.X),
                      reads=[r_rt], writes=[r_rt])
                fw.op("dve", lambda e: e.tensor_scalar(out=rt[:, 48:56], in0=rt[:, 40:48], scalar1=rt[:, 25:26], scalar2=None,
                                                       op0=ALU.is_ge), reads=[r_rt], writes=[r_rt])
                fw.op("dve", lambda e: e.tensor_tensor(out=rt[:, 26:27], in0=rt[:, 25:26], in1=rt[:, 24:25], op=ALU.subtract),
                      reads=[r_rt], writes=[r_rt])
                fw.op("act", lambda e: e.activation(out=rt[:, 27:28], in_=rt[:, 26:27], func=AF.Exp),
                      reads=[r_rt], writes=[r_rt])
                fw.op("dve", lambda e: e.tensor_scalar(out=rt[:, 28:29], in0=rt[:, 27:28], scalar1=1.0, scalar2=None,
                                                       op0=ALU.add), reads=[r_rt], writes=[r_rt])
                fw.op("dve", lambda e: e.reciprocal(out=rt[:, 28:29], in_=rt[:, 28:29]), reads=[r_rt], writes=[r_rt])
                fw.op("dve", lambda e: e.tensor_tensor(out=rt[:, 29:30], in0=rt[:, 27:28], in1=rt[:, 28:29], op=ALU.mult),
                      reads=[r_rt], writes=[r_rt])
                fw.op("dve", lambda e: e.tensor_tensor(out=rt[:, 28:29], in0=rt[:, 28:29], in1=rt[:, 9:10], op=ALU.mult),
                      reads=[r_rt], writes=[r_rt])
                fw.op("dve", lambda e: e.tensor_tensor(out=rt[:, 29:30], in0=rt[:, 29:30], in1=rt[:, 9:10], op=ALU.mult),
                      reads=[r_rt], writes=[r_rt])
                fw.op("dve", lambda e: e.tensor_scalar(out=rt[:, 56:64], in0=rt[:, 32:40], scalar1=rt[:, 28:29], scalar2=None,
                                                       op0=ALU.mult), reads=[r_rt], writes=[r_rt])
                fw.op("dve", lambda e: e.scalar_tensor_tensor(out=rt[:, 56:64], in0=rt[:, 48:56], scalar=rt[:, 29:30],
                                                              in1=rt[:, 56:64], op0=ALU.mult, op1=ALU.add),
                      reads=[r_rt], writes=[r_rt])
                for g in range(4):
                    fw.op("dve", lambda e, g=g, blk=blk: e.tensor_scalar(
                        out=Cw[:, blk, 8 * g:8 * g + 8], in0=rt[:, 56:64], scalar1=rt[:, 10 + g:11 + g], scalar2=None,
                        op0=ALU.mult), reads=[r_rt], writes=[r_C])
                fw.op("dve", lambda e: e.tensor_tensor(out=rt[:, 40:48], in0=rt[:, 32:40], in1=rt[:, 48:56], op=ALU.add),
                      reads=[r_rt], writes=[r_rt])
                for g in range(4):
                    fw.op("dve", lambda e, g=g, blk=blk: e.tensor_scalar(
                        out=Mf[:, blk, 8 * g:8 * g + 8], in0=rt[:, 40:48], scalar1=rt[:, 10 + g:11 + g], scalar2=None,
                        op0=ALU.mult), reads=[r_rt], writes=[r_M])
                fw.op("dve", lambda e, blk=blk: e.tensor_copy(out=Mb[:, blk, :], in_=Mf[:, blk, :]),
                      reads=[r_M], writes=[r_M])
        fw.barrier()
        es5.close()

        CAP = 256
        set_free([[104 * KB, 203 * KB]])
        es6 = ExitStack()
        cur["es"] = es6
        wgb = sb("wgb", [128, 8, 512], BF16); wub = sb("wub", [128, 8, 512], BF16); wdb = sb("wdb", [128, 4, D], BF16)
        r_wgb = Res("wgb"); r_wub = Res("wub"); r_wdb = Res("wdb")
        NSTG = 4
        stg = [sb("stg%d" % i, [128, 2, 512]) for i in range(NSTG)]
        r_stg = [Res("stg%d" % i) for i in range(NSTG)]
        iota_s = sb("iota_s", [128, CAP]); iota_t = sb("iota_t", [128, TOWN])
        ustr = sb("ustr", [128, 128], BF16); tv = sb("tv", [128, 16, 8], BF16)
        r_k5 = Res("k5")
        fw.dma("sp", iota_s[:], iota_s_d[:, :], writes=[r_k5])
        fw.dma("sp", iota_t[:], iota_t_d[:, :], writes=[r_k5])
        fw.dma("sp", ustr[:], ustrict_d[:, :], writes=[r_k5])
        fw.dma("sp", tv[:], tvals_d[:, :, :], writes=[r_k5])
        pfx = sb("pfx", [128, 16, 32]); r_pfx = Res("pfx")
        Sel = sb("Sel", [128, 16, CAP], BF16); r_Sel = Res("Sel")
        XgT = sb("XgT", [128, 8, CAP], BF16); r_XgT = Res("XgT")
        HT = sb("HT", [128, 4, CAP], BF16); r_HT = Res("HT")
        SelT = [sb("SelT%d" % i, [128, 2, TOWN], BF16) for i in range(2)]
        r_SelT = [Res("SelT%d" % i) for i in range(2)]
        Yb = [sb("Yb%d" % i, [128, 2, D], BF16) for i in range(2)]
        r_Yb = [Res("Yb%d" % i) for i in range(2)]
        tokf = sb("tokf", [128, 2]); r_tokf = Res("tokf")
        tokc = sb("tokc", [128, 2, 2]); r_tokc = Res("tokc")
        s1 = [sb("s1_%d" % i, [128, CAP]) for i in range(2)]
        r_s1 = [Res("s1_%d" % i) for i in range(2)]
        th5 = [sb("th5_%d" % i, [128, CAP]) for i in range(2)]
        r_th5 = [Res("th5_%d" % i) for i in range(2)]

        pA_ = [ps("pA5_%d" % i, [128, 512]) for i in range(2)]
        r_pA_ = [Res("pA5_%d" % i) for i in range(2)]
        pGU = [ps("pGU%d" % i, [128, 512]) for i in range(2)]
        r_pGU = [Res("pGU%d" % i) for i in range(2)]
        pTok = ps("pTok", [128, 16, 32]); r_pTok = Res("pTok")
        pC = [ps("pC%d" % i, [128, 512]) for i in range(3)]
        r_pC = [Res("pC%d" % i) for i in range(3)]

        for blk in range(16):
            for b2 in range(blk + 1):
                fw.op("pe", lambda e, blk=blk, b2=b2: e.matmul(
                    pTok[:, blk, :], lhsT=(ustr[:] if b2 == blk else ones_bf[:]), rhs=Mb[:, b2, :],
                    start=(b2 == 0), stop=(b2 == blk)), inc=(b2 == blk), reads=[r_M, r_k5, r_ones], writes=[r_pTok])
        fw.op("dve", lambda e: e.tensor_copy(out=pfx[:], in_=pTok[:]), reads=[r_pTok], writes=[r_pfx])

        cnt5 = {"stg": 0, "gu": 0, "c": 0}

        wq5 = {"todo": [], "issued": []}

        def w_begin(ex_):
            wg_v = w_gate[ex_].rearrange("(c p) n -> p c n", p=128)
            wu_v = w_up[ex_].rearrange("(c p) n -> p c n", p=128)
            wd_v = w_down[ex_].rearrange("(c p) n -> p c n", p=128)
            pcs = []
            for c in range(0, 8, 2):
                pcs.append((wg_v[:, c:c + 2, :], wgb[:, c:c + 2, :], r_wgb))
            for c in range(0, 8, 2):
                pcs.append((wu_v[:, c:c + 2, :], wub[:, c:c + 2, :], r_wub))
            for half in range(2):
                for f in range(0, 4, 2):
                    pcs.append((wd_v[:, f:f + 2, half * 512:(half + 1) * 512],
                                wdb[:, f:f + 2, half * 512:(half + 1) * 512], r_wdb))
            assert not wq5["todo"] and not wq5["issued"]
            wq5["todo"] = pcs

        def w_issue():
            if not wq5["todo"]:
                return
            src, dst, rdst = wq5["todo"].pop(0)
            sg = stg[cnt5["stg"] % NSTG]; rsg = r_stg[cnt5["stg"] % NSTG]
            cnt5["stg"] += 1
            fw.dma("sp", sg[:], src, writes=[rsg])
            wq5["issued"].append((sg, rsg, dst, rdst))

        def w_cast():
            if not wq5["issued"]:
                return
            sg, rsg, dst, rdst = wq5["issued"].pop(0)
            fw.op("act", lambda e: e.activation(out=dst, in_=sg[:], func=AF.Copy), reads=[rsg], writes=[rdst])

        def stage_a(ex_):
            sT = SelT[ex_ % 2]; rsT = r_SelT[ex_ % 2]
            yb = Yb[ex_ % 2]; ryb = r_Yb[ex_ % 2]
            for blk in range(16):
                fw.op("dve", lambda e, blk=blk: e.tensor_scalar(
                    out=Sel[:, blk, :], in0=iota_s[:], scalar1=pfx[:, blk, ex_:ex_ + 1], scalar2=Mf[:, blk, ex_:ex_ + 1],
                    op0=ALU.is_equal, op1=ALU.mult), reads=[r_k5, r_pfx, r_M], writes=[r_Sel])
            yield
            for s_ in range(2):
                for blk in range(16):
                    fw.op("pe", lambda e, s_=s_, blk=blk: e.matmul(
                        pTok[:, s_, 0:8], lhsT=Sel[:, blk, s_ * 128:(s_ + 1) * 128], rhs=tv[:, blk, :],
                        start=(blk == 0), stop=(blk == 15)), inc=(blk == 15), reads=[r_Sel, r_k5], writes=[r_pTok])
            fw.op("act", lambda e: e.activation(out=tokc[:], in_=pTok[:, 0:2, 0:2], func=AF.Copy),
                  reads=[r_pTok], writes=[r_tokc])
            for s_ in range(2):
                fw.op("dve", lambda e, s_=s_: e.scalar_tensor_tensor(
                    out=tokf[:, s_:s_ + 1], in0=tokc[:, s_, 0:1], scalar=64.0, in1=tokc[:, s_, 1:2],
                    op0=ALU.mult, op1=ALU.add), reads=[r_tokc], writes=[r_tokf])
            for s_ in range(2):
                fw.op("dve", lambda e, s_=s_: e.tensor_scalar(
                    out=sT[:, s_, :], in0=iota_t[:], scalar1=tokf[:, s_:s_ + 1], scalar2=None, op0=ALU.is_equal),
                    reads=[r_k5, r_tokf], writes=[rsT])
            yield
            for half in range(2):
                for c2 in range(4):
                    c = 4 * half + c2
                    pa = pA_[c2 // 2]; rpa = r_pA_[c2 // 2]
                    for blk in range(16):
                        fw.op("pe", lambda e, c=c, c2=c2, blk=blk, pa=pa: e.matmul(
                            pa[:, (c2 % 2) * CAP:(c2 % 2 + 1) * CAP], lhsT=hn2k[:, blk, c * 128:(c + 1) * 128], rhs=Sel[:, blk, :],
                            start=(blk == 0), stop=(blk == 15)), inc=(blk == 15), reads=[r_hn2, r_Sel], writes=[rpa])
                    fw.op("act", lambda e, c=c, c2=c2, pa=pa: e.activation(
                        out=XgT[:, c, :], in_=pa[:, (c2 % 2) * CAP:(c2 % 2 + 1) * CAP], func=AF.Copy),
                        reads=[rpa], writes=[r_XgT])
                    yield
            for f in range(4):
                b_ = cnt5["gu"] % 2
                cnt5["gu"] += 1
                for c in range(8):
                    fw.op("pe", lambda e, c=c, f=f, b_=b_: e.matmul(
                        pGU[b_][:, 0:CAP], lhsT=wgb[:, c, f * 128:(f + 1) * 128], rhs=XgT[:, c, :],
                        start=(c == 0), stop=(c == 7)), inc=(c == 7), reads=[r_wgb, r_XgT], writes=[r_pGU[b_]])
                for c in range(8):
                    fw.op("pe", lambda e, c=c, f=f, b_=b_: e.matmul(
                        pGU[b_][:, CAP:2 * CAP], lhsT=wub[:, c, f * 128:(f + 1) * 128], rhs=XgT[:, c, :],
                        start=(c == 0), stop=(c == 7)), inc=(c == 7), reads=[r_wub, r_XgT], writes=[r_pGU[b_]])
                fw.op("act", lambda e, b_=b_: e.activation(out=th5[b_][:], in_=pGU[b_][:, 0:CAP], func=AF.Tanh, scale=0.5),
                      reads=[r_pGU[b_]], writes=[r_th5[b_]])
                fw.op("dve", lambda e, b_=b_: e.scalar_tensor_tensor(out=s1[b_][:], in0=th5[b_][:], scalar=1.0,
                                                                    in1=pGU[b_][:, 0:CAP], op0=ALU.add, op1=ALU.mult),
                      reads=[r_th5[b_], r_pGU[b_]], writes=[r_s1[b_]])
                fw.op("dve", lambda e, b_=b_, f=f: e.scalar_tensor_tensor(
                    out=HT[:, f, :], in0=s1[b_][:], scalar=0.5, in1=pGU[b_][:, CAP:2 * CAP], op0=ALU.mult, op1=ALU.mult),
                    reads=[r_s1[b_], r_pGU[b_]], writes=[r_HT])
                yield
            for s_ in range(2):
                for half in range(2):
                    pa = pA_[half]; rpa = r_pA_[half]
                    for f in range(4):
                        fw.op("pe", lambda e, f=f, s_=s_, half=half, pa=pa: e.matmul(
                            pa[:], lhsT=HT[:, f, s_ * 128:(s_ + 1) * 128], rhs=wdb[:, f, half * 512:(half + 1) * 512],
                            start=(f == 0), stop=(f == 3)), inc=(f == 3), reads=[r_HT, r_wdb], writes=[rpa])
                    fw.op("act", lambda e, s_=s_, half=half, pa=pa: e.activation(
                        out=yb[:, s_, half * 512:(half + 1) * 512], in_=pa[:], func=AF.Copy),
                        reads=[rpa], writes=[ryb])
                    yield

        def stage_b_units(ex_):
            sT = SelT[ex_ % 2]; rsT = r_SelT[ex_ % 2]
            yb = Yb[ex_ % 2]; ryb = r_Yb[ex_ % 2]
            units = []
            for blk in range(16):
                for half in range(2):
                    def unit(blk=blk, half=half):
                        ci = cnt5["c"] % 3
                        cnt5["c"] += 1
                        for s_ in range(2):
                            fw.op("pe", lambda e, s_=s_: e.matmul(
                                pC[ci][:], lhsT=sT[:, s_, blk * 128:(blk + 1) * 128],
                                rhs=yb[:, s_, half * 512:(half + 1) * 512], start=(s_ == 0), stop=(s_ == 1)),
                                inc=(s_ == 1), reads=[rsT, ryb], writes=[r_pC[ci]])
                        fw.op("dve", lambda e: e.scalar_tensor_tensor(
                            out=x1[:, blk, half * 512:(half + 1) * 512], in0=pC[ci][:], scalar=Cw[:, blk, ex_:ex_ + 1],
                            in1=x1[:, blk, half * 512:(half + 1) * 512], op0=ALU.mult, op1=ALU.add),
                            reads=[r_pC[ci], r_C, r_x1], writes=[r_x1])
                    units.append(unit)
            return units

        w_begin(0)
        for _ in range(3):
            w_issue()
        for _ in stage_a(0):
            w_cast()
            w_issue()
        for ex_ in range(32):
            units = stage_b_units(ex_)
            if ex_ + 1 < 32:
                w_begin(ex_ + 1)
                for _ in range(3):
                    w_issue()
                ui = 0
                for _ in stage_a(ex_ + 1):
                    w_cast()
                    w_issue()
                    for _k in range(2):
                        if ui < len(units):
                            units[ui]()
                            ui += 1
                while wq5["todo"] or wq5["issued"]:
                    w_cast()
                    w_issue()
                while ui < len(units):
                    units[ui]()
                    ui += 1
            else:
                for u in units:
                    u()
        fw.barrier()
        es6.close()

        set_free([[104 * KB, 203 * KB]])
        es7 = ExitStack()
        cur["es"] = es7
        gF = sb("gF", [128, D]); r_gF = Res("gF")
        ob = [sb("ob%d" % i, [128, D]) for i in range(2)]
        r_ob = [Res("ob%d" % i) for i in range(2)]
        fin = sb("fin", [128, 4]); r_fin = Res("fin")
        fw.dma("sp", gF[:], g_fin[:, :], writes=[r_gF])
        out_v = out_d.rearrange("(b p) d -> p b d", p=128)
        for blk in range(16):
            o_ = ob[blk % 2]; ro_ = r_ob[blk % 2]
            fw.op("act", lambda e, blk=blk, o_=o_: e.activation(out=o_[:], in_=x1[:, blk, :], func=AF.Square,
                                                               accum_out=fin[:, 0:1]),
                  reads=[r_x1], writes=[ro_, r_fin])
            fw.op("act", lambda e: e.activation(out=fin[:, 1:2], in_=fin[:, 0:1], func=AF.Ln, bias=eps_sb[:, 0:1],
                                                scale=1.0 / D), reads=[r_fin, r_ones], writes=[r_fin])
            fw.op("act", lambda e: e.activation(out=fin[:, 1:2], in_=fin[:, 1:2], func=AF.Exp, scale=-0.5),
                  reads=[r_fin], writes=[r_fin])
            fw.op("dve", lambda e, blk=blk, o_=o_: e.scalar_tensor_tensor(
                out=o_[:], in0=x1[:, blk, :], scalar=fin[:, 1:2], in1=gF[:], op0=ALU.mult, op1=ALU.mult),
                reads=[r_x1, r_fin, r_gF], writes=[ro_])
            fw.dma("sp", out_v[:, blk, :], o_[:], reads=[ro_])
        fw.barrier()
        fw.final_wait("sp")
        es7.close()
    return nc


def _bf(a):
    return np.asarray(a, dtype=np.float32).astype(ml_dtypes.bfloat16)


def make_in_maps(inputs):
    x = np.asarray(inputs["x"], dtype=np.float32)
    f = lambda k: np.asarray(inputs[k], dtype=np.float32)
    pc = lambda v: np.ascontiguousarray(v.reshape(-1, 128).T)
    g_mix = pc(f("norm_mix_g")[0])
    w_in = np.ascontiguousarray(f("w_in")[0])
    conv_w = np.ascontiguousarray(f("conv_w")[0].reshape(4, 4, 128).transpose(2, 1, 0))
    conv_b = pc(f("conv_b")[0])

    def bd(w):
        o = np.zeros((128, 4, 128), np.float32)
        for n in range(8):
            g, hlf = n // 2, n % 2
            o[hlf * 64:(hlf + 1) * 64, g, hlf * 64:(hlf + 1) * 64] = w[n]
        return o
    wr_bd = bd(f("w_r")[0])
    wi_bd = bd(f("w_i")[0])
    tk = np.arange(S)
    kaug = _bf(np.stack([tk // 64, tk % 64, np.ones(S), np.ones(S)]).astype(np.float32))
    w_o_attn = np.ascontiguousarray(f("w_o_attn")[0])
    w_o_lru = np.ascontiguousarray(f("w_o_lru")[0])
    w_out_ = np.ascontiguousarray(f("w_out")[0])
    g_ffn = pc(f("norm_ffn_g")[0])
    w_router = np.ascontiguousarray(np.concatenate(
        [f("w_group")[0], f("w_expert_router")[0].transpose(1, 0, 2).reshape(D, 32)], axis=1))
    w_gate_ = np.ascontiguousarray(f("w_gate")[0])
    w_up_ = np.ascontiguousarray(f("w_up")[0])
    w_down_ = np.ascontiguousarray(f("w_down")[0])
    g_fin = np.ascontiguousarray(np.tile(f("final_norm_g")[None, :], (128, 1)))
    g_ffn_rep = np.ascontiguousarray(np.tile(f("norm_ffn_g")[0][None, :], (128, 1)))
    iota_s = np.ascontiguousarray(np.tile(np.arange(256, dtype=np.float32)[None, :], (128, 1)))
    iota_t = np.ascontiguousarray(np.tile(np.arange(1, TOWN + 1, dtype=np.float32)[None, :], (128, 1)))
    ustrict = _bf((np.arange(128)[:, None] < np.arange(128)[None, :]).astype(np.float32))
    code = (np.arange(16)[None, :] * 128 + np.arange(128)[:, None] + 1)
    tvals = _bf(np.stack([code // 64, code % 64] + [np.zeros_like(code)] * 6, axis=-1).astype(np.float32))
    maps = []
    for c in range(NCORES):
        b, j = c // 4, c % 4
        xn = np.ascontiguousarray(x[b].T)
        own_blocks = [4 * i + j for i in range(16)]
        tq = np.concatenate([np.arange(bl * 128, (bl + 1) * 128) for bl in own_blocks])
        xo = np.ascontiguousarray(x[b][tq].T)
        qa = np.zeros((NH, 4, TOWN), np.float32)
        for h in range(NH):
            s8 = SLOPES[h] * 8.0
            qa[h, 0] = 64.0 * s8
            qa[h, 1] = s8
            qa[h, 2] = -64.0 * s8 * (tq // 64)
            qa[h, 3] = -s8 * (tq % 64)
        cm = np.zeros((128, 4, 128), np.float32)
        for jj in range(4):
            if jj < j:
                cm[:, jj, :] = 1.0
            elif jj == j:
                cm[:, jj, :] = (np.arange(128)[:, None] <= np.arange(128)[None, :])
        sj = np.zeros((128, 4), np.float32)
        sj[:, j] = 1.0
        extra = {
            "xot": np.ascontiguousarray(x[b][tq]),
            "lam_qk": np.ascontiguousarray(f("lambda_qk")[0].reshape(4, 64).T),
            "subln": np.ascontiguousarray(f("subln_g")[0].reshape(128, 1)),
            "w_o_attn": w_o_attn, "w_o_lru": w_o_lru, "w_out": w_out_, "g_ffn": g_ffn,
            "w_router": w_router, "w_gate": w_gate_, "w_up": w_up_, "w_down": w_down_, "g_fin": g_fin, "g_ffn_rep": g_ffn_rep,
            "iota_s": iota_s, "iota_t": iota_t, "ustrict": ustrict, "tvals": tvals,
        }
        maps.append({
            "xn": xn, "xo": xo, "g_mix": g_mix, "w_in": w_in, "kaug": kaug, "qaug": _bf(qa),
            "cmask": _bf(cm), "ident": _bf(np.eye(128, dtype=np.float32)), "selj": sj, "conv_w": conv_w, "conv_b": conv_b,
            "wr_bd": wr_bd, "wi_bd": wi_bd, "b_r": pc(f("b_r")[0]), "b_i": pc(f("b_i")[0]),
            "lru_lam": pc(f("lru_lambda")[0]), **extra,
        })
    return maps


def kernel(**inputs):
    nc = build()
    in_maps = make_in_maps(inputs)
    res = run_bass_kernel_spmd(nc, in_maps, core_ids=list(range(NCORES)))
    out = np.zeros((NB, S, D), np.float32)
    for c in range(NCORES):
        b, j = c // 4, c % 4
        o = np.asarray(res.results[c]["out"])
        for i in range(16):
            bl = 4 * i + j
            out[b, bl * 128:(bl + 1) * 128, :] = o[i * 128:(i + 1) * 128, :]
    return out
```

```python
import numpy as np
import ml_dtypes
from contextlib import ExitStack
import concourse.bass as bass
import concourse.mybir as mybir
from concourse.bass_utils import run_bass_kernel_spmd

F32 = mybir.dt.float32
BF16 = mybir.dt.bfloat16
I32 = mybir.dt.int32
AF = mybir.ActivationFunctionType
ALU = mybir.AluOpType

D = 1024
S = 8192
NB = 2
NH = 4
HD = 64
TOWN = 2048
NCORES = 8
EPS = 1e-6
EPOCH = 12000
KA = 68
SLOPES = [2.0 ** (-8.0 * (h + 1) / NH) for h in range(NH)]
LAM_INIT = 0.8 - 0.6 * 1.0
GELU_K = 0.7978845608028654


class Res:
    __slots__ = ("name", "w", "r")

    def __init__(self, name):
        self.name = name
        self.w = None
        self.r = {}


class EngState:
    def __init__(self, key, eng):
        self.key = key
        self.eng = eng
        self.count = 0
        self.sems = []
        self.known = {}


class FW:
    def __init__(self, nc, es):
        self.nc = nc
        self.es = es
        self.engs = {}
        for key, eng in (("pe", nc.tensor), ("act", nc.scalar), ("dve", nc.vector),
                         ("pool", nc.gpsimd), ("sp", nc.sync)):
            self.engs[key] = EngState(key, eng)
        self.NPOOL = 12
        self.dma_pool = {q: [es.enter_context(nc.semaphore("dq_%s%d" % (q, i))) for i in range(self.NPOOL)]
                         for q in ("sp", "pool")}
        self.dma_pool_uses = {q: [0] * self.NPOOL for q in ("sp", "pool")}
        self.dma_next = {"sp": 0, "pool": 0}
        self.dma_tokens = {}
        self.n_dma = 0

    def _sem_for(self, st, idx):
        e = idx // EPOCH
        while len(st.sems) <= e:
            st.sems.append(self.es.enter_context(
                self.nc.semaphore("e_%s_%d" % (st.key, len(st.sems)))))
        return st.sems[e], idx % EPOCH + 1

    def _wait(self, st, dep):
        key, idx = dep
        if key == "dma":
            if st.known.get(dep, False):
                return
            sem, val = self.dma_tokens[idx]
            st.eng.wait_ge(sem, val)
            st.known[dep] = True
            return
        if key == st.key and key == "pe":
            return
        if st.known.get(key, -1) >= idx:
            return
        sem, val = self._sem_for(self.engs[key], idx)
        st.eng.wait_ge(sem, val)
        st.known[key] = idx

    def _collect(self, st, reads, writes):
        deps = {}
        dma_deps = []

        def add(d):
            if d is None:
                return
            if d[0] == "dma":
                dma_deps.append(d)
            elif deps.get(d[0], -1) < d[1]:
                deps[d[0]] = d[1]
        for r in reads:
            add(r.w)
        for w in writes:
            add(w.w)
            for k, i in w.r.items():
                if k == "dma":
                    for tok in i:
                        add(("dma", tok))
                else:
                    add((k, i))
        for d in dma_deps:
            self._wait(st, d)
        for k, i in deps.items():
            self._wait(st, (k, i))

    def op(self, engkey, fn, reads=(), writes=(), inc=True):
        if getattr(self, "defer", None) is not None:
            self.defer.append((engkey, fn, list(reads), list(writes), inc))
            return None
        st = self.engs[engkey]
        self._collect(st, reads, writes)
        ins = fn(st.eng)
        idx = st.count
        if inc:
            sem, _ = self._sem_for(st, idx)
            ins.then_inc(sem, 1)
            st.count += 1
        for r in reads:
            r.r[engkey] = idx
        for w in writes:
            w.w = (engkey, idx)
            w.r = {}
        return ins

    def dma(self, engkey, out, in_, reads=(), writes=(), **kw):
        st = self.engs[engkey]
        self._collect(st, reads, writes)
        slot = self.dma_next[engkey]
        self.dma_next[engkey] = (slot + 1) % self.NPOOL
        sem = self.dma_pool[engkey][slot]
        prev = self.dma_pool_uses[engkey][slot]
        if prev > 0:
            st.eng.wait_ge(sem, 16 * prev)
        ins = st.eng.dma_start(out=out, in_=in_, **kw)
        ins.then_inc(sem, 16)
        self.dma_pool_uses[engkey][slot] = prev + 1
        tok = self.n_dma
        self.n_dma += 1
        self.dma_tokens[tok] = (sem, 16 * (prev + 1))
        for r in reads:
            r.r.setdefault("dma", []).append(tok)
        for w in writes:
            w.w = ("dma", tok)
            w.r = {}
        return tok

    def barrier(self):
        for st in self.engs.values():
            for k2, st2 in self.engs.items():
                if st2.count > 0 and k2 != st.key:
                    self._wait(st, (k2, st2.count - 1))
            for q in ("sp", "pool"):
                for slot in range(self.NPOOL):
                    u = self.dma_pool_uses[q][slot]
                    if u > 0:
                        st.eng.wait_ge(self.dma_pool[q][slot], 16 * u)

    def final_wait(self, engkey="sp"):
        st = self.engs[engkey]
        for q in ("sp", "pool"):
            for slot in range(self.NPOOL):
                u = self.dma_pool_uses[q][slot]
                if u > 0:
                    st.eng.wait_ge(self.dma_pool[q][slot], 16 * u)


def build(stage=99, debug=False):
    nc = bass.Bass("TRN2", target_bir_lowering=False)
    es = ExitStack()

    def din(name, shape, dt=F32):
        return nc.dram_tensor(name, list(shape), dt, kind="ExternalInput").ap()

    def dout(name, shape, dt=F32):
        return nc.dram_tensor(name, list(shape), dt, kind="ExternalOutput").ap()

    def dscr(name, shape, dt):
        return nc.dram_tensor(name, list(shape), dt, kind="Internal").ap()

    xn = din("xn", [D, S])
    xo = din("xo", [D, TOWN])
    g_mix = din("g_mix", [128, 8])
    w_in = din("w_in", [D, 4608])
    kaug = din("kaug", [4, S], BF16)
    qaug = din("qaug", [NH, 4, TOWN], BF16)
    cmask = din("cmask", [128, 4, 128], BF16)
    ident = din("ident", [128, 128], BF16)
    selj = din("selj", [128, 4])
    conv_w = din("conv_w", [128, 4, 4])
    conv_b = din("conv_b", [128, 4])
    wr_bd = din("wr_bd", [128, 4, 128])
    wi_bd = din("wi_bd", [128, 4, 128])
    b_r = din("b_r", [128, 4])
    b_i = din("b_i", [128, 4])
    lru_lam = din("lru_lam", [128, 4])
    xot = din("xot", [TOWN, D])
    lam_qk = din("lam_qk", [64, 4])
    subln = din("subln", [128, 1])
    w_o_attn = din("w_o_attn", [512, D])
    w_o_lru = din("w_o_lru", [512, D])
    w_out = din("w_out", [D, D])
    g_ffn = din("g_ffn", [128, 8])
    w_router = din("w_router", [D, 36])
    w_gate = din("w_gate", [32, D, 512])
    w_up = din("w_up", [32, D, 512])
    w_down = din("w_down", [32, 512, D])
    g_fin = din("g_fin", [128, D])
    g_ffn_rep = din("g_ffn_rep", [128, D])
    iota_s_d = din("iota_s", [128, 256])
    iota_t_d = din("iota_t", [128, TOWN])
    ustrict_d = din("ustrict", [128, 128], BF16)
    tvals_d = din("tvals", [128, 16, 8], BF16)

    dbg = {}
    if debug:
        dbg["kt"] = dout("dbg_kt", [8, 64, S], BF16)
        dbg["v"] = dout("dbg_v", [S, 512], BF16)
        dbg["lru"] = dout("dbg_lru", [128, 4, TOWN])
    out_d = dout("out", [TOWN, D])

    kt_scr = dbg["kt"] if debug else dscr("kt_scr", [8, 64, S], BF16)
    v_scr = dbg["v"] if debug else dscr("v_scr", [S, 512], BF16)

    with es:
        fw = FW(nc, es)

        KB = 1024
        BASE = 17 * KB
        DTB = {F32: 4, BF16: 2, I32: 4}
        cur = {"iv": [[0, 8 * KB]], "es": es}

        def set_free(intervals):
            cur["iv"] = [list(x) for x in intervals]

        def sb(name, shape, dt=F32):
            n = DTB[dt]
            for d_ in shape[1:]:
                n *= d_
            n = (n + 63) // 64 * 64
            for iv in cur["iv"]:
                if iv[1] - iv[0] >= n:
                    off = iv[0]
                    iv[0] += n
                    return nc.alloc_sbuf_tensor_at(name, list(shape), dt, offset=off + BASE)
            raise RuntimeError("SBUF arena full for %s (%d bytes) free=%s" % (name, n, cur["iv"]))

        def sb_at(name, shape, dt, off):
            return nc.alloc_sbuf_tensor_at(name, list(shape), dt, offset=off + BASE)

        def ps(name, shape, dt=F32):
            return cur["es"].enter_context(nc.psum_tensor(name, list(shape), dt))

        ones_bf = sb("ones_bf", [128, 128], BF16)
        r_ones = Res("ones")
        fw.op("pool", lambda e: e.memset(ones_bf[:], 1.0), writes=[r_ones])
        eps_sb = sb("eps_sb", [128, 1])
        one_sb = sb("one_sb", [128, 1])
        fw.op("pool", lambda e: e.memset(eps_sb[:], EPS), writes=[r_ones])
        fw.op("pool", lambda e: e.memset(one_sb[:], 1.0), writes=[r_ones])
        g_sb = sb("g_sb", [128, 8])
        r_g = Res("g")
        fw.dma("sp", g_sb[:], g_mix[:, :], writes=[r_g])
        Cw = sb("Cw", [128, 16, 32]); r_C = Res("C")
        selj_sb = sb("selj_sb", [128, 4])
        cw_sb = sb("cw_sb", [128, 4, 4])
        cb_sb = sb("cb_sb", [128, 4])
        br_sb = sb("br_sb", [128, 4])
        bi_sb = sb("bi_sb", [128, 4])
        lam_sb = sb("lam_sb", [128, 4])
        r_small = Res("small")
        for t_sb, t_d in ((selj_sb, selj), (cb_sb, conv_b), (br_sb, b_r), (bi_sb, b_i),
                          (lam_sb, lru_lam)):
            fw.dma("sp", t_sb[:], t_d[:, :], writes=[r_small])
        fw.dma("sp", cw_sb[:], conv_w[:, :, :], writes=[r_small])
        wr_sb = sb("wr_sb", [128, 4, 128], BF16)
        wi_sb = sb("wi_sb", [128, 4, 128], BF16)
        r_wgate = Res("wgate")
        fw.dma("pool", wr_sb[:], wr_bd[:, :, :], writes=[r_wgate])
        fw.dma("pool", wi_sb[:], wi_bd[:, :, :], writes=[r_wgate])

        ex = sb("ex", [128, 4])
        pl = sb("pl", [128, 4])
        hc = sb("hc", [128, 4])
        cc = sb("cc", [128, 4])
        hbr = sb("hbr", [128, 4])
        hbi = sb("hbi", [128, 4])
        r_const = Res("lruconst")
        fw.op("act", lambda e: e.activation(out=ex[:], in_=lam_sb[:], func=AF.Exp, scale=-1.0),
              reads=[r_small], writes=[r_const])
        fw.op("dve", lambda e: e.tensor_scalar(out=pl[:], in0=ex[:], scalar1=-0.25, scalar2=1.0 / 3.0,
                                               op0=ALU.mult, op1=ALU.add), reads=[r_const], writes=[r_const])
        fw.op("dve", lambda e: e.tensor_tensor(out=pl[:], in0=pl[:], in1=ex[:], op=ALU.mult),
              reads=[r_const], writes=[r_const])
        fw.op("dve", lambda e: e.tensor_scalar(out=pl[:], in0=pl[:], scalar1=-0.5, scalar2=None,
                                               op0=ALU.add), reads=[r_const], writes=[r_const])
        fw.op("dve", lambda e: e.tensor_tensor(out=pl[:], in0=pl[:], in1=ex[:], op=ALU.mult),
              reads=[r_const], writes=[r_const])
        fw.op("dve", lambda e: e.tensor_scalar(out=pl[:], in0=pl[:], scalar1=1.0, scalar2=None,
                                               op0=ALU.add), reads=[r_const], writes=[r_const])
        fw.op("dve", lambda e: e.tensor_tensor(out=pl[:], in0=pl[:], in1=ex[:], op=ALU.mult),
              reads=[r_const], writes=[r_const])
        fw.op("dve", lambda e: e.tensor_scalar(out=cc[:], in0=pl[:], scalar1=-8.0, scalar2=None,
                                               op0=ALU.mult), reads=[r_const], writes=[r_const])
        fw.op("dve", lambda e: e.tensor_scalar(out=hc[:], in0=pl[:], scalar1=-4.0, scalar2=None,
                                               op0=ALU.mult), reads=[r_const], writes=[r_const])
        fw.op("dve", lambda e: e.tensor_scalar(out=hbr[:], in0=br_sb[:], scalar1=0.5, scalar2=None,
                                               op0=ALU.mult), reads=[r_small], writes=[r_const])
        fw.op("dve", lambda e: e.tensor_scalar(out=hbi[:], in0=bi_sb[:], scalar1=0.5, scalar2=None,
                                               op0=ALU.mult), reads=[r_small], writes=[r_const])

        lru_own = sb_at("lru_own", [128, 4, TOWN], F32, 8 * KB)
        r_lru = Res("lru_own")
        qt = sb_at("qt", [128, 8, TOWN], BF16, 40 * KB)
        hn_own = sb_at("hn_own", [128, 8, TOWN], BF16, 72 * KB)
        lruA = sb_at("lruA", [128, 4, TOWN], BF16, 104 * KB)
        attnT = sb_at("attnT", [128, 4, TOWN], BF16, 120 * KB)
        merged = sb_at("merged", [128, 8, TOWN], BF16, 136 * KB)
        x1 = sb_at("x1", [128, 16, D], F32, 8 * KB)
        hn2k = sb_at("hn2k", [128, 16, D], BF16, 72 * KB)
        Mf = sb_at("Mf", [128, 16, 32], F32, 203 * KB)
        Mb = sb_at("Mb", [128, 16, 32], BF16, 205 * KB)
        r_M = Res("M")
        TOP = 207 * KB
        set_free([[40 * KB, TOP]])
        es1 = ExitStack()
        cur["es"] = es1
        wk_sb = sb("wk_sb", [128, 8, 512], BF16)
        wv_sb = sb("wv_sb", [128, 8, 512], BF16)
        wx_sb = sb("wx_sb", [128, 8, 512], BF16)
        r_w1 = Res("w1")
        w_in_v = w_in.rearrange("(c p) n -> p c n", p=128)

        TT = 512
        NT = S // TT
        xin = [sb("xin%d" % i, [128, 8, TT]) for i in range(2)]
        r_xin = [Res("xin%d" % i) for i in range(2)]
        for i_, (wsb, c0) in enumerate(((wk_sb, 512), (wv_sb, 1024), (wx_sb, 1536))):
            xb_ = xin[i_ % 2]; rxb_ = r_xin[i_ % 2]
            for c in range(0, 8, 4):
                fw.dma("sp", xb_[:, c:c + 4, :], w_in_v[:, c:c + 4, c0:c0 + 512], writes=[rxb_])
            fw.op("act", lambda e, wsb=wsb, xb_=xb_: e.activation(out=wsb[:, 0:4, :], in_=xb_[:, 0:4, :], func=AF.Copy),
                  reads=[rxb_], writes=[r_w1])
            fw.op("dve", lambda e, wsb=wsb, xb_=xb_: e.tensor_copy(out=wsb[:, 4:8, :], in_=xb_[:, 4:8, :]),
                  reads=[rxb_], writes=[r_w1])
        xsq = sb("xsq", [128, 8, TT], BF16)
        r_xsq = Res("xsq")
        rb = sb("rb", [128, TT])
        r_rb = Res("rb")
        hn = [sb("hn%d" % i, [128, 8, TT], BF16) for i in range(2)]
        r_hn = [Res("hn%d" % i) for i in range(2)]
        kst = [sb("kst%d" % i, [64, 8, TT], BF16) for i in range(2)]
        r_kst = [Res("kst%d" % i) for i in range(2)]
        vst = [sb("vst%d" % i, [128, 4, 512], BF16) for i in range(2)]
        r_vst = [Res("vst%d" % i) for i in range(2)]
        xrp = [sb("xrp%d" % i, [128, 4, TT + 3]) for i in range(2)]
        r_xrp = [[Res("xrp%d_%d" % (i, g)) for g in range(4)] for i in range(2)]
        xcL = [sb("xc%d" % i, [128, TT]) for i in range(2)]; r_xcL = [Res("xc%d" % i) for i in range(2)]
        xcbL = [sb("xcb%d" % i, [128, TT], BF16) for i in range(2)]; r_xcbL = [Res("xcb%d" % i) for i in range(2)]
        thrL = [sb("thr%d" % i, [128, TT]) for i in range(2)]; r_thrL = [Res("thr%d" % i) for i in range(2)]
        thiL = [sb("thi%d" % i, [128, TT]) for i in range(2)]; r_thiL = [Res("thi%d" % i) for i in range(2)]
        a_tL = [sb("a_t%d" % i, [128, TT]) for i in range(2)]; r_aL = [Res("a%d" % i) for i in range(2)]
        a2_tL = [sb("a2_t%d" % i, [128, TT]) for i in range(2)]; r_a2L = [Res("a2%d" % i) for i in range(2)]
        u_tL = [sb("u_t%d" % i, [128, TT]) for i in range(2)]; r_uL = [Res("u%d" % i) for i in range(2)]
        h_t = [sb("h_t%d" % g, [128, TT]) for g in range(4)]
        r_h = [Res("h%d" % g) for g in range(4)]
        carry = sb("carry", [128, 4])
        r_carry = [Res("carry%d" % g) for g in range(4)]

        ps_ss = ps("ps_ss", [128, TT]); r_pss = Res("ps_ss")
        ps_k = [ps("ps_k%d" % i, [64, TT]) for i in range(2)]
        r_psk = [Res("ps_k%d" % i) for i in range(2)]
        ps_v = [ps("ps_v%d" % i, [128, 512]) for i in range(2)]
        r_psv = [Res("ps_v%d" % i) for i in range(2)]
        ps_x = ps("ps_x", [128, TT]); r_psx = Res("ps_x")
        ps_r = ps("ps_r", [128, TT]); r_psr = Res("ps_r")
        ps_i = ps("ps_i", [128, TT]); r_psi = Res("ps_i")

        for g in range(4):
            fw.op("pool", lambda e, g=g: e.memset(xrp[0][:, g, 0:3], 0.0), writes=[r_xrp[0][g]])

        xn_v = xn.rearrange("(c p) t -> p c t", p=128)
        cnt = {"k": 0, "v": 0}

        def xload(k):
            t0 = k * TT
            xb = xin[k % 2]; rxb = r_xin[k % 2]
            for c in range(0, 8, 4):
                fw.dma("sp", xb[:, c:c + 4, :], xn_v[:, c:c + 4, t0:t0 + TT], writes=[rxb])

        def front(k):
            xb = xin[k % 2]; rxb = r_xin[k % 2]
            hb = hn[k % 2]; rhb = r_hn[k % 2]
            fw.op("act", lambda e: e.activation(out=xsq[:], in_=xb[:], func=AF.Square),
                  reads=[rxb], writes=[r_xsq])
            for c in range(8):
                fw.op("pe", lambda e, c=c: e.matmul(ps_ss[:], lhsT=ones_bf[:], rhs=xsq[:, c, :],
                                                    start=(c == 0), stop=(c == 7)),
                      inc=(c == 7), reads=[r_ones, r_xsq], writes=[r_pss])
            fw.op("act", lambda e: e.activation(out=rb[:], in_=ps_ss[:], func=AF.Ln, bias=eps_sb[:, 0:1],
                                                scale=1.0 / D), reads=[r_pss, r_ones], writes=[r_rb])
            fw.op("act", lambda e: e.activation(out=rb[:], in_=rb[:], func=AF.Exp, scale=-0.5),
                  reads=[r_rb], writes=[r_rb])
            for c in range(8):
                fw.op("dve", lambda e, c=c: e.scalar_tensor_tensor(
                    out=hb[:, c, :], in0=xb[:, c, :], scalar=g_sb[:, c:c + 1], in1=rb[:],
                    op0=ALU.mult, op1=ALU.mult), reads=[rxb, r_g, r_rb], writes=[rhb])

        def kv_quarter(k, q):
            t0 = k * TT
            hb = hn[k % 2]; rhb = r_hn[k % 2]
            ks = kst[k % 2]; rks = r_kst[k % 2]
            vs = vst[k % 2]; rvs = r_vst[k % 2]
            for hm in (2 * q, 2 * q + 1):
                pk = ps_k[cnt["k"] % 2]; rpk = r_psk[cnt["k"] % 2]
                cnt["k"] += 1
                for c in range(8):
                    fw.op("pe", lambda e, c=c, hm=hm, pk=pk: e.matmul(
                        pk[:], lhsT=wk_sb[:, c, hm * 64:(hm + 1) * 64], rhs=hb[:, c, :],
                        start=(c == 0), stop=(c == 7)), inc=(c == 7), reads=[r_w1, rhb], writes=[rpk])
                fw.op("act", lambda e, hm=hm, pk=pk: e.activation(out=ks[:, hm, :], in_=pk[:], func=AF.Copy),
                      reads=[rpk], writes=[rks])
            tb = q
            pv = ps_v[cnt["v"] % 2]; rpv = r_psv[cnt["v"] % 2]
            cnt["v"] += 1
            for c in range(8):
                fw.op("pe", lambda e, c=c, tb=tb, pv=pv: e.matmul(
                    pv[:], lhsT=hb[:, c, tb * 128:(tb + 1) * 128], rhs=wv_sb[:, c, :],
                    start=(c == 0), stop=(c == 7)), inc=(c == 7), reads=[r_w1, rhb], writes=[rpv])
            fw.op("dve", lambda e, tb=tb, pv=pv: e.tensor_copy(out=vs[:, tb, :], in_=pv[:]),
                  reads=[rpv], writes=[rvs])
            if q == 3:
                fw.dma("pool", kt_scr[:, :, t0:t0 + TT].rearrange("h p t -> p h t"), ks[:, :, :], reads=[rks])
                fw.dma("pool", v_scr[t0:t0 + TT, :].rearrange("(b p) n -> p b n", p=128), vs[:, :, :], reads=[rvs])

        def lru_a(k, g):
            gi = g % 2
            xc = xcL[gi]; r_xc = r_xcL[gi]; xcb = xcbL[gi]; r_xcb = r_xcbL[gi]
            thr = thrL[gi]; r_thr = r_thrL[gi]; thi = thiL[gi]; r_thi = r_thiL[gi]
            a_t = a_tL[gi]; r_a = r_aL[gi]; a2_t = a2_tL[gi]; r_a2 = r_a2L[gi]; u_t = u_tL[gi]; r_u = r_uL[gi]
            hb = hn[k % 2]; rhb = r_hn[k % 2]
            xp = xrp[k % 2]; xp_n = xrp[(k + 1) % 2]
            rxp = r_xrp[k % 2][g]; rxpn = r_xrp[(k + 1) % 2][g]
            for c in range(8):
                fw.op("pe", lambda e, c=c, g=g: e.matmul(
                    ps_x[:], lhsT=wx_sb[:, c, g * 128:(g + 1) * 128], rhs=hb[:, c, :],
                    start=(c == 0), stop=(c == 7)), inc=(c == 7), reads=[r_w1, rhb], writes=[r_psx])
            fw.op("act", lambda e, g=g: e.activation(out=xp[:, g, 3:TT + 3], in_=ps_x[:], func=AF.Copy),
                  reads=[r_psx], writes=[rxp])
            fw.op("pool", lambda e, g=g: e.tensor_copy(out=xp_n[:, g, 0:3], in_=xp[:, g, TT:TT + 3]),
                  reads=[rxp], writes=[rxpn])
            fw.op("dve", lambda e, g=g: e.tensor_scalar(
                out=xc[:], in0=xp[:, g, 0:TT], scalar1=cw_sb[:, g, 0:1], scalar2=cb_sb[:, g:g + 1],
                op0=ALU.mult, op1=ALU.add), reads=[rxp, r_small], writes=[r_xc])
            for j in range(1, 4):
                fw.op("dve", lambda e, g=g, j=j: e.scalar_tensor_tensor(
                    out=xc[:], in0=xp[:, g, j:j + TT], scalar=cw_sb[:, g, j:j + 1], in1=xc[:],
                    op0=ALU.mult, op1=ALU.add), reads=[rxp, r_small, r_xc], writes=[r_xc])
            fw.op("pool", lambda e: e.tensor_copy(out=xcb[:], in_=xc[:]), reads=[r_xc], writes=[r_xcb])

        def lru_b(k, g):
            gi = g % 2
            xc = xcL[gi]; r_xc = r_xcL[gi]; xcb = xcbL[gi]; r_xcb = r_xcbL[gi]
            thr = thrL[gi]; r_thr = r_thrL[gi]; thi = thiL[gi]; r_thi = r_thiL[gi]
            a_t = a_tL[gi]; r_a = r_aL[gi]; a2_t = a2_tL[gi]; r_a2 = r_a2L[gi]; u_t = u_tL[gi]; r_u = r_uL[gi]
            fw.op("pe", lambda e, g=g: e.matmul(ps_r[:], lhsT=wr_sb[:, g, :], rhs=xcb[:], start=True, stop=True),
                  inc=True, reads=[r_wgate, r_xcb], writes=[r_psr])
            fw.op("pe", lambda e, g=g: e.matmul(ps_i[:], lhsT=wi_sb[:, g, :], rhs=xcb[:], start=True, stop=True),
                  inc=True, reads=[r_wgate, r_xcb], writes=[r_psi])
            fw.op("act", lambda e, g=g: e.activation(out=thr[:], in_=ps_r[:], func=AF.Tanh,
                                                     bias=hbr[:, g:g + 1], scale=0.5),
                  reads=[r_psr, r_const], writes=[r_thr])
            fw.op("act", lambda e, g=g: e.activation(out=thi[:], in_=ps_i[:], func=AF.Tanh,
                                                     bias=hbi[:, g:g + 1], scale=0.5),
                  reads=[r_psi, r_const], writes=[r_thi])
            fw.op("act", lambda e, g=g: e.activation(out=a_t[:], in_=thr[:], func=AF.Exp,
                                                     bias=hc[:, g:g + 1], scale=hc[:, g:g + 1]),
                  reads=[r_thr, r_const], writes=[r_a])
            fw.op("act", lambda e, g=g: e.activation(out=a2_t[:], in_=thr[:], func=AF.Exp,
                                                     bias=cc[:, g:g + 1], scale=cc[:, g:g + 1]),
                  reads=[r_thr, r_const], writes=[r_a2])
            fw.op("act", lambda e: e.activation(out=a2_t[:], in_=a2_t[:], func=AF.Ln, bias=one_sb[:, 0:1],
                                                scale=-1.0), reads=[r_a2, r_ones], writes=[r_a2])
            fw.op("act", lambda e: e.activation(out=a2_t[:], in_=a2_t[:], func=AF.Exp, scale=0.5),
                  reads=[r_a2], writes=[r_a2])
            if k == 0:
                fw.op("dve", lambda e: e.memset(a2_t[:, 0:1], 1.0), reads=[r_a2], writes=[r_a2])
            fw.op("dve", lambda e: e.scalar_tensor_tensor(out=u_t[:], in0=thi[:], scalar=1.0, in1=xc[:],
                                                          op0=ALU.add, op1=ALU.mult),
                  reads=[r_thi, r_xc], writes=[r_u])
            fw.op("dve", lambda e: e.scalar_tensor_tensor(out=u_t[:], in0=a2_t[:], scalar=0.5, in1=u_t[:],
                                                          op0=ALU.mult, op1=ALU.mult),
                  reads=[r_a2, r_u], writes=[r_u])
            if k == 0:
                fw.op("dve", lambda e, g=g: e.tensor_tensor_scan(out=h_t[g][:], data0=a_t[:], data1=u_t[:],
                                                                initial=0.0, op0=ALU.mult, op1=ALU.add),
                      reads=[r_a, r_u], writes=[r_h[g]])
            else:
                fw.op("dve", lambda e, g=g: e.tensor_copy(out=carry[:, g:g + 1], in_=h_t[g][:, TT - 1:TT]),
                      reads=[r_h[g]], writes=[r_carry[g]])
                fw.op("dve", lambda e, g=g: e.tensor_tensor_scan(out=h_t[g][:], data0=a_t[:], data1=u_t[:],
                                                                initial=carry[:, g:g + 1], op0=ALU.mult, op1=ALU.add),
                      reads=[r_a, r_u, r_carry[g]], writes=[r_h[g]])
            fw.op("dve", lambda e, g=g, k=k: e.tensor_scalar(
                out=lru_own[:, g, k * 128:(k + 1) * 128], in0=h_t[g][:, 0:128], scalar1=selj_sb[:, 0:1],
                scalar2=None, op0=ALU.mult), reads=[r_h[g], r_small], writes=[r_lru])
            for jj in range(1, 4):
                fw.op("dve", lambda e, g=g, k=k, jj=jj: e.scalar_tensor_tensor(
                    out=lru_own[:, g, k * 128:(k + 1) * 128], in0=h_t[g][:, jj * 128:(jj + 1) * 128],
                    scalar=selj_sb[:, jj:jj + 1], in1=lru_own[:, g, k * 128:(k + 1) * 128],
                    op0=ALU.mult, op1=ALU.add), reads=[r_h[g], r_small, r_lru], writes=[r_lru])

        xload(0)
        xload(1)
        front(0)
        for q in range(4):
            kv_quarter(0, q)
        for k in range(NT):
            lru_a(k, 0)
            if k + 1 < NT:
                front(k + 1)
            for g in range(4):
                if g + 1 < 4:
                    lru_a(k, g + 1)
                if g == 0 and k + 2 < NT:
                    xload(k + 2)
                if k + 1 < NT:
                    kv_quarter(k + 1, g)
                lru_b(k, g)
        fw.barrier()
        es1.close()

        set_free([[120 * KB, TOP]])
        es2 = ExitStack()
        cur["es"] = es2
        wq_sb = sb("wq_sb", [128, 8, 512], BF16)
        wy_sb = sb("wy_sb", [128, 8, 512], BF16)
        r_w2 = Res("w2")
        for wsb, c0 in ((wq_sb, 0), (wy_sb, 2048)):
            for c in range(8):
                fw.dma("pool", wsb[:, c, :], w_in_v[:, c, c0:c0 + 512], writes=[r_w2])
        r_qt = Res("qt")
        for h in range(NH):
            for m_ in range(2):
                fw.dma("sp", qt[64:68, 2 * h + m_, :], qaug[h, :, :], writes=[r_qt])
        xin2 = sb("xin2", [128, 8, TT]); r_xin2 = Res("xin2")
        xsq2 = sb("xsq2", [128, 8, TT], BF16); r_xsq2 = Res("xsq2")
        rb2 = sb("rb2", [128, TT]); r_rb2 = Res("rb2")
        ysb = sb("ysb", [128, TT]); r_ysb = Res("ysb")
        y2 = sb("y2", [128, TT]); r_y2 = Res("y2")
        thy = sb("thy", [128, TT]); r_thy = Res("thy")
        r_hno = Res("hn_own")
        r_lruA = Res("lruA")
        p2_ss = ps("p2_ss", [128, TT]); r_p2ss = Res("p2ss")
        p2_q = [ps("p2_q%d" % i, [64, TT]) for i in range(2)]
        r_p2q = [Res("p2q%d" % i) for i in range(2)]
        p2_y = [ps("p2_y%d" % i, [128, TT]) for i in range(2)]
        r_p2y = [Res("p2y%d" % i) for i in range(2)]
        xo_v = xo.rearrange("(c p) t -> p c t", p=128)
        for m in range(4):
            t0 = m * TT
            for c in range(0, 8, 4):
                fw.dma("sp", xin2[:, c:c + 4, :], xo_v[:, c:c + 4, t0:t0 + TT], writes=[r_xin2])
            fw.op("act", lambda e: e.activation(out=xsq2[:], in_=xin2[:], func=AF.Square),
                  reads=[r_xin2], writes=[r_xsq2])
            for c in range(8):
                fw.op("pe", lambda e, c=c: e.matmul(p2_ss[:], lhsT=ones_bf[:], rhs=xsq2[:, c, :],
                                                    start=(c == 0), stop=(c == 7)),
                      inc=(c == 7), reads=[r_ones, r_xsq2], writes=[r_p2ss])
            fw.op("act", lambda e: e.activation(out=rb2[:], in_=p2_ss[:], func=AF.Ln, bias=eps_sb[:, 0:1],
                                                scale=1.0 / D), reads=[r_p2ss, r_ones], writes=[r_rb2])
            fw.op("act", lambda e: e.activation(out=rb2[:], in_=rb2[:], func=AF.Exp, scale=-0.5),
                  reads=[r_rb2], writes=[r_rb2])
            for c in range(8):
                fw.op("dve", lambda e, c=c: e.scalar_tensor_tensor(
                    out=hn_own[:, c, t0:t0 + TT], in0=xin2[:, c, :], scalar=g_sb[:, c:c + 1], in1=rb2[:],
                    op0=ALU.mult, op1=ALU.mult), reads=[r_xin2, r_g, r_rb2], writes=[r_hno])
            for hm in range(8):
                pq = p2_q[hm % 2]; rpq = r_p2q[hm % 2]
                for c in range(8):
                    fw.op("pe", lambda e, c=c, hm=hm, pq=pq: e.matmul(
                        pq[:], lhsT=wq_sb[:, c, hm * 64:(hm + 1) * 64], rhs=hn_own[:, c, t0:t0 + TT],
                        start=(c == 0), stop=(c == 7)), inc=(c == 7), reads=[r_w2, r_hno], writes=[rpq])
                fw.op("act", lambda e, hm=hm, pq=pq: e.activation(out=qt[0:64, hm, t0:t0 + TT], in_=pq[:], func=AF.Copy),
                      reads=[rpq], writes=[r_qt])
            for g in range(4):
                py = p2_y[g % 2]; rpy = r_p2y[g % 2]
                for c in range(8):
                    fw.op("pe", lambda e, c=c, g=g, py=py: e.matmul(
                        py[:], lhsT=wy_sb[:, c, g * 128:(g + 1) * 128], rhs=hn_own[:, c, t0:t0 + TT],
                        start=(c == 0), stop=(c == 7)), inc=(c == 7), reads=[r_w2, r_hno], writes=[rpy])
                fw.op("act", lambda e, py=py: e.activation(out=ysb[:], in_=py[:], func=AF.Copy),
                      reads=[rpy], writes=[r_ysb])
                fw.op("act", lambda e, py=py: e.activation(out=y2[:], in_=py[:], func=AF.Square),
                      reads=[rpy], writes=[r_y2])
                fw.op("dve", lambda e: e.tensor_scalar(out=y2[:], in0=y2[:], scalar1=0.044715, scalar2=1.0,
                                                       op0=ALU.mult, op1=ALU.add), reads=[r_y2], writes=[r_y2])
                fw.op("dve", lambda e: e.tensor_tensor(out=y2[:], in0=y2[:], in1=ysb[:], op=ALU.mult),
                      reads=[r_y2, r_ysb], writes=[r_y2])
                fw.op("act", lambda e: e.activation(out=thy[:], in_=y2[:], func=AF.Tanh, scale=GELU_K),
                      reads=[r_y2], writes=[r_thy])
                fw.op("dve", lambda e: e.scalar_tensor_tensor(out=thy[:], in0=thy[:], scalar=1.0, in1=ysb[:],
                                                              op0=ALU.add, op1=ALU.mult),
                      reads=[r_thy, r_ysb], writes=[r_thy])
                fw.op("dve", lambda e, g=g: e.scalar_tensor_tensor(
                    out=lruA[:, g, t0:t0 + TT], in0=thy[:], scalar=0.5, in1=lru_own[:, g, t0:t0 + TT],
                    op0=ALU.mult, op1=ALU.mult), reads=[r_thy, r_lru], writes=[r_lruA])
        fw.barrier()
        es2.close()

        set_free([[136 * KB, TOP]])
        es3 = ExitStack()
        cur["es"] = es3
        kt = [sb_at("kt%d" % i, [128, S], BF16, 8 * KB + i * 16 * KB) for i in range(2)]
        r_kt = [Res("kt%d" % i) for i in range(2)]
        vh = sb("vh", [128, 64, 128], BF16); r_vh = Res("vh")
        pt = [sb("pt%d" % i, [128, 2, 512], BF16) for i in range(2)]
        r_pt = [Res("pt%d" % i) for i in range(2)]
        rl = sb("rl", [128, 2, 512]); r_rl = Res("rl")
        dd = sb("dd", [128, 512]); r_dd = Res("dd")
        tmp1 = sb("tmp1", [128, 512]); r_tmp1 = Res("tmp1")
        sq3 = sb("sq3", [128, 512], BF16); r_sq3 = Res("sq3")
        rs3 = sb("rs3", [128, 512]); r_rs3 = Res("rs3")
        cm_sb = sb("cm_sb", [128, 4, 128], BF16); r_cm = Res("cm")
        lp = sb("lp", [64, 4]); lpp = sb("lpp", [64, 2]); ones64 = sb("ones64", [64, 128])
        nlam = sb("nlam", [128, 1]); elam = sb("elam", [128, 2]); gs = sb("gs", [128, 1])
        r_lam = Res("lam")
        fw.dma("sp", cm_sb[:], cmask[:, :, :], writes=[r_cm])
        ident_sb = sb("ident_sb", [128, 128], BF16)
        negm = sb("negm", [128, 4, 128], BF16); r_negm = Res("negm")
        fw.dma("sp", ident_sb[:], ident[:, :], writes=[r_negm])
        fw.op("dve", lambda e: e.tensor_scalar(out=negm[:], in0=cm_sb[:], scalar1=-1.0, scalar2=30000.0,
                                               op0=ALU.add, op1=ALU.mult), reads=[r_cm, r_negm], writes=[r_negm])
        fw.dma("sp", lp[:], lam_qk[:, :], writes=[r_lam])
        fw.dma("sp", gs[:], subln[:, :], writes=[r_lam])
        for i in range(2):
            fw.dma("sp", kt[i][64:68, :], kaug[:, :], writes=[r_kt[i]])
        ps_s = [ps("ps_s%d" % i, [128, 2, 512]) for i in range(2)]
        r_pss3 = [Res("ps_s%d" % i) for i in range(2)]
        po = ps("po", [128, 2, 512]); r_po = Res("po")
        pl_ = ps("pl_", [128, 2, 512]); r_pl = Res("pl")
        fw.op("pool", lambda e: e.memset(ones64[:], 1.0), writes=[r_lam])
        fw.op("dve", lambda e: e.tensor_tensor(out=lpp[:, 0:1], in0=lp[:, 0:1], in1=lp[:, 1:2], op=ALU.mult),
              reads=[r_lam], writes=[r_lam])
        fw.op("dve", lambda e: e.tensor_tensor(out=lpp[:, 1:2], in0=lp[:, 2:3], in1=lp[:, 3:4], op=ALU.mult),
              reads=[r_lam], writes=[r_lam])
        fw.op("pe", lambda e: e.matmul(ps_s[0][:, 0, 0:2], lhsT=ones64[:], rhs=lpp[:], start=True, stop=True),
              inc=True, reads=[r_lam], writes=[r_pss3[0]])
        fw.op("act", lambda e: e.activation(out=elam[:], in_=ps_s[0][:, 0, 0:2], func=AF.Exp),
              reads=[r_pss3[0]], writes=[r_lam])
        fw.op("dve", lambda e: e.tensor_tensor(out=nlam[:], in0=elam[:, 1:2], in1=elam[:, 0:1], op=ALU.subtract),
              reads=[r_lam], writes=[r_lam])
        fw.op("dve", lambda e: e.tensor_scalar(out=nlam[:], in0=nlam[:], scalar1=-LAM_INIT, scalar2=None,
                                               op0=ALU.add), reads=[r_lam], writes=[r_lam])
        fw.op("dve", lambda e: e.tensor_scalar(out=gs[:], in0=gs[:], scalar1=1.0 - LAM_INIT, scalar2=None,
                                               op0=ALU.mult), reads=[r_lam], writes=[r_lam])
        r_attn = Res("attnT")

        def load_k(h):
            for m_ in range(2):
                fw.dma("sp", kt[m_][0:64, :], kt_scr[2 * h + m_, :, :], writes=[r_kt[m_]])

        def load_v(h):
            for q4 in range(4):
                fw.dma("sp", vh[:, q4 * 16:(q4 + 1) * 16, :],
                       v_scr[q4 * 2048:(q4 + 1) * 2048, h * 128:(h + 1) * 128].rearrange("(b p) v -> p b v", p=128),
                       writes=[r_vh])

        steps = []
        for h in range(NH):
            for m in range(4):
                nkb = 16 * m + 16
                for kb in range(nkb):
                    qmin = max(0, (kb - 16 * m) // 4) if kb >= 16 * m else 0
                    steps.append(dict(h=h, m=m, kb=kb, c0=128 * qmin, nkb=nkb, diag=(kb >= 16 * m),
                                      jj=(kb - 16 * m) % 4, i=len(steps)))

        def emit_qk(st):
            h, m, kb, c0 = st["h"], st["m"], st["kb"], st["c0"]
            pss = ps_s[st["i"] % 2]; rps = r_pss3[st["i"] % 2]
            for m_ in range(2):
                fw.op("pe", lambda e, m_=m_: e.matmul(
                    pss[:, m_, c0:512], lhsT=kt[m_][0:KA, kb * 128:(kb + 1) * 128],
                    rhs=qt[0:KA, 2 * h + m_, m * 512 + c0:(m + 1) * 512], start=True, stop=(not st["diag"])),
                    inc=(not st["diag"]), reads=[r_kt[m_], r_qt], writes=[rps])
                if st["diag"]:
                    fw.op("pe", lambda e, m_=m_: e.matmul(
                        pss[:, m_, c0:c0 + 128], lhsT=ident_sb[:], rhs=negm[:, st["jj"], :], start=False, stop=True),
                        inc=True, reads=[r_negm], writes=[rps])

        def emit_exp(st):
            c0 = st["c0"]
            pss = ps_s[st["i"] % 2]; rps = r_pss3[st["i"] % 2]
            ptb = pt[st["i"] % 2]; rptb = r_pt[st["i"] % 2]
            fw.op("act", lambda e: e.activation(out=ptb[:, :, c0:512], in_=pss[:, :, c0:512], func=AF.Exp, scale=0.125),
                  reads=[rps], writes=[rptb])

        def emit_pv(st):
            kb, c0, nkb = st["kb"], st["c0"], st["nkb"]
            ptb = pt[st["i"] % 2]; rptb = r_pt[st["i"] % 2]
            for m_ in range(2):
                fw.op("pe", lambda e, m_=m_: e.matmul(
                    po[:, m_, c0:512], lhsT=vh[:, kb, :], rhs=ptb[:, m_, c0:512],
                    start=(kb == 0), stop=(kb == nkb - 1)), inc=(kb == nkb - 1), reads=[r_vh, rptb], writes=[r_po])
                fw.op("pe", lambda e, m_=m_: e.matmul(
                    pl_[:, m_, c0:512], lhsT=ones_bf[:], rhs=ptb[:, m_, c0:512],
                    start=(kb == 0), stop=(kb == nkb - 1)), inc=(m_ == 1 or kb == nkb - 1), reads=[r_ones, rptb], writes=[r_pl])

        def finalize(h, m, sbuf_i):
            pfin = ps_s[sbuf_i]; rpfin = r_pss3[sbuf_i]
            fw.op("dve", lambda e: e.reciprocal(out=rl[:], in_=pl_[:]), reads=[r_pl], writes=[r_rl])
            fw.op("dve", lambda e: e.tensor_tensor(out=dd[:], in0=po[:, 0, :], in1=rl[:, 0, :], op=ALU.mult),
                  reads=[r_po, r_rl], writes=[r_dd])
            fw.op("dve", lambda e: e.tensor_tensor(out=tmp1[:], in0=po[:, 1, :], in1=rl[:, 1, :], op=ALU.mult),
                  reads=[r_po, r_rl], writes=[r_tmp1])
            fw.op("dve", lambda e: e.scalar_tensor_tensor(out=dd[:], in0=tmp1[:], scalar=nlam[:, 0:1], in1=dd[:],
                                                          op0=ALU.mult, op1=ALU.add),
                  reads=[r_tmp1, r_lam, r_dd], writes=[r_dd])
            fw.op("act", lambda e: e.activation(out=sq3[:], in_=dd[:], func=AF.Square),
                  reads=[r_dd], writes=[r_sq3])
            fw.op("pe", lambda e: e.matmul(pfin[:, 0, :], lhsT=ones_bf[:], rhs=sq3[:], start=True, stop=True),
                  inc=True, reads=[r_ones, r_sq3], writes=[rpfin])
            fw.op("act", lambda e: e.activation(out=rs3[:], in_=pfin[:, 0, :], func=AF.Ln, bias=eps_sb[:, 0:1],
                                                scale=1.0 / 128.0), reads=[rpfin, r_ones], writes=[r_rs3])
            fw.op("act", lambda e: e.activation(out=rs3[:], in_=rs3[:], func=AF.Exp, scale=-0.5),
                  reads=[r_rs3], writes=[r_rs3])
            fw.op("dve", lambda e: e.scalar_tensor_tensor(
                out=attnT[:, h, m * 512:(m + 1) * 512], in0=dd[:], scalar=gs[:, 0:1], in1=rs3[:],
                op0=ALU.mult, op1=ALU.mult), reads=[r_dd, r_lam, r_rs3], writes=[r_attn])

        load_k(0)
        load_v(0)
        emit_qk(steps[0])
        for i, st in enumerate(steps):
            nxt = steps[i + 1] if i + 1 < len(steps) else None
            newh = nxt is not None and nxt["h"] != st["h"]
            if newh:
                load_k(nxt["h"])
            if nxt is not None:
                emit_qk(nxt)
            emit_exp(st)
            emit_pv(st)
            if newh:
                load_v(nxt["h"])
            if st["kb"] == st["nkb"] - 1:
                finalize(st["h"], st["m"], st["i"] % 2)
        fw.barrier()
        es3.close()

        set_free([[8 * KB, 72 * KB], [168 * KB, TOP]])
        es4 = ExitStack()
        cur["es"] = es4
        woa = sb("woa", [128, 4, D], BF16)
        wol = sb("wol", [128, 4, D], BF16)
        r_w4 = Res("w4")
        woa_v = w_o_attn.rearrange("(c p) n -> p c n", p=128)
        wol_v = w_o_lru.rearrange("(c p) n -> p c n", p=128)
        for c in range(4):
            fw.dma("pool", woa[:, c, :], woa_v[:, c, :], writes=[r_w4])
            fw.dma("pool", wol[:, c, :], wol_v[:, c, :], writes=[r_w4])
        wgA = sb("wgA", [128, 8, D], BF16)
        wgL = sb("wgL", [128, 8, D], BF16)
        r_wgA = Res("wgA")
        gstg = [sb("gstg%d" % i, [128, D]) for i in range(3)]
        r_gstg = [Res("gstg%d" % i) for i in range(3)]
        gi = 0
        for c in range(8):
            for dst, c0 in ((wgA, 2560), (wgL, 3584)):
                sg = gstg[gi % 3]; rsg = r_gstg[gi % 3]
                fw.dma("sp", sg[:], w_in_v[:, c, c0:c0 + D], writes=[rsg])
                if gi % 2 == 0:
                    fw.op("act", lambda e, sg=sg, dst=dst, c=c: e.activation(out=dst[:, c, :], in_=sg[:], func=AF.Copy),
                          reads=[rsg], writes=[r_wgA])
                else:
                    fw.op("dve", lambda e, sg=sg, dst=dst, c=c: e.tensor_copy(out=dst[:, c, :], in_=sg[:]),
                          reads=[rsg], writes=[r_wgA])
                gi += 1
        thA = sb("thA", [128, TT]); r_thA = Res("thA")
        thL = sb("thL", [128, TT]); r_thL = Res("thL")
        m1 = sb("m1", [128, TT]); r_m1 = Res("m1")
        m2 = sb("m2", [128, TT]); r_m2 = Res("m2")
        r_mg = Res("merged")
        pA = ps("pA", [128, TT]); r_pA = Res("pA")
        pL = ps("pL", [128, TT]); r_pL = Res("pL")
        pGA = ps("pGA", [128, TT]); r_pGA = Res("pGA")
        pGL = ps("pGL", [128, TT]); r_pGL = Res("pGL")
        for f in range(8):
            for m in range(4):
                t0 = m * TT
                for c in range(4):
                    fw.op("pe", lambda e, c=c, f=f, t0=t0: e.matmul(
                        pA[:], lhsT=woa[:, c, f * 128:(f + 1) * 128], rhs=attnT[:, c, t0:t0 + TT],
                        start=(c == 0), stop=(c == 3)), inc=(c == 3), reads=[r_w4, r_attn], writes=[r_pA])
                for c in range(4):
                    fw.op("pe", lambda e, c=c, f=f, t0=t0: e.matmul(
                        pL[:], lhsT=wol[:, c, f * 128:(f + 1) * 128], rhs=lruA[:, c, t0:t0 + TT],
                        start=(c == 0), stop=(c == 3)), inc=(c == 3), reads=[r_w4, r_lruA], writes=[r_pL])
                for c in range(8):
                    fw.op("pe", lambda e, c=c, f=f, t0=t0: e.matmul(
                        pGA[:], lhsT=wgA[:, c, f * 128:(f + 1) * 128], rhs=hn_own[:, c, t0:t0 + TT],
                        start=(c == 0), stop=(c == 7)), inc=(c == 7), reads=[r_wgA, r_hno], writes=[r_pGA])
                for c in range(8):
                    fw.op("pe", lambda e, c=c, f=f, t0=t0: e.matmul(
                        pGL[:], lhsT=wgL[:, c, f * 128:(f + 1) * 128], rhs=hn_own[:, c, t0:t0 + TT],
                        start=(c == 0), stop=(c == 7)), inc=(c == 7), reads=[r_wgA, r_hno], writes=[r_pGL])
                fw.op("act", lambda e: e.activation(out=thA[:], in_=pGA[:], func=AF.Tanh, scale=0.5),
                      reads=[r_pGA], writes=[r_thA])
                fw.op("act", lambda e: e.activation(out=thL[:], in_=pGL[:], func=AF.Tanh, scale=0.5),
                      reads=[r_pGL], writes=[r_thL])
                fw.op("dve", lambda e: e.scalar_tensor_tensor(out=m1[:], in0=thA[:], scalar=1.0, in1=pA[:],
                                                              op0=ALU.add, op1=ALU.mult),
                      reads=[r_thA, r_pA], writes=[r_m1])
                fw.op("dve", lambda e: e.scalar_tensor_tensor(out=m2[:], in0=thL[:], scalar=1.0, in1=pL[:],
                                                              op0=ALU.add, op1=ALU.mult),
                      reads=[r_thL, r_pL], writes=[r_m2])
                fw.op("dve", lambda e, f=f, t0=t0: e.tensor_tensor(out=merged[:, f, t0:t0 + TT], in0=m1[:], in1=m2[:],
                                                                  op=ALU.add), reads=[r_m1, r_m2], writes=[r_mg])
        fw.barrier()
        es4.close()

        set_free([[104 * KB, 136 * KB], [168 * KB, 203 * KB]])
        es5 = ExitStack()
        cur["es"] = es5
        wout = sb("wout", [128, 8, D], BF16); r_wo = Res("wout")
        wout_v = w_out.rearrange("(c p) n -> p c n", p=128)
        for c in range(8):
            fw.dma("pool", wout[:, c, :], wout_v[:, c, :], writes=[r_wo])
        x1T = sb("x1T", [128, 8, TT]); r_x1T = Res("x1T")
        xtok = [sb("xtok%d" % i, [128, D]) for i in range(2)]
        r_xtok = [Res("xtok%d" % i) for i in range(2)]
        xoc = [sb("xoc%d" % i, [128, TT]) for i in range(2)]
        r_xoc = [Res("xoc%d" % i) for i in range(2)]
        g2rep = sb("g2rep", [128, D]); r_g2rep = Res("g2rep")
        fw.dma("sp", g2rep[:], g_ffn_rep[:, :], writes=[r_g2rep])
        wr_f = sb("wr_f", [128, 8, 36]); r_wr = Res("wr")
        g2_sb = sb("g2_sb", [128, 8])
        lgL = [sb("lg%d" % i, [128, 36]) for i in range(4)]; r_lgL = [Res("lg%d" % i) for i in range(4)]
        rtL = [sb("rt%d" % i, [128, 64]) for i in range(4)]; r_rtL = [Res("rt%d" % i) for i in range(4)]
        junk = sb("junk", [128, D], BF16); r_junk = Res("junk")
        fw.dma("sp", g2_sb[:], g_ffn[:, :], writes=[r_wr])
        fw.dma("sp", wr_f[:], w_router.rearrange("(c p) n -> p c n", p=128), writes=[r_wr])
        for c in range(8):
            fw.op("dve", lambda e, c=c: e.tensor_scalar(out=wr_f[:, c, :], in0=wr_f[:, c, :], scalar1=g2_sb[:, c:c + 1],
                                                        scalar2=None, op0=ALU.mult), reads=[r_wr], writes=[r_wr])
        r_x1 = Res("x1")
        r_hn2 = Res("hn2k")
        p_o = [ps("p_o%d" % i, [128, 512]) for i in range(2)]
        r_p_o = [Res("p_o%d" % i) for i in range(2)]
        p_t = [ps("p_t%d" % i, [128, 512]) for i in range(2)]
        r_p_t = [Res("p_t%d" % i) for i in range(2)]
        p_lg = ps("p_lg", [128, 4, 64]); r_p_lg = Res("p_lg")
        xot_v = xot.rearrange("(b p) d -> p b d", p=128)
        ocnt = 0
        for m in range(4):
            t0 = m * TT
            for tb in range(4):
                blk = 4 * m + tb
                xt_ = xtok[blk % 2]; rxt = r_xtok[blk % 2]
                fw.dma("sp", xt_[:], xot_v[:, blk, :], writes=[rxt])
                for half in range(2):
                    po_ = p_o[ocnt % 2]; rpo_ = r_p_o[ocnt % 2]
                    ocnt += 1
                    for f in range(8):
                        fw.op("pe", lambda e, f=f, tb=tb, half=half, po_=po_, t0=t0: e.matmul(
                            po_[:], lhsT=merged[:, f, t0 + tb * 128:t0 + (tb + 1) * 128],
                            rhs=wout[:, f, half * 512:(half + 1) * 512], start=(f == 0), stop=(f == 7)),
                            inc=(f == 7), reads=[r_mg, r_wo], writes=[rpo_])
                    fw.op("dve", lambda e, blk=blk, half=half, po_=po_, xt_=xt_: e.scalar_tensor_tensor(
                        out=x1[:, blk, half * 512:(half + 1) * 512], in0=po_[:], scalar=0.5,
                        in1=xt_[:, half * 512:(half + 1) * 512], op0=ALU.mult, op1=ALU.add),
                        reads=[rpo_, rxt], writes=[r_x1])
            for f2 in range(8):
                pt_ = p_t[f2 % 2]; rpt_ = r_p_t[f2 % 2]
                xc_ = xoc[f2 % 2]; rxc_ = r_xoc[f2 % 2]
                fw.dma("sp", xc_[:], xo_v[:, f2, t0:t0 + TT], writes=[rxc_])
                for f in range(8):
                    fw.op("pe", lambda e, f=f, f2=f2, pt_=pt_, t0=t0: e.matmul(
                        pt_[:], lhsT=wout[:, f, f2 * 128:(f2 + 1) * 128], rhs=merged[:, f, t0:t0 + TT],
                        start=(f == 0), stop=(f == 7)), inc=(f == 7), reads=[r_mg, r_wo], writes=[rpt_])
                fw.op("dve", lambda e, f2=f2, pt_=pt_, xc_=xc_: e.scalar_tensor_tensor(
                    out=x1T[:, f2, :], in0=pt_[:], scalar=0.5, in1=xc_[:], op0=ALU.mult, op1=ALU.add),
                    reads=[rpt_, rxc_], writes=[r_x1T])
            for tb in range(4):
                for c in range(8):
                    fw.op("pe", lambda e, c=c, tb=tb: e.matmul(
                        p_lg[:, tb, 0:36], lhsT=x1T[:, c, tb * 128:(tb + 1) * 128], rhs=wr_f[:, c, :],
                        start=(c == 0), stop=(c == 7)), inc=(c == 7), reads=[r_x1T, r_wr], writes=[r_p_lg])
            def route_block(tb, blk, rt, lg, r_rt, r_lg):
                    fw.op("act", lambda e, blk=blk: e.activation(out=junk[:], in_=x1[:, blk, :], func=AF.Square,
                                                                 accum_out=rt[:, 0:1]),
                          reads=[r_x1], writes=[r_junk, r_rt])
                    fw.op("act", lambda e: e.activation(out=rt[:, 1:2], in_=rt[:, 0:1], func=AF.Ln, bias=eps_sb[:, 0:1],
                                                        scale=1.0 / D), reads=[r_rt, r_ones], writes=[r_rt])
                    fw.op("act", lambda e: e.activation(out=rt[:, 1:2], in_=rt[:, 1:2], func=AF.Exp, scale=-0.5),
                          reads=[r_rt], writes=[r_rt])
                    fw.op("dve", lambda e, tb=tb: e.tensor_scalar(out=lg[:], in0=p_lg[:, tb, 0:36], scalar1=rt[:, 1:2],
                                                                  scalar2=None, op0=ALU.mult),
                          reads=[r_p_lg, r_rt], writes=[r_lg])
                    fw.op("dve", lambda e, blk=blk: e.scalar_tensor_tensor(
                        out=hn2k[:, blk, :], in0=x1[:, blk, :], scalar=rt[:, 1:2], in1=g2rep[:],
                        op0=ALU.mult, op1=ALU.mult), reads=[r_x1, r_rt, r_g2rep], writes=[r_hn2])
                    fw.op("dve", lambda e: e.reduce_max(out=rt[:, 2:3], in_=lg[:, 0:4], axis=mybir.AxisListType.X),
                          reads=[r_lg, r_rt], writes=[r_rt])
                    fw.op("dve", lambda e: e.tensor_scalar(out=rt[:, 3:4], in0=rt[:, 2:3], scalar1=-1.0, scalar2=None,
                                                           op0=ALU.mult), reads=[r_rt], writes=[r_rt])
                    fw.op("act", lambda e: e.activation(out=rt[:, 4:8], in_=lg[:, 0:4], func=AF.Exp, bias=rt[:, 3:4],
                                                        accum_out=rt[:, 8:9]), reads=[r_lg, r_rt], writes=[r_rt])
                    fw.op("dve", lambda e: e.reciprocal(out=rt[:, 9:10], in_=rt[:, 8:9]), reads=[r_rt], writes=[r_rt])
                    fw.op("dve", lambda e: e.tensor_scalar(out=rt[:, 10:14], in0=lg[:, 0:4], scalar1=rt[:, 2:3], scalar2=None,
                                                           op0=ALU.is_ge), reads=[r_lg, r_rt], writes=[r_rt])
                    fw.op("dve", lambda e: e.tensor_scalar(out=rt[:, 16:24], in0=lg[:, 4:12], scalar1=rt[:, 10:11], scalar2=None,
                                                           op0=ALU.mult), reads=[r_lg, r_rt], writes=[r_rt])
                    for g in range(1, 4):
                        fw.op("dve", lambda e, g=g: e.scalar_tensor_tensor(
                            out=rt[:, 16:24], in0=lg[:, 4 + 8 * g:12 + 8 * g], scalar=rt[:, 10 + g:11 + g], in1=rt[:, 16:24],
                            op0=ALU.mult, op1=ALU.add), reads=[r_lg, r_rt], writes=[r_rt])
                    fw.op("dve", lambda e: e.reduce_max(out=rt[:, 24:25], in_=rt[:, 16:24], axis=mybir.AxisListType.X),
                          reads=[r_rt], writes=[r_rt])
                    fw.op("dve", lambda e: e.tensor_scalar(out=rt[:, 32:40], in0=rt[:, 16:24], scalar1=rt[:, 24:25], scalar2=None,
                                                           op0=ALU.is_ge), reads=[r_rt], writes=[r_rt])
                    fw.op("dve", lambda e: e.scalar_tensor_tensor(out=rt[:, 40:48], in0=rt[:, 32:40], scalar=-1e30, in1=rt[:, 16:24],
                                                                  op0=ALU.mult, op1=ALU.add), reads=[r_rt], writes=[r_rt])
                    fw.op("dve", lambda e: e.reduce_max(out=rt[:, 25:26], in_=rt[:, 40:48], axis=mybir.AxisListType.X),
                          reads=[r_rt], writes=[r_rt])
                    fw.op("dve", lambda e: e.tensor_scalar(out=rt[:, 48:56], in0=rt[:, 40:48], scalar1=rt[:, 25:26], scalar2=None,
                                                           op0=ALU.is_ge), reads=[r_rt], writes=[r_rt])
                    fw.op("dve", lambda e: e.tensor_tensor(out=rt[:, 26:27], in0=rt[:, 25:26], in1=rt[:, 24:25], op=ALU.subtract),
                          reads=[r_rt], writes=[r_rt])
                    fw.op("act", lambda e: e.activation(out=rt[:, 27:28], in_=rt[:, 26:27], func=AF.Exp),
                          reads=[r_rt], writes=[r_rt])
                    fw.op("dve", lambda e: e.tensor_scalar(out=rt[:, 28:29], in0=rt[:, 27:28], scalar1=1.0, scalar2=None,
                                                           op0=ALU.add), reads=[r_rt], writes=[r_rt])
                    fw.op("dve", lambda e: e.reciprocal(out=rt[:, 28:29], in_=rt[:, 28:29]), reads=[r_rt], writes=[r_rt])
                    fw.op("dve", lambda e: e.tensor_tensor(out=rt[:, 29:30], in0=rt[:, 27:28], in1=rt[:, 28:29], op=ALU.mult),
                          reads=[r_rt], writes=[r_rt])
                    fw.op("dve", lambda e: e.tensor_tensor(out=rt[:, 28:29], in0=rt[:, 28:29], in1=rt[:, 9:10], op=ALU.mult),
                          reads=[r_rt], writes=[r_rt])
                    fw.op("dve", lambda e: e.tensor_tensor(out=rt[:, 29:30], in0=rt[:, 29:30], in1=rt[:, 9:10], op=ALU.mult),
                          reads=[r_rt], writes=[r_rt])
                    fw.op("dve", lambda e: e.tensor_scalar(out=rt[:, 56:64], in0=rt[:, 32:40], scalar1=rt[:, 28:29], scalar2=None,
                                                           op0=ALU.mult), reads=[r_rt], writes=[r_rt])
                    fw.op("dve", lambda e: e.scalar_tensor_tensor(out=rt[:, 56:64], in0=rt[:, 48:56], scalar=rt[:, 29:30],
                                                                  in1=rt[:, 56:64], op0=ALU.mult, op1=ALU.add),
                          reads=[r_rt], writes=[r_rt])
                    for g in range(4):
                        fw.op("dve", lambda e, g=g, blk=blk: e.tensor_scalar(
                            out=Cw[:, blk, 8 * g:8 * g + 8], in0=rt[:, 56:64], scalar1=rt[:, 10 + g:11 + g], scalar2=None,
                            op0=ALU.mult), reads=[r_rt], writes=[r_C])
                    fw.op("dve", lambda e: e.tensor_tensor(out=rt[:, 40:48], in0=rt[:, 32:40], in1=rt[:, 48:56], op=ALU.add),
                          reads=[r_rt], writes=[r_rt])
                    for g in range(4):
                        fw.op("dve", lambda e, g=g, blk=blk: e.tensor_scalar(
                            out=Mf[:, blk, 8 * g:8 * g + 8], in0=rt[:, 40:48], scalar1=rt[:, 10 + g:11 + g], scalar2=None,
                            op0=ALU.mult), reads=[r_rt], writes=[r_M])
                    fw.op("dve", lambda e, blk=blk: e.tensor_copy(out=Mb[:, blk, :], in_=Mf[:, blk, :]),
                          reads=[r_M], writes=[r_M])
            chains = []
            for tb in range(4):
                fw.defer = []
                route_block(tb, 4 * m + tb, rtL[tb], lgL[tb], r_rtL[tb], r_lgL[tb])
                chains.append(fw.defer)
                fw.defer = None
            for i_ in range(max(len(c_) for c_ in chains)):
                for c_ in chains:
                    if i_ < len(c_):
                        ek_, fn_, rd_, wr_, inc_ = c_[i_]
                        fw.op(ek_, fn_, reads=rd_, writes=wr_, inc=inc_)
        fw.barrier()
        es5.close()

        CAP = 256
        set_free([[104 * KB, 203 * KB]])
        es6 = ExitStack()
        cur["es"] = es6
        wgb = sb("wgb", [128, 8, 512], BF16); wub = sb("wub", [128, 8, 512], BF16); wdb = sb("wdb", [128, 4, D], BF16)
        r_wgb = Res("wgb"); r_wub = Res("wub"); r_wdb = Res("wdb")
        NSTG = 4
        stg = [sb("stg%d" % i, [128, 2, 512]) for i in range(NSTG)]
        r_stg = [Res("stg%d" % i) for i in range(NSTG)]
        iota_s = sb("iota_s", [128, CAP]); iota_t = sb("iota_t", [128, TOWN])
        ustr = sb("ustr", [128, 128], BF16); tv = sb("tv", [128, 16, 8], BF16)
        r_k5 = Res("k5")
        fw.dma("sp", iota_s[:], iota_s_d[:, :], writes=[r_k5])
        fw.dma("sp", iota_t[:], iota_t_d[:, :], writes=[r_k5])
        fw.dma("sp", ustr[:], ustrict_d[:, :], writes=[r_k5])
        fw.dma("sp", tv[:], tvals_d[:, :, :], writes=[r_k5])
        pfx = sb("pfx", [128, 16, 32]); r_pfx = Res("pfx")
        Sel = sb("Sel", [128, 16, CAP], BF16); r_Sel = Res("Sel")
        XgT = sb("XgT", [128, 8, CAP], BF16); r_XgT = Res("XgT")
        HT = sb("HT", [128, 4, CAP], BF16); r_HT = Res("HT")
        SelT = [sb("SelT%d" % i, [128, 2, TOWN], BF16) for i in range(2)]
        r_SelT = [Res("SelT%d" % i) for i in range(2)]
        Yb = [sb("Yb%d" % i, [128, 2, D], BF16) for i in range(2)]
        r_Yb = [Res("Yb%d" % i) for i in range(2)]
        tokf = sb("tokf", [128, 2]); r_tokf = Res("tokf")
        tokc = sb("tokc", [128, 2, 2]); r_tokc = Res("tokc")
        s1 = [sb("s1_%d" % i, [128, CAP]) for i in range(2)]
        r_s1 = [Res("s1_%d" % i) for i in range(2)]
        th5 = [sb("th5_%d" % i, [128, CAP]) for i in range(2)]
        r_th5 = [Res("th5_%d" % i) for i in range(2)]

        pA_ = [ps("pA5_%d" % i, [128, 512]) for i in range(2)]
        r_pA_ = [Res("pA5_%d" % i) for i in range(2)]
        pGU = [ps("pGU%d" % i, [128, 512]) for i in range(2)]
        r_pGU = [Res("pGU%d" % i) for i in range(2)]
        pTok = ps("pTok", [128, 16, 32]); r_pTok = Res("pTok")
        pC = [ps("pC%d" % i, [128, 512]) for i in range(3)]
        r_pC = [Res("pC%d" % i) for i in range(3)]

        for blk in range(16):
            for b2 in range(blk + 1):
                fw.op("pe", lambda e, blk=blk, b2=b2: e.matmul(
                    pTok[:, blk, :], lhsT=(ustr[:] if b2 == blk else ones_bf[:]), rhs=Mb[:, b2, :],
                    start=(b2 == 0), stop=(b2 == blk)), inc=(b2 == blk), reads=[r_M, r_k5, r_ones], writes=[r_pTok])
        fw.op("dve", lambda e: e.tensor_copy(out=pfx[:], in_=pTok[:]), reads=[r_pTok], writes=[r_pfx])

        cnt5 = {"stg": 0, "gu": 0, "c": 0}

        wq5 = {"todo": [], "issued": []}

        def w_begin(ex_):
            wg_v = w_gate[ex_].rearrange("(c p) n -> p c n", p=128)
            wu_v = w_up[ex_].rearrange("(c p) n -> p c n", p=128)
            wd_v = w_down[ex_].rearrange("(c p) n -> p c n", p=128)
            pcs = []
            for c in range(0, 8, 2):
                pcs.append((wg_v[:, c:c + 2, :], wgb[:, c:c + 2, :], r_wgb))
            for c in range(0, 8, 2):
                pcs.append((wu_v[:, c:c + 2, :], wub[:, c:c + 2, :], r_wub))
            for half in range(2):
                for f in range(0, 4, 2):
                    pcs.append((wd_v[:, f:f + 2, half * 512:(half + 1) * 512],
                                wdb[:, f:f + 2, half * 512:(half + 1) * 512], r_wdb))
            assert not wq5["todo"] and not wq5["issued"]
            wq5["todo"] = pcs

        def w_issue():
            if not wq5["todo"]:
                return
            src, dst, rdst = wq5["todo"].pop(0)
            sg = stg[cnt5["stg"] % NSTG]; rsg = r_stg[cnt5["stg"] % NSTG]
            cnt5["stg"] += 1
            fw.dma("sp", sg[:], src, writes=[rsg])
            wq5["issued"].append((sg, rsg, dst, rdst))

        def w_cast():
            if not wq5["issued"]:
                return
            sg, rsg, dst, rdst = wq5["issued"].pop(0)
            fw.op("act", lambda e: e.activation(out=dst, in_=sg[:], func=AF.Copy), reads=[rsg], writes=[rdst])

        def stage_a(ex_):
            sT = SelT[ex_ % 2]; rsT = r_SelT[ex_ % 2]
            yb = Yb[ex_ % 2]; ryb = r_Yb[ex_ % 2]
            for blk in range(16):
                fw.op("dve", lambda e, blk=blk: e.tensor_scalar(
                    out=Sel[:, blk, :], in0=iota_s[:], scalar1=pfx[:, blk, ex_:ex_ + 1], scalar2=Mf[:, blk, ex_:ex_ + 1],
                    op0=ALU.is_equal, op1=ALU.mult), reads=[r_k5, r_pfx, r_M], writes=[r_Sel])
            yield
            for s_ in range(2):
                for blk in range(16):
                    fw.op("pe", lambda e, s_=s_, blk=blk: e.matmul(
                        pTok[:, s_, 0:8], lhsT=Sel[:, blk, s_ * 128:(s_ + 1) * 128], rhs=tv[:, blk, :],
                        start=(blk == 0), stop=(blk == 15)), inc=(blk == 15), reads=[r_Sel, r_k5], writes=[r_pTok])
            fw.op("act", lambda e: e.activation(out=tokc[:], in_=pTok[:, 0:2, 0:2], func=AF.Copy),
                  reads=[r_pTok], writes=[r_tokc])
            for s_ in range(2):
                fw.op("dve", lambda e, s_=s_: e.scalar_tensor_tensor(
                    out=tokf[:, s_:s_ + 1], in0=tokc[:, s_, 0:1], scalar=64.0, in1=tokc[:, s_, 1:2],
                    op0=ALU.mult, op1=ALU.add), reads=[r_tokc], writes=[r_tokf])
            for s_ in range(2):
                fw.op("dve", lambda e, s_=s_: e.tensor_scalar(
                    out=sT[:, s_, :], in0=iota_t[:], scalar1=tokf[:, s_:s_ + 1], scalar2=None, op0=ALU.is_equal),
                    reads=[r_k5, r_tokf], writes=[rsT])
            yield
            for half in range(2):
                for c2 in range(4):
                    c = 4 * half + c2
                    pa = pA_[c2 // 2]; rpa = r_pA_[c2 // 2]
                    for blk in range(16):
                        fw.op("pe", lambda e, c=c, c2=c2, blk=blk, pa=pa: e.matmul(
                            pa[:, (c2 % 2) * CAP:(c2 % 2 + 1) * CAP], lhsT=hn2k[:, blk, c * 128:(c + 1) * 128], rhs=Sel[:, blk, :],
                            start=(blk == 0), stop=(blk == 15)), inc=(blk == 15), reads=[r_hn2, r_Sel], writes=[rpa])
                    fw.op("act", lambda e, c=c, c2=c2, pa=pa: e.activation(
                        out=XgT[:, c, :], in_=pa[:, (c2 % 2) * CAP:(c2 % 2 + 1) * CAP], func=AF.Copy),
                        reads=[rpa], writes=[r_XgT])
                    yield
            for f in range(4):
                b_ = cnt5["gu"] % 2
                cnt5["gu"] += 1
                for c in range(8):
                    fw.op("pe", lambda e, c=c, f=f, b_=b_: e.matmul(
                        pGU[b_][:, 0:CAP], lhsT=wgb[:, c, f * 128:(f + 1) * 128], rhs=XgT[:, c, :],
                        start=(c == 0), stop=(c == 7)), inc=(c == 7), reads=[r_wgb, r_XgT], writes=[r_pGU[b_]])
                for c in range(8):
                    fw.op("pe", lambda e, c=c, f=f, b_=b_: e.matmul(
                        pGU[b_][:, CAP:2 * CAP], lhsT=wub[:, c, f * 128:(f + 1) * 128], rhs=XgT[:, c, :],
                        start=(c == 0), stop=(c == 7)), inc=(c == 7), reads=[r_wub, r_XgT], writes=[r_pGU[b_]])
                fw.op("act", lambda e, b_=b_: e.activation(out=th5[b_][:], in_=pGU[b_][:, 0:CAP], func=AF.Tanh, scale=0.5),
                      reads=[r_pGU[b_]], writes=[r_th5[b_]])
                fw.op("dve", lambda e, b_=b_: e.scalar_tensor_tensor(out=s1[b_][:], in0=th5[b_][:], scalar=1.0,
                                                                    in1=pGU[b_][:, 0:CAP], op0=ALU.add, op1=ALU.mult),
                      reads=[r_th5[b_], r_pGU[b_]], writes=[r_s1[b_]])
                fw.op("dve", lambda e, b_=b_, f=f: e.scalar_tensor_tensor(
                    out=HT[:, f, :], in0=s1[b_][:], scalar=0.5, in1=pGU[b_][:, CAP:2 * CAP], op0=ALU.mult, op1=ALU.mult),
                    reads=[r_s1[b_], r_pGU[b_]], writes=[r_HT])
                yield
            for s_ in range(2):
                for half in range(2):
                    pa = pA_[half]; rpa = r_pA_[half]
                    for f in range(4):
                        fw.op("pe", lambda e, f=f, s_=s_, half=half, pa=pa: e.matmul(
                            pa[:], lhsT=HT[:, f, s_ * 128:(s_ + 1) * 128], rhs=wdb[:, f, half * 512:(half + 1) * 512],
                            start=(f == 0), stop=(f == 3)), inc=(f == 3), reads=[r_HT, r_wdb], writes=[rpa])
                    fw.op("act", lambda e, s_=s_, half=half, pa=pa: e.activation(
                        out=yb[:, s_, half * 512:(half + 1) * 512], in_=pa[:], func=AF.Copy),
                        reads=[rpa], writes=[ryb])
                    yield

        def stage_b_units(ex_):
            sT = SelT[ex_ % 2]; rsT = r_SelT[ex_ % 2]
            yb = Yb[ex_ % 2]; ryb = r_Yb[ex_ % 2]
            units = []
            for blk in range(16):
                for half in range(2):
                    def unit(blk=blk, half=half):
                        ci = cnt5["c"] % 3
                        cnt5["c"] += 1
                        for s_ in range(2):
                            fw.op("pe", lambda e, s_=s_: e.matmul(
                                pC[ci][:], lhsT=sT[:, s_, blk * 128:(blk + 1) * 128],
                                rhs=yb[:, s_, half * 512:(half + 1) * 512], start=(s_ == 0), stop=(s_ == 1)),
                                inc=(s_ == 1), reads=[rsT, ryb], writes=[r_pC[ci]])
                        fw.op("dve", lambda e: e.scalar_tensor_tensor(
                            out=x1[:, blk, half * 512:(half + 1) * 512], in0=pC[ci][:], scalar=Cw[:, blk, ex_:ex_ + 1],
                            in1=x1[:, blk, half * 512:(half + 1) * 512], op0=ALU.mult, op1=ALU.add),
                            reads=[r_pC[ci], r_C, r_x1], writes=[r_x1])
                    units.append(unit)
            return units

        w_begin(0)
        for _ in range(3):
            w_issue()
        for _ in stage_a(0):
            w_cast()
            w_issue()
        for ex_ in range(32):
            units = stage_b_units(ex_)
            if ex_ + 1 < 32:
                w_begin(ex_ + 1)
                for _ in range(3):
                    w_issue()
                ui = 0
                for _ in stage_a(ex_ + 1):
                    w_cast()
                    w_issue()
                    for _k in range(2):
                        if ui < len(units):
                            units[ui]()
                            ui += 1
                while wq5["todo"] or wq5["issued"]:
                    w_cast()
                    w_issue()
                while ui < len(units):
                    units[ui]()
                    ui += 1
            else:
                for u in units:
                    u()
        fw.barrier()
        es6.close()

        set_free([[104 * KB, 203 * KB]])
        es7 = ExitStack()
        cur["es"] = es7
        gF = sb("gF", [128, D]); r_gF = Res("gF")
        ob = [sb("ob%d" % i, [128, D]) for i in range(2)]
        r_ob = [Res("ob%d" % i) for i in range(2)]
        fin = sb("fin", [128, 4]); r_fin = Res("fin")
        fw.dma("sp", gF[:], g_fin[:, :], writes=[r_gF])
        out_v = out_d.rearrange("(b p) d -> p b d", p=128)
        for blk in range(16):
            o_ = ob[blk % 2]; ro_ = r_ob[blk % 2]
            fw.op("act", lambda e, blk=blk, o_=o_: e.activation(out=o_[:], in_=x1[:, blk, :], func=AF.Square,
                                                               accum_out=fin[:, 0:1]),
                  reads=[r_x1], writes=[ro_, r_fin])
            fw.op("act", lambda e: e.activation(out=fin[:, 1:2], in_=fin[:, 0:1], func=AF.Ln, bias=eps_sb[:, 0:1],
                                                scale=1.0 / D), reads=[r_fin, r_ones], writes=[r_fin])
            fw.op("act", lambda e: e.activation(out=fin[:, 1:2], in_=fin[:, 1:2], func=AF.Exp, scale=-0.5),
                  reads=[r_fin], writes=[r_fin])
            fw.op("dve", lambda e, blk=blk, o_=o_: e.scalar_tensor_tensor(
                out=o_[:], in0=x1[:, blk, :], scalar=fin[:, 1:2], in1=gF[:], op0=ALU.mult, op1=ALU.mult),
                reads=[r_x1, r_fin, r_gF], writes=[ro_])
            fw.dma("sp", out_v[:, blk, :], o_[:], reads=[ro_])
        fw.barrier()
        fw.final_wait("sp")
        es7.close()
    return nc


def _bf(a):
    return np.asarray(a, dtype=np.float32).astype(ml_dtypes.bfloat16)


def make_in_maps(inputs):
    x = np.asarray(inputs["x"], dtype=np.float32)
    f = lambda k: np.asarray(inputs[k], dtype=np.float32)
    pc = lambda v: np.ascontiguousarray(v.reshape(-1, 128).T)
    g_mix = pc(f("norm_mix_g")[0])
    w_in = np.ascontiguousarray(f("w_in")[0])
    conv_w = np.ascontiguousarray(f("conv_w")[0].reshape(4, 4, 128).transpose(2, 1, 0))
    conv_b = pc(f("conv_b")[0])

    def bd(w):
        o = np.zeros((128, 4, 128), np.float32)
        for n in range(8):
            g, hlf = n // 2, n % 2
            o[hlf * 64:(hlf + 1) * 64, g, hlf * 64:(hlf + 1) * 64] = w[n]
        return o
    wr_bd = bd(f("w_r")[0])
    wi_bd = bd(f("w_i")[0])
    tk = np.arange(S)
    kaug = _bf(np.stack([tk // 64, tk % 64, np.ones(S), np.ones(S)]).astype(np.float32))
    w_o_attn = np.ascontiguousarray(f("w_o_attn")[0])
    w_o_lru = np.ascontiguousarray(f("w_o_lru")[0])
    w_out_ = np.ascontiguousarray(f("w_out")[0])
    g_ffn = pc(f("norm_ffn_g")[0])
    w_router = np.ascontiguousarray(np.concatenate(
        [f("w_group")[0], f("w_expert_router")[0].transpose(1, 0, 2).reshape(D, 32)], axis=1))
    w_gate_ = np.ascontiguousarray(f("w_gate")[0])
    w_up_ = np.ascontiguousarray(f("w_up")[0])
    w_down_ = np.ascontiguousarray(f("w_down")[0])
    g_fin = np.ascontiguousarray(np.tile(f("final_norm_g")[None, :], (128, 1)))
    g_ffn_rep = np.ascontiguousarray(np.tile(f("norm_ffn_g")[0][None, :], (128, 1)))
    iota_s = np.ascontiguousarray(np.tile(np.arange(256, dtype=np.float32)[None, :], (128, 1)))
    iota_t = np.ascontiguousarray(np.tile(np.arange(1, TOWN + 1, dtype=np.float32)[None, :], (128, 1)))
    ustrict = _bf((np.arange(128)[:, None] < np.arange(128)[None, :]).astype(np.float32))
    code = (np.arange(16)[None, :] * 128 + np.arange(128)[:, None] + 1)
    tvals = _bf(np.stack([code // 64, code % 64] + [np.zeros_like(code)] * 6, axis=-1).astype(np.float32))
    maps = []
    for c in range(NCORES):
        b, j = c // 4, c % 4
        xn = np.ascontiguousarray(x[b].T)
        own_blocks = [4 * i + j for i in range(16)]
        tq = np.concatenate([np.arange(bl * 128, (bl + 1) * 128) for bl in own_blocks])
        xo = np.ascontiguousarray(x[b][tq].T)
        qa = np.zeros((NH, 4, TOWN), np.float32)
        for h in range(NH):
            s8 = SLOPES[h] * 8.0
            qa[h, 0] = 64.0 * s8
            qa[h, 1] = s8
            qa[h, 2] = -64.0 * s8 * (tq // 64)
            qa[h, 3] = -s8 * (tq % 64)
        cm = np.zeros((128, 4, 128), np.float32)
        for jj in range(4):
            if jj < j:
                cm[:, jj, :] = 1.0
            elif jj == j:
                cm[:, jj, :] = (np.arange(128)[:, None] <= np.arange(128)[None, :])
        sj = np.zeros((128, 4), np.float32)
        sj[:, j] = 1.0
        extra = {
            "xot": np.ascontiguousarray(x[b][tq]),
            "lam_qk": np.ascontiguousarray(f("lambda_qk")[0].reshape(4, 64).T),
            "subln": np.ascontiguousarray(f("subln_g")[0].reshape(128, 1)),
            "w_o_attn": w_o_attn, "w_o_lru": w_o_lru, "w_out": w_out_, "g_ffn": g_ffn,
            "w_router": w_router, "w_gate": w_gate_, "w_up": w_up_, "w_down": w_down_, "g_fin": g_fin, "g_ffn_rep": g_ffn_rep,
            "iota_s": iota_s, "iota_t": iota_t, "ustrict": ustrict, "tvals": tvals,
        }
        maps.append({
            "xn": xn, "xo": xo, "g_mix": g_mix, "w_in": w_in, "kaug": kaug, "qaug": _bf(qa),
            "cmask": _bf(cm), "ident": _bf(np.eye(128, dtype=np.float32)), "selj": sj, "conv_w": conv_w, "conv_b": conv_b,
            "wr_bd": wr_bd, "wi_bd": wi_bd, "b_r": pc(f("b_r")[0]), "b_i": pc(f("b_i")[0]),
            "lru_lam": pc(f("lru_lambda")[0]), **extra,
        })
    return maps


def kernel(**inputs):
    nc = build()
    in_maps = make_in_maps(inputs)
    res = run_bass_kernel_spmd(nc, in_maps, core_ids=list(range(NCORES)))
    out = np.zeros((NB, S, D), np.float32)
    for c in range(NCORES):
        b, j = c // 4, c % 4
        o = np.asarray(res.results[c]["out"])
        for i in range(16):
            bl = 4 * i + j
            out[b, bl * 128:(bl + 1) * 128, :] = o[i * 128:(i + 1) * 128, :]
    return out
```

```python
import numpy as np
import ml_dtypes
from contextlib import ExitStack
import concourse.bass as bass
import concourse.mybir as mybir
from concourse.bass_utils import run_bass_kernel_spmd

F32 = mybir.dt.float32
BF16 = mybir.dt.bfloat16
I32 = mybir.dt.int32
AF = mybir.ActivationFunctionType
ALU = mybir.AluOpType

D = 1024
S = 8192
NB = 2
NH = 4
HD = 64
TOWN = 2048
NCORES = 8
EPS = 1e-6
EPOCH = 12000
KA = 68
SLOPES = [2.0 ** (-8.0 * (h + 1) / NH) for h in range(NH)]
LAM_INIT = 0.8 - 0.6 * 1.0
GELU_K = 0.7978845608028654


class Res:
    __slots__ = ("name", "w", "r")

    def __init__(self, name):
        self.name = name
        self.w = None
        self.r = {}


class EngState:
    def __init__(self, key, eng):
        self.key = key
        self.eng = eng
        self.count = 0
        self.sems = []
        self.known = {}


class FW:
    def __init__(self, nc, es):
        self.nc = nc
        self.es = es
        self.engs = {}
        for key, eng in (("pe", nc.tensor), ("act", nc.scalar), ("dve", nc.vector),
                         ("pool", nc.gpsimd), ("sp", nc.sync)):
            self.engs[key] = EngState(key, eng)
        self.NPOOL = 12
        self.dma_pool = {q: [es.enter_context(nc.semaphore("dq_%s%d" % (q, i))) for i in range(self.NPOOL)]
                         for q in ("sp", "pool")}
        self.dma_pool_uses = {q: [0] * self.NPOOL for q in ("sp", "pool")}
        self.dma_next = {"sp": 0, "pool": 0}
        self.dma_tokens = {}
        self.n_dma = 0

    def _sem_for(self, st, idx):
        e = idx // EPOCH
        while len(st.sems) <= e:
            st.sems.append(self.es.enter_context(
                self.nc.semaphore("e_%s_%d" % (st.key, len(st.sems)))))
        return st.sems[e], idx % EPOCH + 1

    def _wait(self, st, dep):
        key, idx = dep
        if key == "dma":
            if st.known.get(dep, False):
                return
            sem, val = self.dma_tokens[idx]
            st.eng.wait_ge(sem, val)
            st.known[dep] = True
            return
        if key == st.key and key == "pe":
            return
        if st.known.get(key, -1) >= idx:
            return
        sem, val = self._sem_for(self.engs[key], idx)
        st.eng.wait_ge(sem, val)
        st.known[key] = idx

    def _collect(self, st, reads, writes):
        deps = {}
        dma_deps = []

        def add(d):
            if d is None:
                return
            if d[0] == "dma":
                dma_deps.append(d)
            elif deps.get(d[0], -1) < d[1]:
                deps[d[0]] = d[1]
        for r in reads:
            add(r.w)
        for w in writes:
            add(w.w)
            for k, i in w.r.items():
                if k == "dma":
                    for tok in i:
                        add(("dma", tok))
                else:
                    add((k, i))
        for d in dma_deps:
            self._wait(st, d)
        for k, i in deps.items():
            self._wait(st, (k, i))

    def op(self, engkey, fn, reads=(), writes=(), inc=True):
        if getattr(self, "defer", None) is not None:
            self.defer.append((engkey, fn, list(reads), list(writes), inc))
            return None
        st = self.engs[engkey]
        self._collect(st, reads, writes)
        ins = fn(st.eng)
        idx = st.count
        if inc:
            sem, _ = self._sem_for(st, idx)
            ins.then_inc(sem, 1)
            st.count += 1
        for r in reads:
            r.r[engkey] = idx
        for w in writes:
            w.w = (engkey, idx)
            w.r = {}
        return ins

    def dma(self, engkey, out, in_, reads=(), writes=(), **kw):
        st = self.engs[engkey]
        self._collect(st, reads, writes)
        slot = self.dma_next[engkey]
        self.dma_next[engkey] = (slot + 1) % self.NPOOL
        sem = self.dma_pool[engkey][slot]
        prev = self.dma_pool_uses[engkey][slot]
        if prev > 0:
            st.eng.wait_ge(sem, 16 * prev)
        ins = st.eng.dma_start(out=out, in_=in_, **kw)
        ins.then_inc(sem, 16)
        self.dma_pool_uses[engkey][slot] = prev + 1
        tok = self.n_dma
        self.n_dma += 1
        self.dma_tokens[tok] = (sem, 16 * (prev + 1))
        for r in reads:
            r.r.setdefault("dma", []).append(tok)
        for w in writes:
            w.w = ("dma", tok)
            w.r = {}
        return tok

    def barrier(self):
        for st in self.engs.values():
            for k2, st2 in self.engs.items():
                if st2.count > 0 and k2 != st.key:
                    self._wait(st, (k2, st2.count - 1))
            for q in ("sp", "pool"):
                for slot in range(self.NPOOL):
                    u = self.dma_pool_uses[q][slot]
                    if u > 0:
                        st.eng.wait_ge(self.dma_pool[q][slot], 16 * u)

    def final_wait(self, engkey="sp"):
        st = self.engs[engkey]
        for q in ("sp", "pool"):
            for slot in range(self.NPOOL):
                u = self.dma_pool_uses[q][slot]
                if u > 0:
                    st.eng.wait_ge(self.dma_pool[q][slot], 16 * u)


def build(stage=99, debug=False):
    nc = bass.Bass("TRN2", target_bir_lowering=False)
    es = ExitStack()

    def din(name, shape, dt=F32):
        return nc.dram_tensor(name, list(shape), dt, kind="ExternalInput").ap()

    def dout(name, shape, dt=F32):
        return nc.dram_tensor(name, list(shape), dt, kind="ExternalOutput").ap()

    def dscr(name, shape, dt):
        return nc.dram_tensor(name, list(shape), dt, kind="Internal").ap()

    xn = din("xn", [D, S])
    xo = din("xo", [D, TOWN])
    g_mix = din("g_mix", [128, 8])
    w_in = din("w_in", [D, 4608])
    kaug = din("kaug", [4, S], BF16)
    qaug = din("qaug", [NH, 4, TOWN], BF16)
    cmask = din("cmask", [128, 4, 128], BF16)
    ident = din("ident", [128, 128], BF16)
    selj = din("selj", [128, 4])
    conv_w = din("conv_w", [128, 4, 4])
    conv_b = din("conv_b", [128, 4])
    wr_bd = din("wr_bd", [128, 4, 128])
    wi_bd = din("wi_bd", [128, 4, 128])
    b_r = din("b_r", [128, 4])
    b_i = din("b_i", [128, 4])
    lru_lam = din("lru_lam", [128, 4])
    xot = din("xot", [TOWN, D])
    lam_qk = din("lam_qk", [64, 4])
    subln = din("subln", [128, 1])
    w_o_attn = din("w_o_attn", [512, D])
    w_o_lru = din("w_o_lru", [512, D])
    w_out = din("w_out", [D, D])
    g_ffn = din("g_ffn", [128, 8])
    w_router = din("w_router", [D, 36])
    w_gate = din("w_gate", [32, D, 512])
    w_up = din("w_up", [32, D, 512])
    w_down = din("w_down", [32, 512, D])
    g_fin = din("g_fin", [128, D])
    g_ffn_rep = din("g_ffn_rep", [128, D])
    iota_s_d = din("iota_s", [128, 256])
    iota_t_d = din("iota_t", [128, TOWN])
    ustrict_d = din("ustrict", [128, 128], BF16)
    tvals_d = din("tvals", [128, 16, 8], BF16)

    dbg = {}
    if debug:
        dbg["kt"] = dout("dbg_kt", [8, 64, S], BF16)
        dbg["v"] = dout("dbg_v", [S, 512], BF16)
        dbg["lru"] = dout("dbg_lru", [128, 4, TOWN])
    out_d = dout("out", [TOWN, D])

    kt_scr = dbg["kt"] if debug else dscr("kt_scr", [8, 64, S], BF16)
    v_scr = dbg["v"] if debug else dscr("v_scr", [S, 512], BF16)

    with es:
        fw = FW(nc, es)

        KB = 1024
        BASE = 17 * KB
        DTB = {F32: 4, BF16: 2, I32: 4}
        cur = {"iv": [[0, 8 * KB]], "es": es}

        def set_free(intervals):
            cur["iv"] = [list(x) for x in intervals]

        def sb(name, shape, dt=F32):
            n = DTB[dt]
            for d_ in shape[1:]:
                n *= d_
            n = (n + 63) // 64 * 64
            for iv in cur["iv"]:
                if iv[1] - iv[0] >= n:
                    off = iv[0]
                    iv[0] += n
                    return nc.alloc_sbuf_tensor_at(name, list(shape), dt, offset=off + BASE)
            raise RuntimeError("SBUF arena full for %s (%d bytes) free=%s" % (name, n, cur["iv"]))

        def sb_at(name, shape, dt, off):
            return nc.alloc_sbuf_tensor_at(name, list(shape), dt, offset=off + BASE)

        def ps(name, shape, dt=F32):
            return cur["es"].enter_context(nc.psum_tensor(name, list(shape), dt))

        ones_bf = sb("ones_bf", [128, 128], BF16)
        r_ones = Res("ones")
        fw.op("pool", lambda e: e.memset(ones_bf[:], 1.0), writes=[r_ones])
        eps_sb = sb("eps_sb", [128, 1])
        one_sb = sb("one_sb", [128, 1])
        fw.op("pool", lambda e: e.memset(eps_sb[:], EPS), writes=[r_ones])
        fw.op("pool", lambda e: e.memset(one_sb[:], 1.0), writes=[r_ones])
        g_sb = sb("g_sb", [128, 8])
        r_g = Res("g")
        fw.dma("sp", g_sb[:], g_mix[:, :], writes=[r_g])
        Cw = sb("Cw", [128, 16, 32]); r_C = Res("C")
        selj_sb = sb("selj_sb", [128, 4])
        cw_sb = sb("cw_sb", [128, 4, 4])
        cb_sb = sb("cb_sb", [128, 4])
        br_sb = sb("br_sb", [128, 4])
        bi_sb = sb("bi_sb", [128, 4])
        lam_sb = sb("lam_sb", [128, 4])
        r_small = Res("small")
        for t_sb, t_d in ((selj_sb, selj), (cb_sb, conv_b), (br_sb, b_r), (bi_sb, b_i),
                          (lam_sb, lru_lam)):
            fw.dma("sp", t_sb[:], t_d[:, :], writes=[r_small])
        fw.dma("sp", cw_sb[:], conv_w[:, :, :], writes=[r_small])
        wr_sb = sb("wr_sb", [128, 4, 128], BF16)
        wi_sb = sb("wi_sb", [128, 4, 128], BF16)
        r_wgate = Res("wgate")
        fw.dma("pool", wr_sb[:], wr_bd[:, :, :], writes=[r_wgate])
        fw.dma("pool", wi_sb[:], wi_bd[:, :, :], writes=[r_wgate])

        ex = sb("ex", [128, 4])
        pl = sb("pl", [128, 4])
        hc = sb("hc", [128, 4])
        cc = sb("cc", [128, 4])
        hbr = sb("hbr", [128, 4])
        hbi = sb("hbi", [128, 4])
        r_const = Res("lruconst")
        fw.op("act", lambda e: e.activation(out=ex[:], in_=lam_sb[:], func=AF.Exp, scale=-1.0),
              reads=[r_small], writes=[r_const])
        fw.op("dve", lambda e: e.tensor_scalar(out=pl[:], in0=ex[:], scalar1=-0.25, scalar2=1.0 / 3.0,
                                               op0=ALU.mult, op1=ALU.add), reads=[r_const], writes=[r_const])
        fw.op("dve", lambda e: e.tensor_tensor(out=pl[:], in0=pl[:], in1=ex[:], op=ALU.mult),
              reads=[r_const], writes=[r_const])
        fw.op("dve", lambda e: e.tensor_scalar(out=pl[:], in0=pl[:], scalar1=-0.5, scalar2=None,
                                               op0=ALU.add), reads=[r_const], writes=[r_const])
        fw.op("dve", lambda e: e.tensor_tensor(out=pl[:], in0=pl[:], in1=ex[:], op=ALU.mult),
              reads=[r_const], writes=[r_const])
        fw.op("dve", lambda e: e.tensor_scalar(out=pl[:], in0=pl[:], scalar1=1.0, scalar2=None,
                                               op0=ALU.add), reads=[r_const], writes=[r_const])
        fw.op("dve", lambda e: e.tensor_tensor(out=pl[:], in0=pl[:], in1=ex[:], op=ALU.mult),
              reads=[r_const], writes=[r_const])
        fw.op("dve", lambda e: e.tensor_scalar(out=cc[:], in0=pl[:], scalar1=-8.0, scalar2=None,
                                               op0=ALU.mult), reads=[r_const], writes=[r_const])
        fw.op("dve", lambda e: e.tensor_scalar(out=hc[:], in0=pl[:], scalar1=-4.0, scalar2=None,
                                               op0=ALU.mult), reads=[r_const], writes=[r_const])
        fw.op("dve", lambda e: e.tensor_scalar(out=hbr[:], in0=br_sb[:], scalar1=0.5, scalar2=None,
                                               op0=ALU.mult), reads=[r_small], writes=[r_const])
        fw.op("dve", lambda e: e.tensor_scalar(out=hbi[:], in0=bi_sb[:], scalar1=0.5, scalar2=None,
                                               op0=ALU.mult), reads=[r_small], writes=[r_const])

        lru_own = sb_at("lru_own", [128, 4, TOWN], F32, 8 * KB)
        r_lru = Res("lru_own")
        qt = sb_at("qt", [128, 8, TOWN], BF16, 40 * KB)
        hn_own = sb_at("hn_own", [128, 8, TOWN], BF16, 72 * KB)
        lruA = sb_at("lruA", [128, 4, TOWN], BF16, 104 * KB)
        attnT = sb_at("attnT", [128, 4, TOWN], BF16, 120 * KB)
        merged = sb_at("merged", [128, 8, TOWN], BF16, 136 * KB)
        x1 = sb_at("x1", [128, 16, D], F32, 8 * KB)
        hn2k = sb_at("hn2k", [128, 16, D], BF16, 72 * KB)
        Mf = sb_at("Mf", [128, 16, 32], F32, 203 * KB)
        Mb = sb_at("Mb", [128, 16, 32], BF16, 205 * KB)
        r_M = Res("M")
        TOP = 207 * KB
        set_free([[40 * KB, TOP]])
        es1 = ExitStack()
        cur["es"] = es1
        wk_sb = sb("wk_sb", [128, 8, 512], BF16)
        wv_sb = sb("wv_sb", [128, 8, 512], BF16)
        wx_sb = sb("wx_sb", [128, 8, 512], BF16)
        r_w1 = Res("w1")
        w_in_v = w_in.rearrange("(c p) n -> p c n", p=128)

        TT = 512
        NT = S // TT
        xin = [sb("xin%d" % i, [128, 8, TT]) for i in range(2)]
        r_xin = [Res("xin%d" % i) for i in range(2)]
        for i_, (wsb, c0) in enumerate(((wk_sb, 512), (wv_sb, 1024), (wx_sb, 1536))):
            xb_ = xin[i_ % 2]; rxb_ = r_xin[i_ % 2]
            for c in range(0, 8, 4):
                fw.dma("sp", xb_[:, c:c + 4, :], w_in_v[:, c:c + 4, c0:c0 + 512], writes=[rxb_])
            fw.op("act", lambda e, wsb=wsb, xb_=xb_: e.activation(out=wsb[:, 0:4, :], in_=xb_[:, 0:4, :], func=AF.Copy),
                  reads=[rxb_], writes=[r_w1])
            fw.op("dve", lambda e, wsb=wsb, xb_=xb_: e.tensor_copy(out=wsb[:, 4:8, :], in_=xb_[:, 4:8, :]),
                  reads=[rxb_], writes=[r_w1])
        xsq = sb("xsq", [128, 8, TT], BF16)
        r_xsq = Res("xsq")
        rb = sb("rb", [128, TT])
        r_rb = Res("rb")
        hn = [sb("hn%d" % i, [128, 8, TT], BF16) for i in range(2)]
        r_hn = [Res("hn%d" % i) for i in range(2)]
        kst = [sb("kst%d" % i, [64, 8, TT], BF16) for i in range(2)]
        r_kst = [Res("kst%d" % i) for i in range(2)]
        vst = [sb("vst%d" % i, [128, 4, 512], BF16) for i in range(2)]
        r_vst = [Res("vst%d" % i) for i in range(2)]
        xrp = [sb("xrp%d" % i, [128, 4, TT + 3]) for i in range(2)]
        r_xrp = [[Res("xrp%d_%d" % (i, g)) for g in range(4)] for i in range(2)]
        xcL = [sb("xc%d" % i, [128, TT]) for i in range(2)]; r_xcL = [Res("xc%d" % i) for i in range(2)]
        xcbL = [sb("xcb%d" % i, [128, TT], BF16) for i in range(2)]; r_xcbL = [Res("xcb%d" % i) for i in range(2)]
        thrL = [sb("thr%d" % i, [128, TT]) for i in range(2)]; r_thrL = [Res("thr%d" % i) for i in range(2)]
        thiL = [sb("thi%d" % i, [128, TT]) for i in range(2)]; r_thiL = [Res("thi%d" % i) for i in range(2)]
        a_tL = [sb("a_t%d" % i, [128, TT]) for i in range(2)]; r_aL = [Res("a%d" % i) for i in range(2)]
        a2_tL = [sb("a2_t%d" % i, [128, TT]) for i in range(2)]; r_a2L = [Res("a2%d" % i) for i in range(2)]
        u_tL = [sb("u_t%d" % i, [128, TT]) for i in range(2)]; r_uL = [Res("u%d" % i) for i in range(2)]
        h_t = [sb("h_t%d" % g, [128, TT]) for g in range(4)]
        r_h = [Res("h%d" % g) for g in range(4)]
        carry = sb("carry", [128, 4])
        r_carry = [Res("carry%d" % g) for g in range(4)]

        ps_ss = ps("ps_ss", [128, TT]); r_pss = Res("ps_ss")
        ps_k = [ps("ps_k%d" % i, [64, TT]) for i in range(2)]
        r_psk = [Res("ps_k%d" % i) for i in range(2)]
        ps_v = [ps("ps_v%d" % i, [128, 512]) for i in range(2)]
        r_psv = [Res("ps_v%d" % i) for i in range(2)]
        ps_x = ps("ps_x", [128, TT]); r_psx = Res("ps_x")
        ps_r = ps("ps_r", [128, TT]); r_psr = Res("ps_r")
        ps_i = ps("ps_i", [128, TT]); r_psi = Res("ps_i")

        for g in range(4):
            fw.op("pool", lambda e, g=g: e.memset(xrp[0][:, g, 0:3], 0.0), writes=[r_xrp[0][g]])

        xn_v = xn.rearrange("(c p) t -> p c t", p=128)
        cnt = {"k": 0, "v": 0}

        def xload(k):
            t0 = k * TT
            xb = xin[k % 2]; rxb = r_xin[k % 2]
            for c in range(0, 8, 4):
                fw.dma("sp", xb[:, c:c + 4, :], xn_v[:, c:c + 4, t0:t0 + TT], writes=[rxb])

        def front(k):
            xb = xin[k % 2]; rxb = r_xin[k % 2]
            hb = hn[k % 2]; rhb = r_hn[k % 2]
            fw.op("act", lambda e: e.activation(out=xsq[:], in_=xb[:], func=AF.Square),
                  reads=[rxb], writes=[r_xsq])
            for c in range(8):
                fw.op("pe", lambda e, c=c: e.matmul(ps_ss[:], lhsT=ones_bf[:], rhs=xsq[:, c, :],
                                                    start=(c == 0), stop=(c == 7)),
                      inc=(c == 7), reads=[r_ones, r_xsq], writes=[r_pss])
            fw.op("act", lambda e: e.activation(out=rb[:], in_=ps_ss[:], func=AF.Ln, bias=eps_sb[:, 0:1],
                                                scale=1.0 / D), reads=[r_pss, r_ones], writes=[r_rb])
            fw.op("act", lambda e: e.activation(out=rb[:], in_=rb[:], func=AF.Exp, scale=-0.5),
                  reads=[r_rb], writes=[r_rb])
            for c in range(8):
                fw.op("dve", lambda e, c=c: e.scalar_tensor_tensor(
                    out=hb[:, c, :], in0=xb[:, c, :], scalar=g_sb[:, c:c + 1], in1=rb[:],
                    op0=ALU.mult, op1=ALU.mult), reads=[rxb, r_g, r_rb], writes=[rhb])

        def kv_quarter(k, q):
            t0 = k * TT
            hb = hn[k % 2]; rhb = r_hn[k % 2]
            ks = kst[k % 2]; rks = r_kst[k % 2]
            vs = vst[k % 2]; rvs = r_vst[k % 2]
            for hm in (2 * q, 2 * q + 1):
                pk = ps_k[cnt["k"] % 2]; rpk = r_psk[cnt["k"] % 2]
                cnt["k"] += 1
                for c in range(8):
                    fw.op("pe", lambda e, c=c, hm=hm, pk=pk: e.matmul(
                        pk[:], lhsT=wk_sb[:, c, hm * 64:(hm + 1) * 64], rhs=hb[:, c, :],
                        start=(c == 0), stop=(c == 7)), inc=(c == 7), reads=[r_w1, rhb], writes=[rpk])
                fw.op("act", lambda e, hm=hm, pk=pk: e.activation(out=ks[:, hm, :], in_=pk[:], func=AF.Copy),
                      reads=[rpk], writes=[rks])
            tb = q
            pv = ps_v[cnt["v"] % 2]; rpv = r_psv[cnt["v"] % 2]
            cnt["v"] += 1
            for c in range(8):
                fw.op("pe", lambda e, c=c, tb=tb, pv=pv: e.matmul(
                    pv[:], lhsT=hb[:, c, tb * 128:(tb + 1) * 128], rhs=wv_sb[:, c, :],
                    start=(c == 0), stop=(c == 7)), inc=(c == 7), reads=[r_w1, rhb], writes=[rpv])
            fw.op("dve", lambda e, tb=tb, pv=pv: e.tensor_copy(out=vs[:, tb, :], in_=pv[:]),
                  reads=[rpv], writes=[rvs])
            if q == 3:
                fw.dma("pool", kt_scr[:, :, t0:t0 + TT].rearrange("h p t -> p h t"), ks[:, :, :], reads=[rks])
                fw.dma("pool", v_scr[t0:t0 + TT, :].rearrange("(b p) n -> p b n", p=128), vs[:, :, :], reads=[rvs])

        def lru_a(k, g):
            gi = g % 2
            xc = xcL[gi]; r_xc = r_xcL[gi]; xcb = xcbL[gi]; r_xcb = r_xcbL[gi]
            thr = thrL[gi]; r_thr = r_thrL[gi]; thi = thiL[gi]; r_thi = r_thiL[gi]
            a_t = a_tL[gi]; r_a = r_aL[gi]; a2_t = a2_tL[gi]; r_a2 = r_a2L[gi]; u_t = u_tL[gi]; r_u = r_uL[gi]
            hb = hn[k % 2]; rhb = r_hn[k % 2]
            xp = xrp[k % 2]; xp_n = xrp[(k + 1) % 2]
            rxp = r_xrp[k % 2][g]; rxpn = r_xrp[(k + 1) % 2][g]
            for c in range(8):
                fw.op("pe", lambda e, c=c, g=g: e.matmul(
                    ps_x[:], lhsT=wx_sb[:, c, g * 128:(g + 1) * 128], rhs=hb[:, c, :],
                    start=(c == 0), stop=(c == 7)), inc=(c == 7), reads=[r_w1, rhb], writes=[r_psx])
            fw.op("act", lambda e, g=g: e.activation(out=xp[:, g, 3:TT + 3], in_=ps_x[:], func=AF.Copy),
                  reads=[r_psx], writes=[rxp])
            fw.op("pool", lambda e, g=g: e.tensor_copy(out=xp_n[:, g, 0:3], in_=xp[:, g, TT:TT + 3]),
                  reads=[rxp], writes=[rxpn])
            fw.op("dve", lambda e, g=g: e.tensor_scalar(
                out=xc[:], in0=xp[:, g, 0:TT], scalar1=cw_sb[:, g, 0:1], scalar2=cb_sb[:, g:g + 1],
                op0=ALU.mult, op1=ALU.add), reads=[rxp, r_small], writes=[r_xc])
            for j in range(1, 4):
                fw.op("dve", lambda e, g=g, j=j: e.scalar_tensor_tensor(
                    out=xc[:], in0=xp[:, g, j:j + TT], scalar=cw_sb[:, g, j:j + 1], in1=xc[:],
                    op0=ALU.mult, op1=ALU.add), reads=[rxp, r_small, r_xc], writes=[r_xc])
            fw.op("pool", lambda e: e.tensor_copy(out=xcb[:], in_=xc[:]), reads=[r_xc], writes=[r_xcb])

        def lru_b(k, g):
            gi = g % 2
            xc = xcL[gi]; r_xc = r_xcL[gi]; xcb = xcbL[gi]; r_xcb = r_xcbL[gi]
            thr = thrL[gi]; r_thr = r_thrL[gi]; thi = thiL[gi]; r_thi = r_thiL[gi]
            a_t = a_tL[gi]; r_a = r_aL[gi]; a2_t = a2_tL[gi]; r_a2 = r_a2L[gi]; u_t = u_tL[gi]; r_u = r_uL[gi]
            fw.op("pe", lambda e, g=g: e.matmul(ps_r[:], lhsT=wr_sb[:, g, :], rhs=xcb[:], start=True, stop=True),
                  inc=True, reads=[r_wgate, r_xcb], writes=[r_psr])
            fw.op("pe", lambda e, g=g: e.matmul(ps_i[:], lhsT=wi_sb[:, g, :], rhs=xcb[:], start=True, stop=True),
                  inc=True, reads=[r_wgate, r_xcb], writes=[r_psi])
            fw.op("act", lambda e, g=g: e.activation(out=thr[:], in_=ps_r[:], func=AF.Tanh,
                                                     bias=hbr[:, g:g + 1], scale=0.5),
                  reads=[r_psr, r_const], writes=[r_thr])
            fw.op("act", lambda e, g=g: e.activation(out=thi[:], in_=ps_i[:], func=AF.Tanh,
                                                     bias=hbi[:, g:g + 1], scale=0.5),
                  reads=[r_psi, r_const], writes=[r_thi])
            fw.op("act", lambda e, g=g: e.activation(out=a_t[:], in_=thr[:], func=AF.Exp,
                                                     bias=hc[:, g:g + 1], scale=hc[:, g:g + 1]),
                  reads=[r_thr, r_const], writes=[r_a])
            fw.op("act", lambda e, g=g: e.activation(out=a2_t[:], in_=thr[:], func=AF.Exp,
                                                     bias=cc[:, g:g + 1], scale=cc[:, g:g + 1]),
                  reads=[r_thr, r_const], writes=[r_a2])
            fw.op("act", lambda e: e.activation(out=a2_t[:], in_=a2_t[:], func=AF.Ln, bias=one_sb[:, 0:1],
                                                scale=-1.0), reads=[r_a2, r_ones], writes=[r_a2])
            fw.op("act", lambda e: e.activation(out=a2_t[:], in_=a2_t[:], func=AF.Exp, scale=0.5),
                  reads=[r_a2], writes=[r_a2])
            if k == 0:
                fw.op("dve", lambda e: e.memset(a2_t[:, 0:1], 1.0), reads=[r_a2], writes=[r_a2])
            fw.op("dve", lambda e: e.scalar_tensor_tensor(out=u_t[:], in0=thi[:], scalar=1.0, in1=xc[:],
                                                          op0=ALU.add, op1=ALU.mult),
                  reads=[r_thi, r_xc], writes=[r_u])
            fw.op("dve", lambda e: e.scalar_tensor_tensor(out=u_t[:], in0=a2_t[:], scalar=0.5, in1=u_t[:],
                                                          op0=ALU.mult, op1=ALU.mult),
                  reads=[r_a2, r_u], writes=[r_u])
            if k == 0:
                fw.op("dve", lambda e, g=g: e.tensor_tensor_scan(out=h_t[g][:], data0=a_t[:], data1=u_t[:],
                                                                initial=0.0, op0=ALU.mult, op1=ALU.add),
                      reads=[r_a, r_u], writes=[r_h[g]])
            else:
                fw.op("dve", lambda e, g=g: e.tensor_copy(out=carry[:, g:g + 1], in_=h_t[g][:, TT - 1:TT]),
                      reads=[r_h[g]], writes=[r_carry[g]])
                fw.op("dve", lambda e, g=g: e.tensor_tensor_scan(out=h_t[g][:], data0=a_t[:], data1=u_t[:],
                                                                initial=carry[:, g:g + 1], op0=ALU.mult, op1=ALU.add),
                      reads=[r_a, r_u, r_carry[g]], writes=[r_h[g]])
            fw.op("dve", lambda e, g=g, k=k: e.tensor_scalar(
                out=lru_own[:, g, k * 128:(k + 1) * 128], in0=h_t[g][:, 0:128], scalar1=selj_sb[:, 0:1],
                scalar2=None, op0=ALU.mult), reads=[r_h[g], r_small], writes=[r_lru])
            for jj in range(1, 4):
                fw.op("dve", lambda e, g=g, k=k, jj=jj: e.scalar_tensor_tensor(
                    out=lru_own[:, g, k * 128:(k + 1) * 128], in0=h_t[g][:, jj * 128:(jj + 1) * 128],
                    scalar=selj_sb[:, jj:jj + 1], in1=lru_own[:, g, k * 128:(k + 1) * 128],
                    op0=ALU.mult, op1=ALU.add), reads=[r_h[g], r_small, r_lru], writes=[r_lru])

        xload(0)
        xload(1)
        front(0)
        for q in range(4):
            kv_quarter(0, q)
        for k in range(NT):
            lru_a(k, 0)
            if k + 1 < NT:
                front(k + 1)
            for g in range(4):
                if g + 1 < 4:
                    lru_a(k, g + 1)
                if g == 0 and k + 2 < NT:
                    xload(k + 2)
                if k + 1 < NT:
                    kv_quarter(k + 1, g)
                lru_b(k, g)
        fw.barrier()
        es1.close()

        set_free([[120 * KB, TOP]])
        es2 = ExitStack()
        cur["es"] = es2
        wq_sb = sb("wq_sb", [128, 8, 512], BF16)
        wy_sb = sb("wy_sb", [128, 8, 512], BF16)
        r_w2 = Res("w2")
        w2stg = [sb("w2stg%d" % i, [128, 8, 512]) for i in range(2)]
        r_w2stg = [Res("w2stg%d" % i) for i in range(2)]
        for i_, (wsb, c0) in enumerate(((wq_sb, 0), (wy_sb, 2048))):
            for c in range(0, 8, 4):
                fw.dma("sp", w2stg[i_][:, c:c + 4, :], w_in_v[:, c:c + 4, c0:c0 + 512], writes=[r_w2stg[i_]])
            fw.op("act", lambda e, wsb=wsb, i_=i_: e.activation(out=wsb[:, 0:4, :], in_=w2stg[i_][:, 0:4, :], func=AF.Copy),
                  reads=[r_w2stg[i_]], writes=[r_w2])
            fw.op("dve", lambda e, wsb=wsb, i_=i_: e.tensor_copy(out=wsb[:, 4:8, :], in_=w2stg[i_][:, 4:8, :]),
                  reads=[r_w2stg[i_]], writes=[r_w2])
        r_qt = Res("qt")
        for h in range(NH):
            for m_ in range(2):
                fw.dma("sp", qt[64:68, 2 * h + m_, :], qaug[h, :, :], writes=[r_qt])
        xin2 = sb("xin2", [128, 8, TT]); r_xin2 = Res("xin2")
        xsq2 = sb("xsq2", [128, 8, TT], BF16); r_xsq2 = Res("xsq2")
        rb2 = sb("rb2", [128, TT]); r_rb2 = Res("rb2")
        ysb = sb("ysb", [128, TT]); r_ysb = Res("ysb")
        y2 = sb("y2", [128, TT]); r_y2 = Res("y2")
        thy = sb("thy", [128, TT]); r_thy = Res("thy")
        r_hno = Res("hn_own")
        r_lruA = Res("lruA")
        p2_ss = ps("p2_ss", [128, TT]); r_p2ss = Res("p2ss")
        p2_q = [ps("p2_q%d" % i, [64, TT]) for i in range(2)]
        r_p2q = [Res("p2q%d" % i) for i in range(2)]
        p2_y = [ps("p2_y%d" % i, [128, TT]) for i in range(2)]
        r_p2y = [Res("p2y%d" % i) for i in range(2)]
        xo_v = xo.rearrange("(c p) t -> p c t", p=128)
        for m in range(4):
            t0 = m * TT
            for c in range(0, 8, 4):
                fw.dma("sp", xin2[:, c:c + 4, :], xo_v[:, c:c + 4, t0:t0 + TT], writes=[r_xin2])
            fw.op("act", lambda e: e.activation(out=xsq2[:], in_=xin2[:], func=AF.Square),
                  reads=[r_xin2], writes=[r_xsq2])
            for c in range(8):
                fw.op("pe", lambda e, c=c: e.matmul(p2_ss[:], lhsT=ones_bf[:], rhs=xsq2[:, c, :],
                                                    start=(c == 0), stop=(c == 7)),
                      inc=(c == 7), reads=[r_ones, r_xsq2], writes=[r_p2ss])
            fw.op("act", lambda e: e.activation(out=rb2[:], in_=p2_ss[:], func=AF.Ln, bias=eps_sb[:, 0:1],
                                                scale=1.0 / D), reads=[r_p2ss, r_ones], writes=[r_rb2])
            fw.op("act", lambda e: e.activation(out=rb2[:], in_=rb2[:], func=AF.Exp, scale=-0.5),
                  reads=[r_rb2], writes=[r_rb2])
            for c in range(8):
                fw.op("dve", lambda e, c=c: e.scalar_tensor_tensor(
                    out=hn_own[:, c, t0:t0 + TT], in0=xin2[:, c, :], scalar=g_sb[:, c:c + 1], in1=rb2[:],
                    op0=ALU.mult, op1=ALU.mult), reads=[r_xin2, r_g, r_rb2], writes=[r_hno])
            for hm in range(8):
                pq = p2_q[hm % 2]; rpq = r_p2q[hm % 2]
                for c in range(8):
                    fw.op("pe", lambda e, c=c, hm=hm, pq=pq: e.matmul(
                        pq[:], lhsT=wq_sb[:, c, hm * 64:(hm + 1) * 64], rhs=hn_own[:, c, t0:t0 + TT],
                        start=(c == 0), stop=(c == 7)), inc=(c == 7), reads=[r_w2, r_hno], writes=[rpq])
                fw.op("act", lambda e, hm=hm, pq=pq: e.activation(out=qt[0:64, hm, t0:t0 + TT], in_=pq[:], func=AF.Copy),
                      reads=[rpq], writes=[r_qt])
            for g in range(4):
                py = p2_y[g % 2]; rpy = r_p2y[g % 2]
                for c in range(8):
                    fw.op("pe", lambda e, c=c, g=g, py=py: e.matmul(
                        py[:], lhsT=wy_sb[:, c, g * 128:(g + 1) * 128], rhs=hn_own[:, c, t0:t0 + TT],
                        start=(c == 0), stop=(c == 7)), inc=(c == 7), reads=[r_w2, r_hno], writes=[rpy])
                fw.op("act", lambda e, py=py: e.activation(out=ysb[:], in_=py[:], func=AF.Copy),
                      reads=[rpy], writes=[r_ysb])
                fw.op("act", lambda e, py=py: e.activation(out=y2[:], in_=py[:], func=AF.Square),
                      reads=[rpy], writes=[r_y2])
                fw.op("dve", lambda e: e.tensor_scalar(out=y2[:], in0=y2[:], scalar1=0.044715, scalar2=1.0,
                                                       op0=ALU.mult, op1=ALU.add), reads=[r_y2], writes=[r_y2])
                fw.op("dve", lambda e: e.tensor_tensor(out=y2[:], in0=y2[:], in1=ysb[:], op=ALU.mult),
                      reads=[r_y2, r_ysb], writes=[r_y2])
                fw.op("act", lambda e: e.activation(out=thy[:], in_=y2[:], func=AF.Tanh, scale=GELU_K),
                      reads=[r_y2], writes=[r_thy])
                fw.op("dve", lambda e: e.scalar_tensor_tensor(out=thy[:], in0=thy[:], scalar=1.0, in1=ysb[:],
                                                              op0=ALU.add, op1=ALU.mult),
                      reads=[r_thy, r_ysb], writes=[r_thy])
                fw.op("dve", lambda e, g=g: e.scalar_tensor_tensor(
                    out=lruA[:, g, t0:t0 + TT], in0=thy[:], scalar=0.5, in1=lru_own[:, g, t0:t0 + TT],
                    op0=ALU.mult, op1=ALU.mult), reads=[r_thy, r_lru], writes=[r_lruA])
        fw.barrier()
        es2.close()

        set_free([[136 * KB, 172 * KB]])
        wgA = sb_at("wgA", [128, 8, D], BF16, 172 * KB)
        wgL = sb_at("wgL", [128, 8, D], BF16, 188 * KB)
        r_wgA = Res("wgA")
        for c in range(8):
            fw.dma("pool", wgA[:, c, :], w_in_v[:, c, 2560:2560 + D], writes=[r_wgA])
            fw.dma("pool", wgL[:, c, :], w_in_v[:, c, 3584:3584 + D], writes=[r_wgA])
        es3 = ExitStack()
        cur["es"] = es3
        kt = [sb_at("kt%d" % i, [128, S], BF16, 8 * KB + i * 16 * KB) for i in range(2)]
        r_kt = [Res("kt%d" % i) for i in range(2)]
        vh = sb("vh", [128, 64, 128], BF16); r_vh = Res("vh")
        pt = [sb("pt%d" % i, [128, 2, 512], BF16) for i in range(2)]
        r_pt = [Res("pt%d" % i) for i in range(2)]
        rl = sb("rl", [128, 2, 512]); r_rl = Res("rl")
        dd = sb("dd", [128, 512]); r_dd = Res("dd")
        tmp1 = sb("tmp1", [128, 512]); r_tmp1 = Res("tmp1")
        sq3 = sb("sq3", [128, 512], BF16); r_sq3 = Res("sq3")
        rs3 = sb("rs3", [128, 512]); r_rs3 = Res("rs3")
        cm_sb = sb("cm_sb", [128, 4, 128], BF16); r_cm = Res("cm")
        lp = sb("lp", [64, 4]); lpp = sb("lpp", [64, 2]); ones64 = sb("ones64", [64, 128])
        nlam = sb("nlam", [128, 1]); elam = sb("elam", [128, 2]); gs = sb("gs", [128, 1])
        r_lam = Res("lam")
        fw.dma("sp", cm_sb[:], cmask[:, :, :], writes=[r_cm])
        ident_sb = sb("ident_sb", [128, 128], BF16)
        negm = sb("negm", [128, 4, 128], BF16); r_negm = Res("negm")
        fw.dma("sp", ident_sb[:], ident[:, :], writes=[r_negm])
        fw.op("dve", lambda e: e.tensor_scalar(out=negm[:], in0=cm_sb[:], scalar1=-1.0, scalar2=30000.0,
                                               op0=ALU.add, op1=ALU.mult), reads=[r_cm, r_negm], writes=[r_negm])
        fw.dma("sp", lp[:], lam_qk[:, :], writes=[r_lam])
        fw.dma("sp", gs[:], subln[:, :], writes=[r_lam])
        for i in range(2):
            fw.dma("sp", kt[i][64:68, :], kaug[:, :], writes=[r_kt[i]])
        ps_s = [ps("ps_s%d" % i, [128, 2, 512]) for i in range(2)]
        r_pss3 = [Res("ps_s%d" % i) for i in range(2)]
        po = ps("po", [128, 2, 512]); r_po = Res("po")
        pl_ = ps("pl_", [128, 2, 512]); r_pl = Res("pl")
        fw.op("pool", lambda e: e.memset(ones64[:], 1.0), writes=[r_lam])
        fw.op("dve", lambda e: e.tensor_tensor(out=lpp[:, 0:1], in0=lp[:, 0:1], in1=lp[:, 1:2], op=ALU.mult),
              reads=[r_lam], writes=[r_lam])
        fw.op("dve", lambda e: e.tensor_tensor(out=lpp[:, 1:2], in0=lp[:, 2:3], in1=lp[:, 3:4], op=ALU.mult),
              reads=[r_lam], writes=[r_lam])
        fw.op("pe", lambda e: e.matmul(ps_s[0][:, 0, 0:2], lhsT=ones64[:], rhs=lpp[:], start=True, stop=True),
              inc=True, reads=[r_lam], writes=[r_pss3[0]])
        fw.op("act", lambda e: e.activation(out=elam[:], in_=ps_s[0][:, 0, 0:2], func=AF.Exp),
              reads=[r_pss3[0]], writes=[r_lam])
        fw.op("dve", lambda e: e.tensor_tensor(out=nlam[:], in0=elam[:, 1:2], in1=elam[:, 0:1], op=ALU.subtract),
              reads=[r_lam], writes=[r_lam])
        fw.op("dve", lambda e: e.tensor_scalar(out=nlam[:], in0=nlam[:], scalar1=-LAM_INIT, scalar2=None,
                                               op0=ALU.add), reads=[r_lam], writes=[r_lam])
        fw.op("dve", lambda e: e.tensor_scalar(out=gs[:], in0=gs[:], scalar1=1.0 - LAM_INIT, scalar2=None,
                                               op0=ALU.mult), reads=[r_lam], writes=[r_lam])
        r_attn = Res("attnT")

        def load_k(h):
            for m_ in range(2):
                fw.dma("sp", kt[m_][0:64, :], kt_scr[2 * h + m_, :, :], writes=[r_kt[m_]])

        def load_v(h):
            for q4 in range(4):
                fw.dma("sp", vh[:, q4 * 16:(q4 + 1) * 16, :],
                       v_scr[q4 * 2048:(q4 + 1) * 2048, h * 128:(h + 1) * 128].rearrange("(b p) v -> p b v", p=128),
                       writes=[r_vh])

        steps = []
        for h in range(NH):
            for m in range(4):
                nkb = 16 * m + 16
                for kb in range(nkb):
                    qmin = max(0, (kb - 16 * m) // 4) if kb >= 16 * m else 0
                    steps.append(dict(h=h, m=m, kb=kb, c0=128 * qmin, nkb=nkb, diag=(kb >= 16 * m),
                                      jj=(kb - 16 * m) % 4, i=len(steps)))

        def emit_qk(st):
            h, m, kb, c0 = st["h"], st["m"], st["kb"], st["c0"]
            pss = ps_s[st["i"] % 2]; rps = r_pss3[st["i"] % 2]
            for m_ in range(2):
                fw.op("pe", lambda e, m_=m_: e.matmul(
                    pss[:, m_, c0:512], lhsT=kt[m_][0:KA, kb * 128:(kb + 1) * 128],
                    rhs=qt[0:KA, 2 * h + m_, m * 512 + c0:(m + 1) * 512], start=True, stop=(not st["diag"])),
                    inc=(not st["diag"]), reads=[r_kt[m_], r_qt], writes=[rps])
                if st["diag"]:
                    fw.op("pe", lambda e, m_=m_: e.matmul(
                        pss[:, m_, c0:c0 + 128], lhsT=ident_sb[:], rhs=negm[:, st["jj"], :], start=False, stop=True),
                        inc=True, reads=[r_negm], writes=[rps])

        def emit_exp(st):
            c0 = st["c0"]
            pss = ps_s[st["i"] % 2]; rps = r_pss3[st["i"] % 2]
            ptb = pt[st["i"] % 2]; rptb = r_pt[st["i"] % 2]
            fw.op("act", lambda e: e.activation(out=ptb[:, :, c0:512], in_=pss[:, :, c0:512], func=AF.Exp, scale=0.125),
                  reads=[rps], writes=[rptb])

        def emit_pv(st):
            kb, c0, nkb = st["kb"], st["c0"], st["nkb"]
            ptb = pt[st["i"] % 2]; rptb = r_pt[st["i"] % 2]
            for m_ in range(2):
                fw.op("pe", lambda e, m_=m_: e.matmul(
                    po[:, m_, c0:512], lhsT=vh[:, kb, :], rhs=ptb[:, m_, c0:512],
                    start=(kb == 0), stop=(kb == nkb - 1)), inc=(kb == nkb - 1), reads=[r_vh, rptb], writes=[r_po])
                fw.op("pe", lambda e, m_=m_: e.matmul(
                    pl_[:, m_, c0:512], lhsT=ones_bf[:], rhs=ptb[:, m_, c0:512],
                    start=(kb == 0), stop=(kb == nkb - 1)), inc=(m_ == 1 or kb == nkb - 1), reads=[r_ones, rptb], writes=[r_pl])

        def finalize(h, m, sbuf_i):
            pfin = ps_s[sbuf_i]; rpfin = r_pss3[sbuf_i]
            fw.op("dve", lambda e: e.reciprocal(out=rl[:], in_=pl_[:]), reads=[r_pl], writes=[r_rl])
            fw.op("dve", lambda e: e.tensor_tensor(out=dd[:], in0=po[:, 0, :], in1=rl[:, 0, :], op=ALU.mult),
                  reads=[r_po, r_rl], writes=[r_dd])
            fw.op("dve", lambda e: e.tensor_tensor(out=tmp1[:], in0=po[:, 1, :], in1=rl[:, 1, :], op=ALU.mult),
                  reads=[r_po, r_rl], writes=[r_tmp1])
            fw.op("dve", lambda e: e.scalar_tensor_tensor(out=dd[:], in0=tmp1[:], scalar=nlam[:, 0:1], in1=dd[:],
                                                          op0=ALU.mult, op1=ALU.add),
                  reads=[r_tmp1, r_lam, r_dd], writes=[r_dd])
            fw.op("act", lambda e: e.activation(out=sq3[:], in_=dd[:], func=AF.Square),
                  reads=[r_dd], writes=[r_sq3])
            fw.op("pe", lambda e: e.matmul(pfin[:, 0, :], lhsT=ones_bf[:], rhs=sq3[:], start=True, stop=True),
                  inc=True, reads=[r_ones, r_sq3], writes=[rpfin])
            fw.op("act", lambda e: e.activation(out=rs3[:], in_=pfin[:, 0, :], func=AF.Ln, bias=eps_sb[:, 0:1],
                                                scale=1.0 / 128.0), reads=[rpfin, r_ones], writes=[r_rs3])
            fw.op("act", lambda e: e.activation(out=rs3[:], in_=rs3[:], func=AF.Exp, scale=-0.5),
                  reads=[r_rs3], writes=[r_rs3])
            fw.op("dve", lambda e: e.scalar_tensor_tensor(
                out=attnT[:, h, m * 512:(m + 1) * 512], in0=dd[:], scalar=gs[:, 0:1], in1=rs3[:],
                op0=ALU.mult, op1=ALU.mult), reads=[r_dd, r_lam, r_rs3], writes=[r_attn])

        load_k(0)
        load_v(0)
        emit_qk(steps[0])
        for i, st in enumerate(steps):
            nxt = steps[i + 1] if i + 1 < len(steps) else None
            newh = nxt is not None and nxt["h"] != st["h"]
            if newh:
                load_k(nxt["h"])
            if nxt is not None:
                emit_qk(nxt)
            emit_exp(st)
            emit_pv(st)
            if newh:
                load_v(nxt["h"])
            if st["kb"] == st["nkb"] - 1:
                finalize(st["h"], st["m"], st["i"] % 2)
        fw.barrier()
        es3.close()

        set_free([[8 * KB, 72 * KB], [168 * KB, 172 * KB]])
        es4 = ExitStack()
        cur["es"] = es4
        woa = sb("woa", [128, 4, D], BF16)
        wol = sb("wol", [128, 4, D], BF16)
        r_w4 = Res("w4")
        woa_v = w_o_attn.rearrange("(c p) n -> p c n", p=128)
        wol_v = w_o_lru.rearrange("(c p) n -> p c n", p=128)
        w4stg = [sb("w4stg%d" % i, [128, 4, D]) for i in range(2)]
        r_w4stg = [Res("w4stg%d" % i) for i in range(2)]
        for i_, (wsb, wv_) in enumerate(((woa, woa_v), (wol, wol_v))):
            for c in range(0, 4, 2):
                fw.dma("sp", w4stg[i_][:, c:c + 2, :], wv_[:, c:c + 2, :], writes=[r_w4stg[i_]])
            fw.op("act", lambda e, wsb=wsb, i_=i_: e.activation(out=wsb[:, 0:2, :], in_=w4stg[i_][:, 0:2, :], func=AF.Copy),
                  reads=[r_w4stg[i_]], writes=[r_w4])
            fw.op("dve", lambda e, wsb=wsb, i_=i_: e.tensor_copy(out=wsb[:, 2:4, :], in_=w4stg[i_][:, 2:4, :]),
                  reads=[r_w4stg[i_]], writes=[r_w4])
        thA = sb("thA", [128, TT]); r_thA = Res("thA")
        thL = sb("thL", [128, TT]); r_thL = Res("thL")
        m1 = sb("m1", [128, TT]); r_m1 = Res("m1")
        m2 = sb("m2", [128, TT]); r_m2 = Res("m2")
        r_mg = Res("merged")
        pA = ps("pA", [128, TT]); r_pA = Res("pA")
        pL = ps("pL", [128, TT]); r_pL = Res("pL")
        pGA = ps("pGA", [128, TT]); r_pGA = Res("pGA")
        pGL = ps("pGL", [128, TT]); r_pGL = Res("pGL")
        for f in range(8):
            for m in range(4):
                t0 = m * TT
                for c in range(4):
                    fw.op("pe", lambda e, c=c, f=f, t0=t0: e.matmul(
                        pA[:], lhsT=woa[:, c, f * 128:(f + 1) * 128], rhs=attnT[:, c, t0:t0 + TT],
                        start=(c == 0), stop=(c == 3)), inc=(c == 3), reads=[r_w4, r_attn], writes=[r_pA])
                for c in range(4):
                    fw.op("pe", lambda e, c=c, f=f, t0=t0: e.matmul(
                        pL[:], lhsT=wol[:, c, f * 128:(f + 1) * 128], rhs=lruA[:, c, t0:t0 + TT],
                        start=(c == 0), stop=(c == 3)), inc=(c == 3), reads=[r_w4, r_lruA], writes=[r_pL])
                for c in range(8):
                    fw.op("pe", lambda e, c=c, f=f, t0=t0: e.matmul(
                        pGA[:], lhsT=wgA[:, c, f * 128:(f + 1) * 128], rhs=hn_own[:, c, t0:t0 + TT],
                        start=(c == 0), stop=(c == 7)), inc=(c == 7), reads=[r_wgA, r_hno], writes=[r_pGA])
                for c in range(8):
                    fw.op("pe", lambda e, c=c, f=f, t0=t0: e.matmul(
                        pGL[:], lhsT=wgL[:, c, f * 128:(f + 1) * 128], rhs=hn_own[:, c, t0:t0 + TT],
                        start=(c == 0), stop=(c == 7)), inc=(c == 7), reads=[r_wgA, r_hno], writes=[r_pGL])
                fw.op("act", lambda e: e.activation(out=thA[:], in_=pGA[:], func=AF.Tanh, scale=0.5),
                      reads=[r_pGA], writes=[r_thA])
                fw.op("act", lambda e: e.activation(out=thL[:], in_=pGL[:], func=AF.Tanh, scale=0.5),
                      reads=[r_pGL], writes=[r_thL])
                fw.op("dve", lambda e: e.scalar_tensor_tensor(out=m1[:], in0=thA[:], scalar=1.0, in1=pA[:],
                                                              op0=ALU.add, op1=ALU.mult),
                      reads=[r_thA, r_pA], writes=[r_m1])
                fw.op("dve", lambda e: e.scalar_tensor_tensor(out=m2[:], in0=thL[:], scalar=1.0, in1=pL[:],
                                                              op0=ALU.add, op1=ALU.mult),
                      reads=[r_thL, r_pL], writes=[r_m2])
                fw.op("dve", lambda e, f=f, t0=t0: e.tensor_tensor(out=merged[:, f, t0:t0 + TT], in0=m1[:], in1=m2[:],
                                                                  op=ALU.add), reads=[r_m1, r_m2], writes=[r_mg])
        fw.barrier()
        es4.close()

        set_free([[104 * KB, 136 * KB], [168 * KB, 203 * KB]])
        es5 = ExitStack()
        cur["es"] = es5
        wout = sb("wout", [128, 8, D], BF16); r_wo = Res("wout")
        wout_v = w_out.rearrange("(c p) n -> p c n", p=128)
        x1T = sb("x1T", [128, 8, TT]); r_x1T = Res("x1T")
        for hf in range(2):
            for c in range(4):
                fw.dma("sp", x1T[:, 2 * c:2 * c + 2, :], wout_v[:, 4 * hf + c, :].rearrange("p (a b) -> p a b", a=2),
                       writes=[r_x1T])
            for c in range(4):
                if c % 2 == 0:
                    fw.op("act", lambda e, c=c, hf=hf: e.activation(
                        out=wout[:, 4 * hf + c, :].rearrange("p (a b) -> p a b", a=2), in_=x1T[:, 2 * c:2 * c + 2, :],
                        func=AF.Copy), reads=[r_x1T], writes=[r_wo])
                else:
                    fw.op("dve", lambda e, c=c, hf=hf: e.tensor_copy(
                        out=wout[:, 4 * hf + c, :].rearrange("p (a b) -> p a b", a=2), in_=x1T[:, 2 * c:2 * c + 2, :]),
                        reads=[r_x1T], writes=[r_wo])
        xtok = [sb("xtok%d" % i, [128, D]) for i in range(2)]
        r_xtok = [Res("xtok%d" % i) for i in range(2)]
        xoc = [sb("xoc%d" % i, [128, TT]) for i in range(2)]
        r_xoc = [Res("xoc%d" % i) for i in range(2)]
        g2rep = sb("g2rep", [128, D]); r_g2rep = Res("g2rep")
        fw.dma("sp", g2rep[:], g_ffn_rep[:, :], writes=[r_g2rep])
        wr_f = sb("wr_f", [128, 8, 36]); r_wr = Res("wr")
        g2_sb = sb("g2_sb", [128, 8])
        lgL = [sb("lg%d" % i, [128, 36]) for i in range(4)]; r_lgL = [Res("lg%d" % i) for i in range(4)]
        rtL = [sb("rt%d" % i, [128, 64]) for i in range(4)]; r_rtL = [Res("rt%d" % i) for i in range(4)]
        junk = sb("junk", [128, D], BF16); r_junk = Res("junk")
        fw.dma("sp", g2_sb[:], g_ffn[:, :], writes=[r_wr])
        fw.dma("sp", wr_f[:], w_router.rearrange("(c p) n -> p c n", p=128), writes=[r_wr])
        for c in range(8):
            fw.op("dve", lambda e, c=c: e.tensor_scalar(out=wr_f[:, c, :], in0=wr_f[:, c, :], scalar1=g2_sb[:, c:c + 1],
                                                        scalar2=None, op0=ALU.mult), reads=[r_wr], writes=[r_wr])
        r_x1 = Res("x1")
        r_hn2 = Res("hn2k")
        p_o = [ps("p_o%d" % i, [128, 512]) for i in range(2)]
        r_p_o = [Res("p_o%d" % i) for i in range(2)]
        p_t = [ps("p_t%d" % i, [128, 512]) for i in range(2)]
        r_p_t = [Res("p_t%d" % i) for i in range(2)]
        p_lg = ps("p_lg", [128, 4, 64]); r_p_lg = Res("p_lg")
        xot_v = xot.rearrange("(b p) d -> p b d", p=128)
        ocnt = 0
        for m in range(4):
            t0 = m * TT
            for tb in range(4):
                blk = 4 * m + tb
                xt_ = xtok[blk % 2]; rxt = r_xtok[blk % 2]
                fw.dma("sp", xt_[:], xot_v[:, blk, :], writes=[rxt])
                for half in range(2):
                    po_ = p_o[ocnt % 2]; rpo_ = r_p_o[ocnt % 2]
                    ocnt += 1
                    for f in range(8):
                        fw.op("pe", lambda e, f=f, tb=tb, half=half, po_=po_, t0=t0: e.matmul(
                            po_[:], lhsT=merged[:, f, t0 + tb * 128:t0 + (tb + 1) * 128],
                            rhs=wout[:, f, half * 512:(half + 1) * 512], start=(f == 0), stop=(f == 7)),
                            inc=(f == 7), reads=[r_mg, r_wo], writes=[rpo_])
                    fw.op("dve", lambda e, blk=blk, half=half, po_=po_, xt_=xt_: e.scalar_tensor_tensor(
                        out=x1[:, blk, half * 512:(half + 1) * 512], in0=po_[:], scalar=0.5,
                        in1=xt_[:, half * 512:(half + 1) * 512], op0=ALU.mult, op1=ALU.add),
                        reads=[rpo_, rxt], writes=[r_x1])
            for f2 in range(8):
                pt_ = p_t[f2 % 2]; rpt_ = r_p_t[f2 % 2]
                xc_ = xoc[f2 % 2]; rxc_ = r_xoc[f2 % 2]
                fw.dma("sp", xc_[:], xo_v[:, f2, t0:t0 + TT], writes=[rxc_])
                for f in range(8):
                    fw.op("pe", lambda e, f=f, f2=f2, pt_=pt_, t0=t0: e.matmul(
                        pt_[:], lhsT=wout[:, f, f2 * 128:(f2 + 1) * 128], rhs=merged[:, f, t0:t0 + TT],
                        start=(f == 0), stop=(f == 7)), inc=(f == 7), reads=[r_mg, r_wo], writes=[rpt_])
                fw.op("dve", lambda e, f2=f2, pt_=pt_, xc_=xc_: e.scalar_tensor_tensor(
                    out=x1T[:, f2, :], in0=pt_[:], scalar=0.5, in1=xc_[:], op0=ALU.mult, op1=ALU.add),
                    reads=[rpt_, rxc_], writes=[r_x1T])
            for tb in range(4):
                for c in range(8):
                    fw.op("pe", lambda e, c=c, tb=tb: e.matmul(
                        p_lg[:, tb, 0:36], lhsT=x1T[:, c, tb * 128:(tb + 1) * 128], rhs=wr_f[:, c, :],
                        start=(c == 0), stop=(c == 7)), inc=(c == 7), reads=[r_x1T, r_wr], writes=[r_p_lg])
            def route_block(tb, blk, rt, lg, r_rt, r_lg):
                    fw.op("act", lambda e, blk=blk: e.activation(out=junk[:], in_=x1[:, blk, :], func=AF.Square,
                                                                 accum_out=rt[:, 0:1]),
                          reads=[r_x1], writes=[r_junk, r_rt])
                    fw.op("act", lambda e: e.activation(out=rt[:, 1:2], in_=rt[:, 0:1], func=AF.Ln, bias=eps_sb[:, 0:1],
                                                        scale=1.0 / D), reads=[r_rt, r_ones], writes=[r_rt])
                    fw.op("act", lambda e: e.activation(out=rt[:, 1:2], in_=rt[:, 1:2], func=AF.Exp, scale=-0.5),
                          reads=[r_rt], writes=[r_rt])
                    fw.op("dve", lambda e, tb=tb: e.tensor_scalar(out=lg[:], in0=p_lg[:, tb, 0:36], scalar1=rt[:, 1:2],
                                                                  scalar2=None, op0=ALU.mult),
                          reads=[r_p_lg, r_rt], writes=[r_lg])
                    fw.op("dve", lambda e, blk=blk: e.scalar_tensor_tensor(
                        out=hn2k[:, blk, :], in0=x1[:, blk, :], scalar=rt[:, 1:2], in1=g2rep[:],
                        op0=ALU.mult, op1=ALU.mult), reads=[r_x1, r_rt, r_g2rep], writes=[r_hn2])
                    fw.op("dve", lambda e: e.reduce_max(out=rt[:, 2:3], in_=lg[:, 0:4], axis=mybir.AxisListType.X),
                          reads=[r_lg, r_rt], writes=[r_rt])
                    fw.op("dve", lambda e: e.tensor_scalar(out=rt[:, 3:4], in0=rt[:, 2:3], scalar1=-1.0, scalar2=None,
                                                           op0=ALU.mult), reads=[r_rt], writes=[r_rt])
                    fw.op("act", lambda e: e.activation(out=rt[:, 4:8], in_=lg[:, 0:4], func=AF.Exp, bias=rt[:, 3:4],
                                                        accum_out=rt[:, 8:9]), reads=[r_lg, r_rt], writes=[r_rt])
                    fw.op("dve", lambda e: e.reciprocal(out=rt[:, 9:10], in_=rt[:, 8:9]), reads=[r_rt], writes=[r_rt])
                    fw.op("dve", lambda e: e.tensor_scalar(out=rt[:, 10:14], in0=lg[:, 0:4], scalar1=rt[:, 2:3], scalar2=None,
                                                           op0=ALU.is_ge), reads=[r_lg, r_rt], writes=[r_rt])
                    fw.op("dve", lambda e: e.tensor_scalar(out=rt[:, 16:24], in0=lg[:, 4:12], scalar1=rt[:, 10:11], scalar2=None,
                                                           op0=ALU.mult), reads=[r_lg, r_rt], writes=[r_rt])
                    for g in range(1, 4):
                        fw.op("dve", lambda e, g=g: e.scalar_tensor_tensor(
                            out=rt[:, 16:24], in0=lg[:, 4 + 8 * g:12 + 8 * g], scalar=rt[:, 10 + g:11 + g], in1=rt[:, 16:24],
                            op0=ALU.mult, op1=ALU.add), reads=[r_lg, r_rt], writes=[r_rt])
                    fw.op("dve", lambda e: e.reduce_max(out=rt[:, 24:25], in_=rt[:, 16:24], axis=mybir.AxisListType.X),
                          reads=[r_rt], writes=[r_rt])
                    fw.op("dve", lambda e: e.tensor_scalar(out=rt[:, 32:40], in0=rt[:, 16:24], scalar1=rt[:, 24:25], scalar2=None,
                                                           op0=ALU.is_ge), reads=[r_rt], writes=[r_rt])
                    fw.op("dve", lambda e: e.scalar_tensor_tensor(out=rt[:, 40:48], in0=rt[:, 32:40], scalar=-1e30, in1=rt[:, 16:24],
                                                                  op0=ALU.mult, op1=ALU.add), reads=[r_rt], writes=[r_rt])
                    fw.op("dve", lambda e: e.reduce_max(out=rt[:, 25:26], in_=rt[:, 40:48], axis=mybir.AxisListType.X),
                          reads=[r_rt], writes=[r_rt])
                    fw.op("dve", lambda e: e.tensor_scalar(out=rt[:, 48:56], in0=rt[:, 40:48], scalar1=rt[:, 25:26], scalar2=None,
                                                           op0=ALU.is_ge), reads=[r_rt], writes=[r_rt])
                    fw.op("dve", lambda e: e.tensor_tensor(out=rt[:, 26:27], in0=rt[:, 25:26], in1=rt[:, 24:25], op=ALU.subtract),
                          reads=[r_rt], writes=[r_rt])
                    fw.op("act", lambda e: e.activation(out=rt[:, 27:28], in_=rt[:, 26:27], func=AF.Exp),
                          reads=[r_rt], writes=[r_rt])
                    fw.op("dve", lambda e: e.tensor_scalar(out=rt[:, 28:29], in0=rt[:, 27:28], scalar1=1.0, scalar2=None,
                                                           op0=ALU.add), reads=[r_rt], writes=[r_rt])
                    fw.op("dve", lambda e: e.reciprocal(out=rt[:, 28:29], in_=rt[:, 28:29]), reads=[r_rt], writes=[r_rt])
                    fw.op("dve", lambda e: e.tensor_tensor(out=rt[:, 29:30], in0=rt[:, 27:28], in1=rt[:, 28:29], op=ALU.mult),
                          reads=[r_rt], writes=[r_rt])
                    fw.op("dve", lambda e: e.tensor_tensor(out=rt[:, 28:29], in0=rt[:, 28:29], in1=rt[:, 9:10], op=ALU.mult),
                          reads=[r_rt], writes=[r_rt])
                    fw.op("dve", lambda e: e.tensor_tensor(out=rt[:, 29:30], in0=rt[:, 29:30], in1=rt[:, 9:10], op=ALU.mult),
                          reads=[r_rt], writes=[r_rt])
                    fw.op("dve", lambda e: e.tensor_scalar(out=rt[:, 56:64], in0=rt[:, 32:40], scalar1=rt[:, 28:29], scalar2=None,
                                                           op0=ALU.mult), reads=[r_rt], writes=[r_rt])
                    fw.op("dve", lambda e: e.scalar_tensor_tensor(out=rt[:, 56:64], in0=rt[:, 48:56], scalar=rt[:, 29:30],
                                                                  in1=rt[:, 56:64], op0=ALU.mult, op1=ALU.add),
                          reads=[r_rt], writes=[r_rt])
                    for g in range(4):
                        fw.op("dve", lambda e, g=g, blk=blk: e.tensor_scalar(
                            out=Cw[:, blk, 8 * g:8 * g + 8], in0=rt[:, 56:64], scalar1=rt[:, 10 + g:11 + g], scalar2=None,
                            op0=ALU.mult), reads=[r_rt], writes=[r_C])
                    fw.op("dve", lambda e: e.tensor_tensor(out=rt[:, 40:48], in0=rt[:, 32:40], in1=rt[:, 48:56], op=ALU.add),
                          reads=[r_rt], writes=[r_rt])
                    for g in range(4):
                        fw.op("dve", lambda e, g=g, blk=blk: e.tensor_scalar(
                            out=Mf[:, blk, 8 * g:8 * g + 8], in0=rt[:, 40:48], scalar1=rt[:, 10 + g:11 + g], scalar2=None,
                            op0=ALU.mult), reads=[r_rt], writes=[r_M])
                    fw.op("dve", lambda e, blk=blk: e.tensor_copy(out=Mb[:, blk, :], in_=Mf[:, blk, :]),
                          reads=[r_M], writes=[r_M])
            chains = []
            for tb in range(4):
                fw.defer = []
                route_block(tb, 4 * m + tb, rtL[tb], lgL[tb], r_rtL[tb], r_lgL[tb])
                chains.append(fw.defer)
                fw.defer = None
            for i_ in range(max(len(c_) for c_ in chains)):
                for c_ in chains:
                    if i_ < len(c_):
                        ek_, fn_, rd_, wr_, inc_ = c_[i_]
                        fw.op(ek_, fn_, reads=rd_, writes=wr_, inc=inc_)
        fw.barrier()
        es5.close()

        CAP = 256
        set_free([[104 * KB, 203 * KB]])
        es6 = ExitStack()
        cur["es"] = es6
        wgb = sb("wgb", [128, 8, 512], BF16); wub = sb("wub", [128, 8, 512], BF16); wdb = sb("wdb", [128, 4, D], BF16)
        r_wgb = Res("wgb"); r_wub = Res("wub"); r_wdb = Res("wdb")
        NSTG = 4
        stg = [sb("stg%d" % i, [128, 2, 512]) for i in range(NSTG)]
        r_stg = [Res("stg%d" % i) for i in range(NSTG)]
        iota_s = sb("iota_s", [128, CAP]); iota_t = sb("iota_t", [128, TOWN])
        ustr = sb("ustr", [128, 128], BF16); tv = sb("tv", [128, 16, 8], BF16)
        r_k5 = Res("k5")
        fw.dma("sp", iota_s[:], iota_s_d[:, :], writes=[r_k5])
        fw.dma("sp", iota_t[:], iota_t_d[:, :], writes=[r_k5])
        fw.dma("sp", ustr[:], ustrict_d[:, :], writes=[r_k5])
        fw.dma("sp", tv[:], tvals_d[:, :, :], writes=[r_k5])
        pfx = sb("pfx", [128, 16, 32]); r_pfx = Res("pfx")
        Sel = sb("Sel", [128, 16, CAP], BF16); r_Sel = Res("Sel")
        XgT = sb("XgT", [128, 8, CAP], BF16); r_XgT = Res("XgT")
        HT = sb("HT", [128, 4, CAP], BF16); r_HT = Res("HT")
        SelT = [sb("SelT%d" % i, [128, 2, TOWN], BF16) for i in range(2)]
        r_SelT = [Res("SelT%d" % i) for i in range(2)]
        Yb = [sb("Yb%d" % i, [128, 2, D], BF16) for i in range(2)]
        r_Yb = [Res("Yb%d" % i) for i in range(2)]
        tokf = sb("tokf", [128, 2]); r_tokf = Res("tokf")
        tokc = sb("tokc", [128, 2, 2]); r_tokc = Res("tokc")
        s1 = [sb("s1_%d" % i, [128, CAP]) for i in range(2)]
        r_s1 = [Res("s1_%d" % i) for i in range(2)]
        th5 = [sb("th5_%d" % i, [128, CAP]) for i in range(2)]
        r_th5 = [Res("th5_%d" % i) for i in range(2)]

        pA_ = [ps("pA5_%d" % i, [128, 512]) for i in range(2)]
        r_pA_ = [Res("pA5_%d" % i) for i in range(2)]
        pGU = [ps("pGU%d" % i, [128, 512]) for i in range(2)]
        r_pGU = [Res("pGU%d" % i) for i in range(2)]
        pTok = ps("pTok", [128, 16, 32]); r_pTok = Res("pTok")
        pC = [ps("pC%d" % i, [128, 512]) for i in range(3)]
        r_pC = [Res("pC%d" % i) for i in range(3)]

        for blk in range(16):
            for b2 in range(blk + 1):
                fw.op("pe", lambda e, blk=blk, b2=b2: e.matmul(
                    pTok[:, blk, :], lhsT=(ustr[:] if b2 == blk else ones_bf[:]), rhs=Mb[:, b2, :],
                    start=(b2 == 0), stop=(b2 == blk)), inc=(b2 == blk), reads=[r_M, r_k5, r_ones], writes=[r_pTok])
        fw.op("dve", lambda e: e.tensor_copy(out=pfx[:], in_=pTok[:]), reads=[r_pTok], writes=[r_pfx])

        cnt5 = {"stg": 0, "gu": 0, "c": 0}

        wq5 = {"todo": [], "issued": []}

        def w_begin(ex_):
            wg_v = w_gate[ex_].rearrange("(c p) n -> p c n", p=128)
            wu_v = w_up[ex_].rearrange("(c p) n -> p c n", p=128)
            wd_v = w_down[ex_].rearrange("(c p) n -> p c n", p=128)
            pcs = []
            for c in range(0, 8, 2):
                pcs.append((wg_v[:, c:c + 2, :], wgb[:, c:c + 2, :], r_wgb))
            for c in range(0, 8, 2):
                pcs.append((wu_v[:, c:c + 2, :], wub[:, c:c + 2, :], r_wub))
            for half in range(2):
                for f in range(0, 4, 2):
                    pcs.append((wd_v[:, f:f + 2, half * 512:(half + 1) * 512],
                                wdb[:, f:f + 2, half * 512:(half + 1) * 512], r_wdb))
            assert not wq5["todo"] and not wq5["issued"]
            wq5["todo"] = pcs

        def w_issue():
            if not wq5["todo"]:
                return
            src, dst, rdst = wq5["todo"].pop(0)
            sg = stg[cnt5["stg"] % NSTG]; rsg = r_stg[cnt5["stg"] % NSTG]
            cnt5["stg"] += 1
            fw.dma("sp", sg[:], src, writes=[rsg])
            wq5["issued"].append((sg, rsg, dst, rdst))

        def w_cast():
            if not wq5["issued"]:
                return
            sg, rsg, dst, rdst = wq5["issued"].pop(0)
            fw.op("act", lambda e: e.activation(out=dst, in_=sg[:], func=AF.Copy), reads=[rsg], writes=[rdst])

        def stage_a(ex_):
            sT = SelT[ex_ % 2]; rsT = r_SelT[ex_ % 2]
            yb = Yb[ex_ % 2]; ryb = r_Yb[ex_ % 2]
            for blk in range(16):
                fw.op("dve", lambda e, blk=blk: e.tensor_scalar(
                    out=Sel[:, blk, :], in0=iota_s[:], scalar1=pfx[:, blk, ex_:ex_ + 1], scalar2=Mf[:, blk, ex_:ex_ + 1],
                    op0=ALU.is_equal, op1=ALU.mult), reads=[r_k5, r_pfx, r_M], writes=[r_Sel])
            yield
            for s_ in range(2):
                for blk in range(16):
                    fw.op("pe", lambda e, s_=s_, blk=blk: e.matmul(
                        pTok[:, s_, 0:8], lhsT=Sel[:, blk, s_ * 128:(s_ + 1) * 128], rhs=tv[:, blk, :],
                        start=(blk == 0), stop=(blk == 15)), inc=(blk == 15), reads=[r_Sel, r_k5], writes=[r_pTok])
            fw.op("act", lambda e: e.activation(out=tokc[:], in_=pTok[:, 0:2, 0:2], func=AF.Copy),
                  reads=[r_pTok], writes=[r_tokc])
            for s_ in range(2):
                fw.op("dve", lambda e, s_=s_: e.scalar_tensor_tensor(
                    out=tokf[:, s_:s_ + 1], in0=tokc[:, s_, 0:1], scalar=64.0, in1=tokc[:, s_, 1:2],
                    op0=ALU.mult, op1=ALU.add), reads=[r_tokc], writes=[r_tokf])
            for s_ in range(2):
                fw.op("dve", lambda e, s_=s_: e.tensor_scalar(
                    out=sT[:, s_, :], in0=iota_t[:], scalar1=tokf[:, s_:s_ + 1], scalar2=None, op0=ALU.is_equal),
                    reads=[r_k5, r_tokf], writes=[rsT])
            yield
            for half in range(2):
                for c2 in range(4):
                    c = 4 * half + c2
                    pa = pA_[c2 // 2]; rpa = r_pA_[c2 // 2]
                    for blk in range(16):
                        fw.op("pe", lambda e, c=c, c2=c2, blk=blk, pa=pa: e.matmul(
                            pa[:, (c2 % 2) * CAP:(c2 % 2 + 1) * CAP], lhsT=hn2k[:, blk, c * 128:(c + 1) * 128], rhs=Sel[:, blk, :],
                            start=(blk == 0), stop=(blk == 15)), inc=(blk == 15), reads=[r_hn2, r_Sel], writes=[rpa])
                    fw.op("act", lambda e, c=c, c2=c2, pa=pa: e.activation(
                        out=XgT[:, c, :], in_=pa[:, (c2 % 2) * CAP:(c2 % 2 + 1) * CAP], func=AF.Copy),
                        reads=[rpa], writes=[r_XgT])
                    yield
            for f in range(4):
                b_ = cnt5["gu"] % 2
                cnt5["gu"] += 1
                for c in range(8):
                    fw.op("pe", lambda e, c=c, f=f, b_=b_: e.matmul(
                        pGU[b_][:, 0:CAP], lhsT=wgb[:, c, f * 128:(f + 1) * 128], rhs=XgT[:, c, :],
                        start=(c == 0), stop=(c == 7)), inc=(c == 7), reads=[r_wgb, r_XgT], writes=[r_pGU[b_]])
                for c in range(8):
                    fw.op("pe", lambda e, c=c, f=f, b_=b_: e.matmul(
                        pGU[b_][:, CAP:2 * CAP], lhsT=wub[:, c, f * 128:(f + 1) * 128], rhs=XgT[:, c, :],
                        start=(c == 0), stop=(c == 7)), inc=(c == 7), reads=[r_wub, r_XgT], writes=[r_pGU[b_]])
                fw.op("act", lambda e, b_=b_: e.activation(out=th5[b_][:], in_=pGU[b_][:, 0:CAP], func=AF.Tanh, scale=0.5),
                      reads=[r_pGU[b_]], writes=[r_th5[b_]])
                fw.op("dve", lambda e, b_=b_: e.scalar_tensor_tensor(out=s1[b_][:], in0=th5[b_][:], scalar=1.0,
                                                                    in1=pGU[b_][:, 0:CAP], op0=ALU.add, op1=ALU.mult),
                      reads=[r_th5[b_], r_pGU[b_]], writes=[r_s1[b_]])
                fw.op("dve", lambda e, b_=b_, f=f: e.scalar_tensor_tensor(
                    out=HT[:, f, :], in0=s1[b_][:], scalar=0.5, in1=pGU[b_][:, CAP:2 * CAP], op0=ALU.mult, op1=ALU.mult),
                    reads=[r_s1[b_], r_pGU[b_]], writes=[r_HT])
                yield
            for s_ in range(2):
                for half in range(2):
                    pa = pA_[half]; rpa = r_pA_[half]
                    for f in range(4):
                        fw.op("pe", lambda e, f=f, s_=s_, half=half, pa=pa: e.matmul(
                            pa[:], lhsT=HT[:, f, s_ * 128:(s_ + 1) * 128], rhs=wdb[:, f, half * 512:(half + 1) * 512],
                            start=(f == 0), stop=(f == 3)), inc=(f == 3), reads=[r_HT, r_wdb], writes=[rpa])
                    fw.op("act", lambda e, s_=s_, half=half, pa=pa: e.activation(
                        out=yb[:, s_, half * 512:(half + 1) * 512], in_=pa[:], func=AF.Copy),
                        reads=[rpa], writes=[ryb])
                    yield

        def stage_b_units(ex_):
            sT = SelT[ex_ % 2]; rsT = r_SelT[ex_ % 2]
            yb = Yb[ex_ % 2]; ryb = r_Yb[ex_ % 2]
            units = []
            for blk in range(16):
                for half in range(2):
                    def unit(blk=blk, half=half):
                        ci = cnt5["c"] % 3
                        cnt5["c"] += 1
                        for s_ in range(2):
                            fw.op("pe", lambda e, s_=s_: e.matmul(
                                pC[ci][:], lhsT=sT[:, s_, blk * 128:(blk + 1) * 128],
                                rhs=yb[:, s_, half * 512:(half + 1) * 512], start=(s_ == 0), stop=(s_ == 1)),
                                inc=(s_ == 1), reads=[rsT, ryb], writes=[r_pC[ci]])
                        fw.op("dve", lambda e: e.scalar_tensor_tensor(
                            out=x1[:, blk, half * 512:(half + 1) * 512], in0=pC[ci][:], scalar=Cw[:, blk, ex_:ex_ + 1],
                            in1=x1[:, blk, half * 512:(half + 1) * 512], op0=ALU.mult, op1=ALU.add),
                            reads=[r_pC[ci], r_C, r_x1], writes=[r_x1])
                    units.append(unit)
            return units

        w_begin(0)
        for _ in range(3):
            w_issue()
        for _ in stage_a(0):
            w_cast()
            w_issue()
        for ex_ in range(32):
            units = stage_b_units(ex_)
            if ex_ + 1 < 32:
                w_begin(ex_ + 1)
                for _ in range(3):
                    w_issue()
                ui = 0
                for _ in stage_a(ex_ + 1):
                    w_cast()
                    w_issue()
                    for _k in range(2):
                        if ui < len(units):
                            units[ui]()
                            ui += 1
                while wq5["todo"] or wq5["issued"]:
                    w_cast()
                    w_issue()
                while ui < len(units):
                    units[ui]()
                    ui += 1
            else:
                for u in units:
                    u()
        fw.barrier()
        es6.close()

        set_free([[104 * KB, 203 * KB]])
        es7 = ExitStack()
        cur["es"] = es7
        gF = sb("gF", [128, D]); r_gF = Res("gF")
        ob = [sb("ob%d" % i, [128, D]) for i in range(2)]
        r_ob = [Res("ob%d" % i) for i in range(2)]
        fin = sb("fin", [128, 4]); r_fin = Res("fin")
        fw.dma("sp", gF[:], g_fin[:, :], writes=[r_gF])
        out_v = out_d.rearrange("(b p) d -> p b d", p=128)
        for blk in range(16):
            o_ = ob[blk % 2]; ro_ = r_ob[blk % 2]
            fw.op("act", lambda e, blk=blk, o_=o_: e.activation(out=o_[:], in_=x1[:, blk, :], func=AF.Square,
                                                               accum_out=fin[:, 0:1]),
                  reads=[r_x1], writes=[ro_, r_fin])
            fw.op("act", lambda e: e.activation(out=fin[:, 1:2], in_=fin[:, 0:1], func=AF.Ln, bias=eps_sb[:, 0:1],
                                                scale=1.0 / D), reads=[r_fin, r_ones], writes=[r_fin])
            fw.op("act", lambda e: e.activation(out=fin[:, 1:2], in_=fin[:, 1:2], func=AF.Exp, scale=-0.5),
                  reads=[r_fin], writes=[r_fin])
            fw.op("dve", lambda e, blk=blk, o_=o_: e.scalar_tensor_tensor(
                out=o_[:], in0=x1[:, blk, :], scalar=fin[:, 1:2], in1=gF[:], op0=ALU.mult, op1=ALU.mult),
                reads=[r_x1, r_fin, r_gF], writes=[ro_])
            fw.dma("sp", out_v[:, blk, :], o_[:], reads=[ro_])
        fw.barrier()
        fw.final_wait("sp")
        es7.close()
    return nc


def _bf(a):
    return np.asarray(a, dtype=np.float32).astype(ml_dtypes.bfloat16)


def make_in_maps(inputs):
    x = np.asarray(inputs["x"], dtype=np.float32)
    f = lambda k: np.asarray(inputs[k], dtype=np.float32)
    pc = lambda v: np.ascontiguousarray(v.reshape(-1, 128).T)
    g_mix = pc(f("norm_mix_g")[0])
    w_in = np.ascontiguousarray(f("w_in")[0])
    conv_w = np.ascontiguousarray(f("conv_w")[0].reshape(4, 4, 128).transpose(2, 1, 0))
    conv_b = pc(f("conv_b")[0])

    def bd(w):
        o = np.zeros((128, 4, 128), np.float32)
        for n in range(8):
            g, hlf = n // 2, n % 2
            o[hlf * 64:(hlf + 1) * 64, g, hlf * 64:(hlf + 1) * 64] = w[n]
        return o
    wr_bd = bd(f("w_r")[0])
    wi_bd = bd(f("w_i")[0])
    tk = np.arange(S)
    kaug = _bf(np.stack([tk // 64, tk % 64, np.ones(S), np.ones(S)]).astype(np.float32))
    w_o_attn = np.ascontiguousarray(f("w_o_attn")[0])
    w_o_lru = np.ascontiguousarray(f("w_o_lru")[0])
    w_out_ = np.ascontiguousarray(f("w_out")[0])
    g_ffn = pc(f("norm_ffn_g")[0])
    w_router = np.ascontiguousarray(np.concatenate(
        [f("w_group")[0], f("w_expert_router")[0].transpose(1, 0, 2).reshape(D, 32)], axis=1))
    w_gate_ = np.ascontiguousarray(f("w_gate")[0])
    w_up_ = np.ascontiguousarray(f("w_up")[0])
    w_down_ = np.ascontiguousarray(f("w_down")[0])
    g_fin = np.ascontiguousarray(np.tile(f("final_norm_g")[None, :], (128, 1)))
    g_ffn_rep = np.ascontiguousarray(np.tile(f("norm_ffn_g")[0][None, :], (128, 1)))
    iota_s = np.ascontiguousarray(np.tile(np.arange(256, dtype=np.float32)[None, :], (128, 1)))
    iota_t = np.ascontiguousarray(np.tile(np.arange(1, TOWN + 1, dtype=np.float32)[None, :], (128, 1)))
    ustrict = _bf((np.arange(128)[:, None] < np.arange(128)[None, :]).astype(np.float32))
    code = (np.arange(16)[None, :] * 128 + np.arange(128)[:, None] + 1)
    tvals = _bf(np.stack([code // 64, code % 64] + [np.zeros_like(code)] * 6, axis=-1).astype(np.float32))
    maps = []
    for c in range(NCORES):
        b, j = c // 4, c % 4
        xn = np.ascontiguousarray(x[b].T)
        own_blocks = [4 * i + j for i in range(16)]
        tq = np.concatenate([np.arange(bl * 128, (bl + 1) * 128) for bl in own_blocks])
        xo = np.ascontiguousarray(x[b][tq].T)
        qa = np.zeros((NH, 4, TOWN), np.float32)
        for h in range(NH):
            s8 = SLOPES[h] * 8.0
            qa[h, 0] = 64.0 * s8
            qa[h, 1] = s8
            qa[h, 2] = -64.0 * s8 * (tq // 64)
            qa[h, 3] = -s8 * (tq % 64)
        cm = np.zeros((128, 4, 128), np.float32)
        for jj in range(4):
            if jj < j:
                cm[:, jj, :] = 1.0
            elif jj == j:
                cm[:, jj, :] = (np.arange(128)[:, None] <= np.arange(128)[None, :])
        sj = np.zeros((128, 4), np.float32)
        sj[:, j] = 1.0
        extra = {
            "xot": np.ascontiguousarray(x[b][tq]),
            "lam_qk": np.ascontiguousarray(f("lambda_qk")[0].reshape(4, 64).T),
            "subln": np.ascontiguousarray(f("subln_g")[0].reshape(128, 1)),
            "w_o_attn": w_o_attn, "w_o_lru": w_o_lru, "w_out": w_out_, "g_ffn": g_ffn,
            "w_router": w_router, "w_gate": w_gate_, "w_up": w_up_, "w_down": w_down_, "g_fin": g_fin, "g_ffn_rep": g_ffn_rep,
            "iota_s": iota_s, "iota_t": iota_t, "ustrict": ustrict, "tvals": tvals,
        }
        maps.append({
            "xn": xn, "xo": xo, "g_mix": g_mix, "w_in": w_in, "kaug": kaug, "qaug": _bf(qa),
            "cmask": _bf(cm), "ident": _bf(np.eye(128, dtype=np.float32)), "selj": sj, "conv_w": conv_w, "conv_b": conv_b,
            "wr_bd": wr_bd, "wi_bd": wi_bd, "b_r": pc(f("b_r")[0]), "b_i": pc(f("b_i")[0]),
            "lru_lam": pc(f("lru_lambda")[0]), **extra,
        })
    return maps


def kernel(**inputs):
    nc = build()
    in_maps = make_in_maps(inputs)
    res = run_bass_kernel_spmd(nc, in_maps, core_ids=list(range(NCORES)))
    out = np.zeros((NB, S, D), np.float32)
    for c in range(NCORES):
        b, j = c // 4, c % 4
        o = np.asarray(res.results[c]["out"])
        for i in range(16):
            bl = 4 * i + j
            out[b, bl * 128:(bl + 1) * 128, :] = o[i * 128:(i + 1) * 128, :]
    return out
```

```python
import numpy as np
import ml_dtypes
from contextlib import ExitStack
import concourse.bass as bass
import concourse.mybir as mybir
from concourse.bass_utils import run_bass_kernel_spmd

F32 = mybir.dt.float32
BF16 = mybir.dt.bfloat16
I32 = mybir.dt.int32
AF = mybir.ActivationFunctionType
ALU = mybir.AluOpType

D = 1024
S = 8192
NB = 2
NH = 4
HD = 64
TOWN = 2048
NCORES = 8
EPS = 1e-6
EPOCH = 12000
KA = 68
SLOPES = [2.0 ** (-8.0 * (h + 1) / NH) for h in range(NH)]
LAM_INIT = 0.8 - 0.6 * 1.0
GELU_K = 0.7978845608028654


class Res:
    __slots__ = ("name", "w", "r")

    def __init__(self, name):
        self.name = name
        self.w = None
        self.r = {}


class EngState:
    def __init__(self, key, eng):
        self.key = key
        self.eng = eng
        self.count = 0
        self.sems = []
        self.known = {}


class FW:
    def __init__(self, nc, es):
        self.nc = nc
        self.es = es
        self.engs = {}
        for key, eng in (("pe", nc.tensor), ("act", nc.scalar), ("dve", nc.vector),
                         ("pool", nc.gpsimd), ("sp", nc.sync)):
            self.engs[key] = EngState(key, eng)
        self.NPOOL = 12
        self.dma_pool = {q: [es.enter_context(nc.semaphore("dq_%s%d" % (q, i))) for i in range(self.NPOOL)]
                         for q in ("sp", "pool")}
        self.dma_pool_uses = {q: [0] * self.NPOOL for q in ("sp", "pool")}
        self.dma_next = {"sp": 0, "pool": 0}
        self.dma_tokens = {}
        self.n_dma = 0

    def _sem_for(self, st, idx):
        e = idx // EPOCH
        while len(st.sems) <= e:
            st.sems.append(self.es.enter_context(
                self.nc.semaphore("e_%s_%d" % (st.key, len(st.sems)))))
        return st.sems[e], idx % EPOCH + 1

    def _wait(self, st, dep):
        key, idx = dep
        if key == "dma":
            if st.known.get(dep, False):
                return
            sem, val = self.dma_tokens[idx]
            st.eng.wait_ge(sem, val)
            st.known[dep] = True
            return
        if key == st.key and key == "pe":
            return
        if st.known.get(key, -1) >= idx:
            return
        sem, val = self._sem_for(self.engs[key], idx)
        st.eng.wait_ge(sem, val)
        st.known[key] = idx

    def _collect(self, st, reads, writes):
        deps = {}
        dma_deps = []

        def add(d):
            if d is None:
                return
            if d[0] == "dma":
                dma_deps.append(d)
            elif deps.get(d[0], -1) < d[1]:
                deps[d[0]] = d[1]
        for r in reads:
            add(r.w)
        for w in writes:
            add(w.w)
            for k, i in w.r.items():
                if k == "dma":
                    for tok in i:
                        add(("dma", tok))
                else:
                    add((k, i))
        for d in dma_deps:
            self._wait(st, d)
        for k, i in deps.items():
            self._wait(st, (k, i))

    def op(self, engkey, fn, reads=(), writes=(), inc=True):
        if getattr(self, "defer", None) is not None:
            self.defer.append((engkey, fn, list(reads), list(writes), inc))
            return None
        st = self.engs[engkey]
        self._collect(st, reads, writes)
        ins = fn(st.eng)
        idx = st.count
        if inc:
            sem, _ = self._sem_for(st, idx)
            ins.then_inc(sem, 1)
            st.count += 1
        for r in reads:
            r.r[engkey] = idx
        for w in writes:
            w.w = (engkey, idx)
            w.r = {}
        return ins

    def dma(self, engkey, out, in_, reads=(), writes=(), **kw):
        st = self.engs[engkey]
        self._collect(st, reads, writes)
        slot = self.dma_next[engkey]
        self.dma_next[engkey] = (slot + 1) % self.NPOOL
        sem = self.dma_pool[engkey][slot]
        prev = self.dma_pool_uses[engkey][slot]
        if prev > 0:
            st.eng.wait_ge(sem, 16 * prev)
        ins = st.eng.dma_start(out=out, in_=in_, **kw)
        ins.then_inc(sem, 16)
        self.dma_pool_uses[engkey][slot] = prev + 1
        tok = self.n_dma
        self.n_dma += 1
        self.dma_tokens[tok] = (sem, 16 * (prev + 1))
        for r in reads:
            r.r.setdefault("dma", []).append(tok)
        for w in writes:
            w.w = ("dma", tok)
            w.r = {}
        return tok

    def barrier(self):
        for st in self.engs.values():
            for k2, st2 in self.engs.items():
                if st2.count > 0 and k2 != st.key:
                    self._wait(st, (k2, st2.count - 1))
            for q in ("sp", "pool"):
                for slot in range(self.NPOOL):
                    u = self.dma_pool_uses[q][slot]
                    if u > 0:
                        st.eng.wait_ge(self.dma_pool[q][slot], 16 * u)

    def final_wait(self, engkey="sp"):
        st = self.engs[engkey]
        for q in ("sp", "pool"):
            for slot in range(self.NPOOL):
                u = self.dma_pool_uses[q][slot]
                if u > 0:
                    st.eng.wait_ge(self.dma_pool[q][slot], 16 * u)


def build(stage=99, debug=False):
    nc = bass.Bass("TRN2", target_bir_lowering=False)
    es = ExitStack()

    def din(name, shape, dt=F32):
        return nc.dram_tensor(name, list(shape), dt, kind="ExternalInput").ap()

    def dout(name, shape, dt=F32):
        return nc.dram_tensor(name, list(shape), dt, kind="ExternalOutput").ap()

    def dscr(name, shape, dt):
        return nc.dram_tensor(name, list(shape), dt, kind="Internal").ap()

    xn = din("xn", [D, S])
    xo = din("xo", [D, TOWN])
    g_mix = din("g_mix", [128, 8])
    w_in = din("w_in", [D, 4608])
    kaug = din("kaug", [4, S], BF16)
    qaug = din("qaug", [NH, 4, TOWN], BF16)
    cmask = din("cmask", [128, 4, 128], BF16)
    ident = din("ident", [128, 128], BF16)
    selj = din("selj", [128, 4])
    conv_w = din("conv_w", [128, 4, 4])
    conv_b = din("conv_b", [128, 4])
    wr_bd = din("wr_bd", [128, 4, 128])
    wi_bd = din("wi_bd", [128, 4, 128])
    b_r = din("b_r", [128, 4])
    b_i = din("b_i", [128, 4])
    lru_lam = din("lru_lam", [128, 4])
    xot = din("xot", [TOWN, D])
    lam_qk = din("lam_qk", [64, 4])
    subln = din("subln", [128, 1])
    w_o_attn = din("w_o_attn", [512, D])
    w_o_lru = din("w_o_lru", [512, D])
    w_out = din("w_out", [D, D])
    g_ffn = din("g_ffn", [128, 8])
    w_router = din("w_router", [D, 36])
    w_gate = din("w_gate", [32, D, 512])
    w_up = din("w_up", [32, D, 512])
    w_down = din("w_down", [32, 512, D])
    g_fin = din("g_fin", [128, D])
    g_ffn_rep = din("g_ffn_rep", [128, D])
    iota_s_d = din("iota_s", [128, 256])
    iota_t_d = din("iota_t", [128, TOWN])
    ustrict_d = din("ustrict", [128, 128], BF16)
    tvals_d = din("tvals", [128, 16, 8], BF16)

    dbg = {}
    if debug:
        dbg["kt"] = dout("dbg_kt", [8, 64, S], BF16)
        dbg["v"] = dout("dbg_v", [S, 512], BF16)
        dbg["lru"] = dout("dbg_lru", [128, 4, TOWN])
    out_d = dout("out", [TOWN, D])

    kt_scr = dbg["kt"] if debug else dscr("kt_scr", [8, 64, S], BF16)
    v_scr = dbg["v"] if debug else dscr("v_scr", [S, 512], BF16)

    with es:
        fw = FW(nc, es)

        KB = 1024
        BASE = 17 * KB
        DTB = {F32: 4, BF16: 2, I32: 4}
        cur = {"iv": [[0, 8 * KB]], "es": es}

        def set_free(intervals):
            cur["iv"] = [list(x) for x in intervals]

        def sb(name, shape, dt=F32):
            n = DTB[dt]
            for d_ in shape[1:]:
                n *= d_
            n = (n + 63) // 64 * 64
            for iv in cur["iv"]:
                if iv[1] - iv[0] >= n:
                    off = iv[0]
                    iv[0] += n
                    return nc.alloc_sbuf_tensor_at(name, list(shape), dt, offset=off + BASE)
            raise RuntimeError("SBUF arena full for %s (%d bytes) free=%s" % (name, n, cur["iv"]))

        def sb_at(name, shape, dt, off):
            return nc.alloc_sbuf_tensor_at(name, list(shape), dt, offset=off + BASE)

        def ps(name, shape, dt=F32):
            return cur["es"].enter_context(nc.psum_tensor(name, list(shape), dt))

        ones_bf = sb("ones_bf", [128, 128], BF16)
        r_ones = Res("ones")
        fw.op("pool", lambda e: e.memset(ones_bf[:], 1.0), writes=[r_ones])
        eps_sb = sb("eps_sb", [128, 1])
        one_sb = sb("one_sb", [128, 1])
        fw.op("pool", lambda e: e.memset(eps_sb[:], EPS), writes=[r_ones])
        fw.op("pool", lambda e: e.memset(one_sb[:], 1.0), writes=[r_ones])
        g_sb = sb("g_sb", [128, 8])
        r_g = Res("g")
        fw.dma("sp", g_sb[:], g_mix[:, :], writes=[r_g])
        Cw = sb("Cw", [128, 16, 32]); r_C = Res("C")
        selj_sb = sb("selj_sb", [128, 4])
        cw_sb = sb("cw_sb", [128, 4, 4])
        cb_sb = sb("cb_sb", [128, 4])
        br_sb = sb("br_sb", [128, 4])
        bi_sb = sb("bi_sb", [128, 4])
        lam_sb = sb("lam_sb", [128, 4])
        r_small = Res("small")
        for t_sb, t_d in ((selj_sb, selj), (cb_sb, conv_b), (br_sb, b_r), (bi_sb, b_i),
                          (lam_sb, lru_lam)):
            fw.dma("sp", t_sb[:], t_d[:, :], writes=[r_small])
        fw.dma("sp", cw_sb[:], conv_w[:, :, :], writes=[r_small])
        wr_sb = sb("wr_sb", [128, 4, 128], BF16)
        wi_sb = sb("wi_sb", [128, 4, 128], BF16)
        r_wgate = Res("wgate")
        fw.dma("pool", wr_sb[:], wr_bd[:, :, :], writes=[r_wgate])
        fw.dma("pool", wi_sb[:], wi_bd[:, :, :], writes=[r_wgate])

        ex = sb("ex", [128, 4])
        pl = sb("pl", [128, 4])
        hc = sb("hc", [128, 4])
        cc = sb("cc", [128, 4])
        hbr = sb("hbr", [128, 4])
        hbi = sb("hbi", [128, 4])
        r_const = Res("lruconst")
        fw.op("act", lambda e: e.activation(out=ex[:], in_=lam_sb[:], func=AF.Exp, scale=-1.0),
              reads=[r_small], writes=[r_const])
        fw.op("dve", lambda e: e.tensor_scalar(out=pl[:], in0=ex[:], scalar1=-0.25, scalar2=1.0 / 3.0,
                                               op0=ALU.mult, op1=ALU.add), reads=[r_const], writes=[r_const])
        fw.op("dve", lambda e: e.tensor_tensor(out=pl[:], in0=pl[:], in1=ex[:], op=ALU.mult),
              reads=[r_const], writes=[r_const])
        fw.op("dve", lambda e: e.tensor_scalar(out=pl[:], in0=pl[:], scalar1=-0.5, scalar2=None,
                                               op0=ALU.add), reads=[r_const], writes=[r_const])
        fw.op("dve", lambda e: e.tensor_tensor(out=pl[:], in0=pl[:], in1=ex[:], op=ALU.mult),
              reads=[r_const], writes=[r_const])
        fw.op("dve", lambda e: e.tensor_scalar(out=pl[:], in0=pl[:], scalar1=1.0, scalar2=None,
                                               op0=ALU.add), reads=[r_const], writes=[r_const])
        fw.op("dve", lambda e: e.tensor_tensor(out=pl[:], in0=pl[:], in1=ex[:], op=ALU.mult),
              reads=[r_const], writes=[r_const])
        fw.op("dve", lambda e: e.tensor_scalar(out=cc[:], in0=pl[:], scalar1=-8.0, scalar2=None,
                                               op0=ALU.mult), reads=[r_const], writes=[r_const])
        fw.op("dve", lambda e: e.tensor_scalar(out=hc[:], in0=pl[:], scalar1=-4.0, scalar2=None,
                                               op0=ALU.mult), reads=[r_const], writes=[r_const])
        fw.op("dve", lambda e: e.tensor_scalar(out=hbr[:], in0=br_sb[:], scalar1=0.5, scalar2=None,
                                               op0=ALU.mult), reads=[r_small], writes=[r_const])
        fw.op("dve", lambda e: e.tensor_scalar(out=hbi[:], in0=bi_sb[:], scalar1=0.5, scalar2=None,
                                               op0=ALU.mult), reads=[r_small], writes=[r_const])

        lru_own = sb_at("lru_own", [128, 4, TOWN], F32, 8 * KB)
        r_lru = Res("lru_own")
        qt = sb_at("qt", [128, 8, TOWN], BF16, 40 * KB)
        hn_own = sb_at("hn_own", [128, 8, TOWN], BF16, 72 * KB)
        lruA = sb_at("lruA", [128, 4, TOWN], BF16, 104 * KB)
        attnT = sb_at("attnT", [128, 4, TOWN], BF16, 120 * KB)
        merged = sb_at("merged", [128, 8, TOWN], BF16, 136 * KB)
        x1 = sb_at("x1", [128, 16, D], F32, 8 * KB)
        hn2k = sb_at("hn2k", [128, 16, D], BF16, 72 * KB)
        Mf = sb_at("Mf", [128, 16, 32], F32, 203 * KB)
        Mb = sb_at("Mb", [128, 16, 32], BF16, 205 * KB)
        r_M = Res("M")
        TOP = 207 * KB
        set_free([[40 * KB, TOP]])
        es1 = ExitStack()
        cur["es"] = es1
        wk_sb = sb("wk_sb", [128, 8, 512], BF16)
        wv_sb = sb("wv_sb", [128, 8, 512], BF16)
        wx_sb = sb("wx_sb", [128, 8, 512], BF16)
        r_w1 = Res("w1")
        w_in_v = w_in.rearrange("(c p) n -> p c n", p=128)

        TT = 512
        NT = S // TT
        xin = [sb("xin%d" % i, [128, 8, TT]) for i in range(2)]
        r_xin = [Res("xin%d" % i) for i in range(2)]
        for i_, (wsb, c0) in enumerate(((wk_sb, 512), (wv_sb, 1024), (wx_sb, 1536))):
            xb_ = xin[i_ % 2]; rxb_ = r_xin[i_ % 2]
            for c in range(0, 8, 4):
                fw.dma("sp", xb_[:, c:c + 4, :], w_in_v[:, c:c + 4, c0:c0 + 512], writes=[rxb_])
            fw.op("act", lambda e, wsb=wsb, xb_=xb_: e.activation(out=wsb[:, 0:4, :], in_=xb_[:, 0:4, :], func=AF.Copy),
                  reads=[rxb_], writes=[r_w1])
            fw.op("dve", lambda e, wsb=wsb, xb_=xb_: e.tensor_copy(out=wsb[:, 4:8, :], in_=xb_[:, 4:8, :]),
                  reads=[rxb_], writes=[r_w1])
        xsq = sb("xsq", [128, 8, TT], BF16)
        r_xsq = Res("xsq")
        rb = sb("rb", [128, TT])
        r_rb = Res("rb")
        hn = [sb("hn%d" % i, [128, 8, TT], BF16) for i in range(2)]
        r_hn = [Res("hn%d" % i) for i in range(2)]
        kst = [sb("kst%d" % i, [64, 8, TT], BF16) for i in range(2)]
        r_kst = [Res("kst%d" % i) for i in range(2)]
        vst = [sb("vst%d" % i, [128, 4, 512], BF16) for i in range(2)]
        r_vst = [Res("vst%d" % i) for i in range(2)]
        xrp = [sb("xrp%d" % i, [128, 4, TT + 3]) for i in range(2)]
        r_xrp = [[Res("xrp%d_%d" % (i, g)) for g in range(4)] for i in range(2)]
        xcL = [sb("xc%d" % i, [128, TT]) for i in range(2)]; r_xcL = [Res("xc%d" % i) for i in range(2)]
        xcbL = [sb("xcb%d" % i, [128, TT], BF16) for i in range(2)]; r_xcbL = [Res("xcb%d" % i) for i in range(2)]
        thrL = [sb("thr%d" % i, [128, TT]) for i in range(2)]; r_thrL = [Res("thr%d" % i) for i in range(2)]
        thiL = [sb("thi%d" % i, [128, TT]) for i in range(2)]; r_thiL = [Res("thi%d" % i) for i in range(2)]
        a_tL = [sb("a_t%d" % i, [128, TT]) for i in range(2)]; r_aL = [Res("a%d" % i) for i in range(2)]
        a2_tL = [sb("a2_t%d" % i, [128, TT]) for i in range(2)]; r_a2L = [Res("a2%d" % i) for i in range(2)]
        u_tL = [sb("u_t%d" % i, [128, TT]) for i in range(2)]; r_uL = [Res("u%d" % i) for i in range(2)]
        h_t = [sb("h_t%d" % g, [128, TT]) for g in range(4)]
        r_h = [Res("h%d" % g) for g in range(4)]
        carry = sb("carry", [128, 4])
        r_carry = [Res("carry%d" % g) for g in range(4)]

        ps_ss = ps("ps_ss", [128, TT]); r_pss = Res("ps_ss")
        ps_k = [ps("ps_k%d" % i, [64, TT]) for i in range(2)]
        r_psk = [Res("ps_k%d" % i) for i in range(2)]
        ps_v = [ps("ps_v%d" % i, [128, 512]) for i in range(2)]
        r_psv = [Res("ps_v%d" % i) for i in range(2)]
        ps_x = ps("ps_x", [128, TT]); r_psx = Res("ps_x")
        ps_r = ps("ps_r", [128, TT]); r_psr = Res("ps_r")
        ps_i = ps("ps_i", [128, TT]); r_psi = Res("ps_i")

        for g in range(4):
            fw.op("pool", lambda e, g=g: e.memset(xrp[0][:, g, 0:3], 0.0), writes=[r_xrp[0][g]])

        xn_v = xn.rearrange("(c p) t -> p c t", p=128)
        cnt = {"k": 0, "v": 0}

        def xload(k):
            t0 = k * TT
            xb = xin[k % 2]; rxb = r_xin[k % 2]
            for c in range(0, 8, 4):
                fw.dma("sp", xb[:, c:c + 4, :], xn_v[:, c:c + 4, t0:t0 + TT], writes=[rxb])

        def front(k):
            xb = xin[k % 2]; rxb = r_xin[k % 2]
            hb = hn[k % 2]; rhb = r_hn[k % 2]
            fw.op("act", lambda e: e.activation(out=xsq[:], in_=xb[:], func=AF.Square),
                  reads=[rxb], writes=[r_xsq])
            for c in range(8):
                fw.op("pe", lambda e, c=c: e.matmul(ps_ss[:], lhsT=ones_bf[:], rhs=xsq[:, c, :],
                                                    start=(c == 0), stop=(c == 7)),
                      inc=(c == 7), reads=[r_ones, r_xsq], writes=[r_pss])
            fw.op("act", lambda e: e.activation(out=rb[:], in_=ps_ss[:], func=AF.Ln, bias=eps_sb[:, 0:1],
                                                scale=1.0 / D), reads=[r_pss, r_ones], writes=[r_rb])
            fw.op("act", lambda e: e.activation(out=rb[:], in_=rb[:], func=AF.Exp, scale=-0.5),
                  reads=[r_rb], writes=[r_rb])
            for c in range(8):
                fw.op("dve", lambda e, c=c: e.scalar_tensor_tensor(
                    out=hb[:, c, :], in0=xb[:, c, :], scalar=g_sb[:, c:c + 1], in1=rb[:],
                    op0=ALU.mult, op1=ALU.mult), reads=[rxb, r_g, r_rb], writes=[rhb])

        def kv_quarter(k, q):
            t0 = k * TT
            hb = hn[k % 2]; rhb = r_hn[k % 2]
            ks = kst[k % 2]; rks = r_kst[k % 2]
            vs = vst[k % 2]; rvs = r_vst[k % 2]
            for hm in (2 * q, 2 * q + 1):
                pk = ps_k[cnt["k"] % 2]; rpk = r_psk[cnt["k"] % 2]
                cnt["k"] += 1
                for c in range(8):
                    fw.op("pe", lambda e, c=c, hm=hm, pk=pk: e.matmul(
                        pk[:], lhsT=wk_sb[:, c, hm * 64:(hm + 1) * 64], rhs=hb[:, c, :],
                        start=(c == 0), stop=(c == 7)), inc=(c == 7), reads=[r_w1, rhb], writes=[rpk])
                fw.op("act", lambda e, hm=hm, pk=pk: e.activation(out=ks[:, hm, :], in_=pk[:], func=AF.Copy),
                      reads=[rpk], writes=[rks])
            tb = q
            pv = ps_v[cnt["v"] % 2]; rpv = r_psv[cnt["v"] % 2]
            cnt["v"] += 1
            for c in range(8):
                fw.op("pe", lambda e, c=c, tb=tb, pv=pv: e.matmul(
                    pv[:], lhsT=hb[:, c, tb * 128:(tb + 1) * 128], rhs=wv_sb[:, c, :],
                    start=(c == 0), stop=(c == 7)), inc=(c == 7), reads=[r_w1, rhb], writes=[rpv])
            fw.op("dve", lambda e, tb=tb, pv=pv: e.tensor_copy(out=vs[:, tb, :], in_=pv[:]),
                  reads=[rpv], writes=[rvs])
            if q == 3:
                fw.dma("pool", kt_scr[:, :, t0:t0 + TT].rearrange("h p t -> p h t"), ks[:, :, :], reads=[rks])
                fw.dma("pool", v_scr[t0:t0 + TT, :].rearrange("(b p) n -> p b n", p=128), vs[:, :, :], reads=[rvs])

        def lru_a(k, g):
            gi = g % 2
            xc = xcL[gi]; r_xc = r_xcL[gi]; xcb = xcbL[gi]; r_xcb = r_xcbL[gi]
            thr = thrL[gi]; r_thr = r_thrL[gi]; thi = thiL[gi]; r_thi = r_thiL[gi]
            a_t = a_tL[gi]; r_a = r_aL[gi]; a2_t = a2_tL[gi]; r_a2 = r_a2L[gi]; u_t = u_tL[gi]; r_u = r_uL[gi]
            hb = hn[k % 2]; rhb = r_hn[k % 2]
            xp = xrp[k % 2]; xp_n = xrp[(k + 1) % 2]
            rxp = r_xrp[k % 2][g]; rxpn = r_xrp[(k + 1) % 2][g]
            for c in range(8):
                fw.op("pe", lambda e, c=c, g=g: e.matmul(
                    ps_x[:], lhsT=wx_sb[:, c, g * 128:(g + 1) * 128], rhs=hb[:, c, :],
                    start=(c == 0), stop=(c == 7)), inc=(c == 7), reads=[r_w1, rhb], writes=[r_psx])
            fw.op("act", lambda e, g=g: e.activation(out=xp[:, g, 3:TT + 3], in_=ps_x[:], func=AF.Copy),
                  reads=[r_psx], writes=[rxp])
            fw.op("pool", lambda e, g=g: e.tensor_copy(out=xp_n[:, g, 0:3], in_=xp[:, g, TT:TT + 3]),
                  reads=[rxp], writes=[rxpn])
            fw.op("dve", lambda e, g=g: e.tensor_scalar(
                out=xc[:], in0=xp[:, g, 0:TT], scalar1=cw_sb[:, g, 0:1], scalar2=cb_sb[:, g:g + 1],
                op0=ALU.mult, op1=ALU.add), reads=[rxp, r_small], writes=[r_xc])
            for j in range(1, 4):
                fw.op("dve", lambda e, g=g, j=j: e.scalar_tensor_tensor(
                    out=xc[:], in0=xp[:, g, j:j + TT], scalar=cw_sb[:, g, j:j + 1], in1=xc[:],
                    op0=ALU.mult, op1=ALU.add), reads=[rxp, r_small, r_xc], writes=[r_xc])
            fw.op("pool", lambda e: e.tensor_copy(out=xcb[:], in_=xc[:]), reads=[r_xc], writes=[r_xcb])

        def lru_b(k, g):
            gi = g % 2
            xc = xcL[gi]; r_xc = r_xcL[gi]; xcb = xcbL[gi]; r_xcb = r_xcbL[gi]
            thr = thrL[gi]; r_thr = r_thrL[gi]; thi = thiL[gi]; r_thi = r_thiL[gi]
            a_t = a_tL[gi]; r_a = r_aL[gi]; a2_t = a2_tL[gi]; r_a2 = r_a2L[gi]; u_t = u_tL[gi]; r_u = r_uL[gi]
            fw.op("pe", lambda e, g=g: e.matmul(ps_r[:], lhsT=wr_sb[:, g, :], rhs=xcb[:], start=True, stop=True),
                  inc=True, reads=[r_wgate, r_xcb], writes=[r_psr])
            fw.op("pe", lambda e, g=g: e.matmul(ps_i[:], lhsT=wi_sb[:, g, :], rhs=xcb[:], start=True, stop=True),
                  inc=True, reads=[r_wgate, r_xcb], writes=[r_psi])
            fw.op("act", lambda e, g=g: e.activation(out=thr[:], in_=ps_r[:], func=AF.Tanh,
                                                     bias=hbr[:, g:g + 1], scale=0.5),
                  reads=[r_psr, r_const], writes=[r_thr])
            fw.op("act", lambda e, g=g: e.activation(out=thi[:], in_=ps_i[:], func=AF.Tanh,
                                                     bias=hbi[:, g:g + 1], scale=0.5),
                  reads=[r_psi, r_const], writes=[r_thi])
            fw.op("act", lambda e, g=g: e.activation(out=a_t[:], in_=thr[:], func=AF.Exp,
                                                     bias=hc[:, g:g + 1], scale=hc[:, g:g + 1]),
                  reads=[r_thr, r_const], writes=[r_a])
            fw.op("act", lambda e, g=g: e.activation(out=a2_t[:], in_=thr[:], func=AF.Exp,
                                                     bias=cc[:, g:g + 1], scale=cc[:, g:g + 1]),
                  reads=[r_thr, r_const], writes=[r_a2])
            fw.op("act", lambda e: e.activation(out=a2_t[:], in_=a2_t[:], func=AF.Ln, bias=one_sb[:, 0:1],
                                                scale=-1.0), reads=[r_a2, r_ones], writes=[r_a2])
            fw.op("act", lambda e: e.activation(out=a2_t[:], in_=a2_t[:], func=AF.Exp, scale=0.5),
                  reads=[r_a2], writes=[r_a2])
            if k == 0:
                fw.op("dve", lambda e: e.memset(a2_t[:, 0:1], 1.0), reads=[r_a2], writes=[r_a2])
            fw.op("dve", lambda e: e.scalar_tensor_tensor(out=u_t[:], in0=thi[:], scalar=1.0, in1=xc[:],
                                                          op0=ALU.add, op1=ALU.mult),
                  reads=[r_thi, r_xc], writes=[r_u])
            fw.op("dve", lambda e: e.scalar_tensor_tensor(out=u_t[:], in0=a2_t[:], scalar=0.5, in1=u_t[:],
                                                          op0=ALU.mult, op1=ALU.mult),
                  reads=[r_a2, r_u], writes=[r_u])
            if k == 0:
                fw.op("dve", lambda e, g=g: e.tensor_tensor_scan(out=h_t[g][:], data0=a_t[:], data1=u_t[:],
                                                                initial=0.0, op0=ALU.mult, op1=ALU.add),
                      reads=[r_a, r_u], writes=[r_h[g]])
            else:
                fw.op("dve", lambda e, g=g: e.tensor_copy(out=carry[:, g:g + 1], in_=h_t[g][:, TT - 1:TT]),
                      reads=[r_h[g]], writes=[r_carry[g]])
                fw.op("dve", lambda e, g=g: e.tensor_tensor_scan(out=h_t[g][:], data0=a_t[:], data1=u_t[:],
                                                                initial=carry[:, g:g + 1], op0=ALU.mult, op1=ALU.add),
                      reads=[r_a, r_u, r_carry[g]], writes=[r_h[g]])
            fw.op("dve", lambda e, g=g, k=k: e.tensor_scalar(
                out=lru_own[:, g, k * 128:(k + 1) * 128], in0=h_t[g][:, 0:128], scalar1=selj_sb[:, 0:1],
                scalar2=None, op0=ALU.mult), reads=[r_h[g], r_small], writes=[r_lru])
            for jj in range(1, 4):
                fw.op("dve", lambda e, g=g, k=k, jj=jj: e.scalar_tensor_tensor(
                    out=lru_own[:, g, k * 128:(k + 1) * 128], in0=h_t[g][:, jj * 128:(jj + 1) * 128],
                    scalar=selj_sb[:, jj:jj + 1], in1=lru_own[:, g, k * 128:(k + 1) * 128],
                    op0=ALU.mult, op1=ALU.add), reads=[r_h[g], r_small, r_lru], writes=[r_lru])

        xload(0)
        xload(1)
        front(0)
        for q in range(4):
            kv_quarter(0, q)
        for k in range(NT):
            lru_a(k, 0)
            if k + 1 < NT:
                front(k + 1)
            for g in range(4):
                if g + 1 < 4:
                    lru_a(k, g + 1)
                if g == 0 and k + 2 < NT:
                    xload(k + 2)
                if k + 1 < NT:
                    kv_quarter(k + 1, g)
                lru_b(k, g)
        fw.barrier()
        es1.close()

        set_free([[120 * KB, TOP]])
        es2 = ExitStack()
        cur["es"] = es2
        wq_sb = sb("wq_sb", [128, 8, 512], BF16)
        wy_sb = sb("wy_sb", [128, 8, 512], BF16)
        r_w2 = Res("w2")
        w2stg = [sb("w2stg%d" % i, [128, 8, 512]) for i in range(2)]
        r_w2stg = [Res("w2stg%d" % i) for i in range(2)]
        for i_, (wsb, c0) in enumerate(((wq_sb, 0), (wy_sb, 2048))):
            for c in range(0, 8, 4):
                fw.dma("sp", w2stg[i_][:, c:c + 4, :], w_in_v[:, c:c + 4, c0:c0 + 512], writes=[r_w2stg[i_]])
            fw.op("act", lambda e, wsb=wsb, i_=i_: e.activation(out=wsb[:, 0:4, :], in_=w2stg[i_][:, 0:4, :], func=AF.Copy),
                  reads=[r_w2stg[i_]], writes=[r_w2])
            fw.op("dve", lambda e, wsb=wsb, i_=i_: e.tensor_copy(out=wsb[:, 4:8, :], in_=w2stg[i_][:, 4:8, :]),
                  reads=[r_w2stg[i_]], writes=[r_w2])
        r_qt = Res("qt")
        for h in range(NH):
            for m_ in range(2):
                fw.dma("sp", qt[64:68, 2 * h + m_, :], qaug[h, :, :], writes=[r_qt])
        xin2 = sb("xin2", [128, 8, TT]); r_xin2 = Res("xin2")
        xsq2 = sb("xsq2", [128, 8, TT], BF16); r_xsq2 = Res("xsq2")
        rb2 = sb("rb2", [128, TT]); r_rb2 = Res("rb2")
        ysb = sb("ysb", [128, TT]); r_ysb = Res("ysb")
        y2 = sb("y2", [128, TT]); r_y2 = Res("y2")
        thy = sb("thy", [128, TT]); r_thy = Res("thy")
        r_hno = Res("hn_own")
        r_lruA = Res("lruA")
        p2_ss = ps("p2_ss", [128, TT]); r_p2ss = Res("p2ss")
        p2_q = [ps("p2_q%d" % i, [64, TT]) for i in range(2)]
        r_p2q = [Res("p2q%d" % i) for i in range(2)]
        p2_y = [ps("p2_y%d" % i, [128, TT]) for i in range(2)]
        r_p2y = [Res("p2y%d" % i) for i in range(2)]
        xo_v = xo.rearrange("(c p) t -> p c t", p=128)
        for m in range(4):
            t0 = m * TT
            for c in range(0, 8, 4):
                fw.dma("sp", xin2[:, c:c + 4, :], xo_v[:, c:c + 4, t0:t0 + TT], writes=[r_xin2])
            fw.op("act", lambda e: e.activation(out=xsq2[:], in_=xin2[:], func=AF.Square),
                  reads=[r_xin2], writes=[r_xsq2])
            for c in range(8):
                fw.op("pe", lambda e, c=c: e.matmul(p2_ss[:], lhsT=ones_bf[:], rhs=xsq2[:, c, :],
                                                    start=(c == 0), stop=(c == 7)),
                      inc=(c == 7), reads=[r_ones, r_xsq2], writes=[r_p2ss])
            fw.op("act", lambda e: e.activation(out=rb2[:], in_=p2_ss[:], func=AF.Ln, bias=eps_sb[:, 0:1],
                                                scale=1.0 / D), reads=[r_p2ss, r_ones], writes=[r_rb2])
            fw.op("act", lambda e: e.activation(out=rb2[:], in_=rb2[:], func=AF.Exp, scale=-0.5),
                  reads=[r_rb2], writes=[r_rb2])
            for c in range(8):
                fw.op("dve", lambda e, c=c: e.scalar_tensor_tensor(
                    out=hn_own[:, c, t0:t0 + TT], in0=xin2[:, c, :], scalar=g_sb[:, c:c + 1], in1=rb2[:],
                    op0=ALU.mult, op1=ALU.mult), reads=[r_xin2, r_g, r_rb2], writes=[r_hno])
            for hm in range(8):
                pq = p2_q[hm % 2]; rpq = r_p2q[hm % 2]
                for c in range(8):
                    fw.op("pe", lambda e, c=c, hm=hm, pq=pq: e.matmul(
                        pq[:], lhsT=wq_sb[:, c, hm * 64:(hm + 1) * 64], rhs=hn_own[:, c, t0:t0 + TT],
                        start=(c == 0), stop=(c == 7)), inc=(c == 7), reads=[r_w2, r_hno], writes=[rpq])
                fw.op("act", lambda e, hm=hm, pq=pq: e.activation(out=qt[0:64, hm, t0:t0 + TT], in_=pq[:], func=AF.Copy),
                      reads=[rpq], writes=[r_qt])
            for g in range(4):
                py = p2_y[g % 2]; rpy = r_p2y[g % 2]
                for c in range(8):
                    fw.op("pe", lambda e, c=c, g=g, py=py: e.matmul(
                        py[:], lhsT=wy_sb[:, c, g * 128:(g + 1) * 128], rhs=hn_own[:, c, t0:t0 + TT],
                        start=(c == 0), stop=(c == 7)), inc=(c == 7), reads=[r_w2, r_hno], writes=[rpy])
                fw.op("act", lambda e, py=py: e.activation(out=ysb[:], in_=py[:], func=AF.Copy),
                      reads=[rpy], writes=[r_ysb])
                fw.op("act", lambda e, py=py: e.activation(out=y2[:], in_=py[:], func=AF.Square),
                      reads=[rpy], writes=[r_y2])
                fw.op("dve", lambda e: e.tensor_scalar(out=y2[:], in0=y2[:], scalar1=0.044715, scalar2=1.0,
                                                       op0=ALU.mult, op1=ALU.add), reads=[r_y2], writes=[r_y2])
                fw.op("dve", lambda e: e.tensor_tensor(out=y2[:], in0=y2[:], in1=ysb[:], op=ALU.mult),
                      reads=[r_y2, r_ysb], writes=[r_y2])
                fw.op("act", lambda e: e.activation(out=thy[:], in_=y2[:], func=AF.Tanh, scale=GELU_K),
                      reads=[r_y2], writes=[r_thy])
                fw.op("dve", lambda e: e.scalar_tensor_tensor(out=thy[:], in0=thy[:], scalar=1.0, in1=ysb[:],
                                                              op0=ALU.add, op1=ALU.mult),
                      reads=[r_thy, r_ysb], writes=[r_thy])
                fw.op("dve", lambda e, g=g: e.scalar_tensor_tensor(
                    out=lruA[:, g, t0:t0 + TT], in0=thy[:], scalar=0.5, in1=lru_own[:, g, t0:t0 + TT],
                    op0=ALU.mult, op1=ALU.mult), reads=[r_thy, r_lru], writes=[r_lruA])
        fw.barrier()
        es2.close()

        set_free([[136 * KB, 172 * KB]])
        wgA = sb_at("wgA", [128, 8, D], BF16, 172 * KB)
        wgL = sb_at("wgL", [128, 8, D], BF16, 188 * KB)
        r_wgA = Res("wgA")

        es3 = ExitStack()
        cur["es"] = es3
        kt = [sb_at("kt%d" % i, [128, S], BF16, 8 * KB + i * 16 * KB) for i in range(2)]
        r_kt = [Res("kt%d" % i) for i in range(2)]
        vh = sb("vh", [128, 64, 128], BF16); r_vh = Res("vh")
        pt = [sb("pt%d" % i, [128, 2, 512], BF16) for i in range(2)]
        r_pt = [Res("pt%d" % i) for i in range(2)]
        rl = sb("rl", [128, 2, 512]); r_rl = Res("rl")
        dd = sb("dd", [128, 512]); r_dd = Res("dd")
        tmp1 = sb("tmp1", [128, 512]); r_tmp1 = Res("tmp1")
        sq3 = sb("sq3", [128, 512], BF16); r_sq3 = Res("sq3")
        rs3 = sb("rs3", [128, 512]); r_rs3 = Res("rs3")
        cm_sb = sb("cm_sb", [128, 4, 128], BF16); r_cm = Res("cm")
        lp = sb("lp", [64, 4]); lpp = sb("lpp", [64, 2]); ones64 = sb("ones64", [64, 128])
        nlam = sb("nlam", [128, 1]); elam = sb("elam", [128, 2]); gs = sb("gs", [128, 1])
        r_lam = Res("lam")
        fw.dma("sp", cm_sb[:], cmask[:, :, :], writes=[r_cm])
        ident_sb = sb("ident_sb", [128, 128], BF16)
        negm = sb("negm", [128, 4, 128], BF16); r_negm = Res("negm")
        fw.dma("sp", ident_sb[:], ident[:, :], writes=[r_negm])
        fw.op("dve", lambda e: e.tensor_scalar(out=negm[:], in0=cm_sb[:], scalar1=-1.0, scalar2=30000.0,
                                               op0=ALU.add, op1=ALU.mult), reads=[r_cm, r_negm], writes=[r_negm])
        fw.dma("sp", lp[:], lam_qk[:, :], writes=[r_lam])
        fw.dma("sp", gs[:], subln[:, :], writes=[r_lam])
        for i in range(2):
            fw.dma("sp", kt[i][64:68, :], kaug[:, :], writes=[r_kt[i]])
        ps_s = [ps("ps_s%d" % i, [128, 2, 512]) for i in range(2)]
        r_pss3 = [Res("ps_s%d" % i) for i in range(2)]
        po = ps("po", [128, 2, 512]); r_po = Res("po")
        pl_ = ps("pl_", [128, 2, 512]); r_pl = Res("pl")
        fw.op("pool", lambda e: e.memset(ones64[:], 1.0), writes=[r_lam])
        fw.op("dve", lambda e: e.tensor_tensor(out=lpp[:, 0:1], in0=lp[:, 0:1], in1=lp[:, 1:2], op=ALU.mult),
              reads=[r_lam], writes=[r_lam])
        fw.op("dve", lambda e: e.tensor_tensor(out=lpp[:, 1:2], in0=lp[:, 2:3], in1=lp[:, 3:4], op=ALU.mult),
              reads=[r_lam], writes=[r_lam])
        fw.op("pe", lambda e: e.matmul(ps_s[0][:, 0, 0:2], lhsT=ones64[:], rhs=lpp[:], start=True, stop=True),
              inc=True, reads=[r_lam], writes=[r_pss3[0]])
        fw.op("act", lambda e: e.activation(out=elam[:], in_=ps_s[0][:, 0, 0:2], func=AF.Exp),
              reads=[r_pss3[0]], writes=[r_lam])
        fw.op("dve", lambda e: e.tensor_tensor(out=nlam[:], in0=elam[:, 1:2], in1=elam[:, 0:1], op=ALU.subtract),
              reads=[r_lam], writes=[r_lam])
        fw.op("dve", lambda e: e.tensor_scalar(out=nlam[:], in0=nlam[:], scalar1=-LAM_INIT, scalar2=None,
                                               op0=ALU.add), reads=[r_lam], writes=[r_lam])
        fw.op("dve", lambda e: e.tensor_scalar(out=gs[:], in0=gs[:], scalar1=1.0 - LAM_INIT, scalar2=None,
                                               op0=ALU.mult), reads=[r_lam], writes=[r_lam])
        r_attn = Res("attnT")

        def load_k(h):
            for m_ in range(2):
                fw.dma("sp", kt[m_][0:64, :], kt_scr[2 * h + m_, :, :], writes=[r_kt[m_]])

        def load_v(h):
            for q4 in range(4):
                fw.dma("sp", vh[:, q4 * 16:(q4 + 1) * 16, :],
                       v_scr[q4 * 2048:(q4 + 1) * 2048, h * 128:(h + 1) * 128].rearrange("(b p) v -> p b v", p=128),
                       writes=[r_vh])

        steps = []
        for h in range(NH):
            for m in range(4):
                nkb = 16 * m + 16
                for kb in range(nkb):
                    qmin = max(0, (kb - 16 * m) // 4) if kb >= 16 * m else 0
                    steps.append(dict(h=h, m=m, kb=kb, c0=128 * qmin, nkb=nkb, diag=(kb >= 16 * m),
                                      jj=(kb - 16 * m) % 4, i=len(steps)))

        def emit_qk(st):
            h, m, kb, c0 = st["h"], st["m"], st["kb"], st["c0"]
            pss = ps_s[st["i"] % 2]; rps = r_pss3[st["i"] % 2]
            for m_ in range(2):
                fw.op("pe", lambda e, m_=m_: e.matmul(
                    pss[:, m_, c0:512], lhsT=kt[m_][0:KA, kb * 128:(kb + 1) * 128],
                    rhs=qt[0:KA, 2 * h + m_, m * 512 + c0:(m + 1) * 512], start=True, stop=(not st["diag"])),
                    inc=(not st["diag"]), reads=[r_kt[m_], r_qt], writes=[rps])
                if st["diag"]:
                    fw.op("pe", lambda e, m_=m_: e.matmul(
                        pss[:, m_, c0:c0 + 128], lhsT=ident_sb[:], rhs=negm[:, st["jj"], :], start=False, stop=True),
                        inc=True, reads=[r_negm], writes=[rps])

        def emit_exp(st):
            c0 = st["c0"]
            pss = ps_s[st["i"] % 2]; rps = r_pss3[st["i"] % 2]
            ptb = pt[st["i"] % 2]; rptb = r_pt[st["i"] % 2]
            fw.op("act", lambda e: e.activation(out=ptb[:, :, c0:512], in_=pss[:, :, c0:512], func=AF.Exp, scale=0.125),
                  reads=[rps], writes=[rptb])

        def emit_pv(st):
            kb, c0, nkb = st["kb"], st["c0"], st["nkb"]
            ptb = pt[st["i"] % 2]; rptb = r_pt[st["i"] % 2]
            for m_ in range(2):
                fw.op("pe", lambda e, m_=m_: e.matmul(
                    po[:, m_, c0:512], lhsT=vh[:, kb, :], rhs=ptb[:, m_, c0:512],
                    start=(kb == 0), stop=(kb == nkb - 1)), inc=(kb == nkb - 1), reads=[r_vh, rptb], writes=[r_po])
                fw.op("pe", lambda e, m_=m_: e.matmul(
                    pl_[:, m_, c0:512], lhsT=ones_bf[:], rhs=ptb[:, m_, c0:512],
                    start=(kb == 0), stop=(kb == nkb - 1)), inc=(m_ == 1 or kb == nkb - 1), reads=[r_ones, rptb], writes=[r_pl])

        def finalize(h, m, sbuf_i):
            pfin = ps_s[sbuf_i]; rpfin = r_pss3[sbuf_i]
            fw.op("dve", lambda e: e.reciprocal(out=rl[:], in_=pl_[:]), reads=[r_pl], writes=[r_rl])
            fw.op("dve", lambda e: e.tensor_tensor(out=dd[:], in0=po[:, 0, :], in1=rl[:, 0, :], op=ALU.mult),
                  reads=[r_po, r_rl], writes=[r_dd])
            fw.op("dve", lambda e: e.tensor_tensor(out=tmp1[:], in0=po[:, 1, :], in1=rl[:, 1, :], op=ALU.mult),
                  reads=[r_po, r_rl], writes=[r_tmp1])
            fw.op("dve", lambda e: e.scalar_tensor_tensor(out=dd[:], in0=tmp1[:], scalar=nlam[:, 0:1], in1=dd[:],
                                                          op0=ALU.mult, op1=ALU.add),
                  reads=[r_tmp1, r_lam, r_dd], writes=[r_dd])
            fw.op("act", lambda e: e.activation(out=sq3[:], in_=dd[:], func=AF.Square),
                  reads=[r_dd], writes=[r_sq3])
            fw.op("pe", lambda e: e.matmul(pfin[:, 0, :], lhsT=ones_bf[:], rhs=sq3[:], start=True, stop=True),
                  inc=True, reads=[r_ones, r_sq3], writes=[rpfin])
            fw.op("act", lambda e: e.activation(out=rs3[:], in_=pfin[:, 0, :], func=AF.Ln, bias=eps_sb[:, 0:1],
                                                scale=1.0 / 128.0), reads=[rpfin, r_ones], writes=[r_rs3])
            fw.op("act", lambda e: e.activation(out=rs3[:], in_=rs3[:], func=AF.Exp, scale=-0.5),
                  reads=[r_rs3], writes=[r_rs3])
            fw.op("dve", lambda e: e.scalar_tensor_tensor(
                out=attnT[:, h, m * 512:(m + 1) * 512], in0=dd[:], scalar=gs[:, 0:1], in1=rs3[:],
                op0=ALU.mult, op1=ALU.mult), reads=[r_dd, r_lam, r_rs3], writes=[r_attn])

        load_k(0)
        load_v(0)
        emit_qk(steps[0])
        for c in range(8):
            fw.dma("pool", wgA[:, c, :], w_in_v[:, c, 2560:2560 + D], writes=[r_wgA])
            fw.dma("pool", wgL[:, c, :], w_in_v[:, c, 3584:3584 + D], writes=[r_wgA])
        for i, st in enumerate(steps):
            nxt = steps[i + 1] if i + 1 < len(steps) else None
            newh = nxt is not None and nxt["h"] != st["h"]
            if newh:
                load_k(nxt["h"])
            if nxt is not None:
                emit_qk(nxt)
            emit_exp(st)
            emit_pv(st)
            if newh:
                load_v(nxt["h"])
            if st["kb"] == st["nkb"] - 1:
                finalize(st["h"], st["m"], st["i"] % 2)
        fw.barrier()
        es3.close()

        set_free([[8 * KB, 72 * KB], [168 * KB, 172 * KB]])
        es4 = ExitStack()
        cur["es"] = es4
        woa = sb("woa", [128, 4, D], BF16)
        wol = sb("wol", [128, 4, D], BF16)
        r_w4 = Res("w4")
        woa_v = w_o_attn.rearrange("(c p) n -> p c n", p=128)
        wol_v = w_o_lru.rearrange("(c p) n -> p c n", p=128)
        w4stg = [sb("w4stg%d" % i, [128, 4, D]) for i in range(2)]
        r_w4stg = [Res("w4stg%d" % i) for i in range(2)]
        for i_, (wsb, wv_) in enumerate(((woa, woa_v), (wol, wol_v))):
            for c in range(0, 4, 2):
                fw.dma("sp", w4stg[i_][:, c:c + 2, :], wv_[:, c:c + 2, :], writes=[r_w4stg[i_]])
            fw.op("act", lambda e, wsb=wsb, i_=i_: e.activation(out=wsb[:, 0:2, :], in_=w4stg[i_][:, 0:2, :], func=AF.Copy),
                  reads=[r_w4stg[i_]], writes=[r_w4])
            fw.op("dve", lambda e, wsb=wsb, i_=i_: e.tensor_copy(out=wsb[:, 2:4, :], in_=w4stg[i_][:, 2:4, :]),
                  reads=[r_w4stg[i_]], writes=[r_w4])
        thA = sb("thA", [128, TT]); r_thA = Res("thA")
        thL = sb("thL", [128, TT]); r_thL = Res("thL")
        m1 = sb("m1", [128, TT]); r_m1 = Res("m1")
        m2 = sb("m2", [128, TT]); r_m2 = Res("m2")
        r_mg = Res("merged")
        pA = ps("pA", [128, TT]); r_pA = Res("pA")
        pL = ps("pL", [128, TT]); r_pL = Res("pL")
        pGA = ps("pGA", [128, TT]); r_pGA = Res("pGA")
        pGL = ps("pGL", [128, TT]); r_pGL = Res("pGL")
        for f in range(8):
            for m in range(4):
                t0 = m * TT
                for c in range(4):
                    fw.op("pe", lambda e, c=c, f=f, t0=t0: e.matmul(
                        pA[:], lhsT=woa[:, c, f * 128:(f + 1) * 128], rhs=attnT[:, c, t0:t0 + TT],
                        start=(c == 0), stop=(c == 3)), inc=(c == 3), reads=[r_w4, r_attn], writes=[r_pA])
                for c in range(4):
                    fw.op("pe", lambda e, c=c, f=f, t0=t0: e.matmul(
                        pL[:], lhsT=wol[:, c, f * 128:(f + 1) * 128], rhs=lruA[:, c, t0:t0 + TT],
                        start=(c == 0), stop=(c == 3)), inc=(c == 3), reads=[r_w4, r_lruA], writes=[r_pL])
                for c in range(8):
                    fw.op("pe", lambda e, c=c, f=f, t0=t0: e.matmul(
                        pGA[:], lhsT=wgA[:, c, f * 128:(f + 1) * 128], rhs=hn_own[:, c, t0:t0 + TT],
                        start=(c == 0), stop=(c == 7)), inc=(c == 7), reads=[r_wgA, r_hno], writes=[r_pGA])
                for c in range(8):
                    fw.op("pe", lambda e, c=c, f=f, t0=t0: e.matmul(
                        pGL[:], lhsT=wgL[:, c, f * 128:(f + 1) * 128], rhs=hn_own[:, c, t0:t0 + TT],
                        start=(c == 0), stop=(c == 7)), inc=(c == 7), reads=[r_wgA, r_hno], writes=[r_pGL])
                fw.op("act", lambda e: e.activation(out=thA[:], in_=pGA[:], func=AF.Tanh, scale=0.5),
                      reads=[r_pGA], writes=[r_thA])
                fw.op("act", lambda e: e.activation(out=thL[:], in_=pGL[:], func=AF.Tanh, scale=0.5),
                      reads=[r_pGL], writes=[r_thL])
                fw.op("dve", lambda e: e.scalar_tensor_tensor(out=m1[:], in0=thA[:], scalar=1.0, in1=pA[:],
                                                              op0=ALU.add, op1=ALU.mult),
                      reads=[r_thA, r_pA], writes=[r_m1])
                fw.op("dve", lambda e: e.scalar_tensor_tensor(out=m2[:], in0=thL[:], scalar=1.0, in1=pL[:],
                                                              op0=ALU.add, op1=ALU.mult),
                      reads=[r_thL, r_pL], writes=[r_m2])
                fw.op("dve", lambda e, f=f, t0=t0: e.tensor_tensor(out=merged[:, f, t0:t0 + TT], in0=m1[:], in1=m2[:],
                                                                  op=ALU.add), reads=[r_m1, r_m2], writes=[r_mg])
        fw.barrier()
        es4.close()

        set_free([[104 * KB, 136 * KB], [168 * KB, 203 * KB]])
        es5 = ExitStack()
        cur["es"] = es5
        wout = sb("wout", [128, 8, D], BF16); r_wo = Res("wout")
        wout_v = w_out.rearrange("(c p) n -> p c n", p=128)
        x1T = sb("x1T", [128, 8, TT]); r_x1T = Res("x1T")
        for hf in range(2):
            for c in range(4):
                fw.dma("sp", x1T[:, 2 * c:2 * c + 2, :], wout_v[:, 4 * hf + c, :].rearrange("p (a b) -> p a b", a=2),
                       writes=[r_x1T])
            for c in range(4):
                if c % 2 == 0:
                    fw.op("act", lambda e, c=c, hf=hf: e.activation(
                        out=wout[:, 4 * hf + c, :].rearrange("p (a b) -> p a b", a=2), in_=x1T[:, 2 * c:2 * c + 2, :],
                        func=AF.Copy), reads=[r_x1T], writes=[r_wo])
                else:
                    fw.op("dve", lambda e, c=c, hf=hf: e.tensor_copy(
                        out=wout[:, 4 * hf + c, :].rearrange("p (a b) -> p a b", a=2), in_=x1T[:, 2 * c:2 * c + 2, :]),
                        reads=[r_x1T], writes=[r_wo])
        xtok = [sb("xtok%d" % i, [128, D]) for i in range(2)]
        r_xtok = [Res("xtok%d" % i) for i in range(2)]
        xoc = [sb("xoc%d" % i, [128, TT]) for i in range(2)]
        r_xoc = [Res("xoc%d" % i) for i in range(2)]
        g2rep = sb("g2rep", [128, D]); r_g2rep = Res("g2rep")
        fw.dma("sp", g2rep[:], g_ffn_rep[:, :], writes=[r_g2rep])
        wr_f = sb("wr_f", [128, 8, 36]); r_wr = Res("wr")
        g2_sb = sb("g2_sb", [128, 8])
        lgL = [sb("lg%d" % i, [128, 36]) for i in range(4)]; r_lgL = [Res("lg%d" % i) for i in range(4)]
        rtL = [sb("rt%d" % i, [128, 64]) for i in range(4)]; r_rtL = [Res("rt%d" % i) for i in range(4)]
        junk = sb("junk", [128, D], BF16); r_junk = Res("junk")
        fw.dma("sp", g2_sb[:], g_ffn[:, :], writes=[r_wr])
        fw.dma("sp", wr_f[:], w_router.rearrange("(c p) n -> p c n", p=128), writes=[r_wr])
        for c in range(8):
            fw.op("dve", lambda e, c=c: e.tensor_scalar(out=wr_f[:, c, :], in0=wr_f[:, c, :], scalar1=g2_sb[:, c:c + 1],
                                                        scalar2=None, op0=ALU.mult), reads=[r_wr], writes=[r_wr])
        r_x1 = Res("x1")
        r_hn2 = Res("hn2k")
        p_o = [ps("p_o%d" % i, [128, 512]) for i in range(2)]
        r_p_o = [Res("p_o%d" % i) for i in range(2)]
        p_t = [ps("p_t%d" % i, [128, 512]) for i in range(2)]
        r_p_t = [Res("p_t%d" % i) for i in range(2)]
        p_lg = ps("p_lg", [128, 4, 64]); r_p_lg = Res("p_lg")
        xot_v = xot.rearrange("(b p) d -> p b d", p=128)
        ocnt = 0
        for m in range(4):
            t0 = m * TT
            for tb in range(4):
                blk = 4 * m + tb
                xt_ = xtok[blk % 2]; rxt = r_xtok[blk % 2]
                fw.dma("sp", xt_[:], xot_v[:, blk, :], writes=[rxt])
                for half in range(2):
                    po_ = p_o[ocnt % 2]; rpo_ = r_p_o[ocnt % 2]
                    ocnt += 1
                    for f in range(8):
                        fw.op("pe", lambda e, f=f, tb=tb, half=half, po_=po_, t0=t0: e.matmul(
                            po_[:], lhsT=merged[:, f, t0 + tb * 128:t0 + (tb + 1) * 128],
                            rhs=wout[:, f, half * 512:(half + 1) * 512], start=(f == 0), stop=(f == 7)),
                            inc=(f == 7), reads=[r_mg, r_wo], writes=[rpo_])
                    fw.op("dve", lambda e, blk=blk, half=half, po_=po_, xt_=xt_: e.scalar_tensor_tensor(
                        out=x1[:, blk, half * 512:(half + 1) * 512], in0=po_[:], scalar=0.5,
                        in1=xt_[:, half * 512:(half + 1) * 512], op0=ALU.mult, op1=ALU.add),
                        reads=[rpo_, rxt], writes=[r_x1])
            for f2 in range(8):
                pt_ = p_t[f2 % 2]; rpt_ = r_p_t[f2 % 2]
                xc_ = xoc[f2 % 2]; rxc_ = r_xoc[f2 % 2]
                fw.dma("sp", xc_[:], xo_v[:, f2, t0:t0 + TT], writes=[rxc_])
                for f in range(8):
                    fw.op("pe", lambda e, f=f, f2=f2, pt_=pt_, t0=t0: e.matmul(
                        pt_[:], lhsT=wout[:, f, f2 * 128:(f2 + 1) * 128], rhs=merged[:, f, t0:t0 + TT],
                        start=(f == 0), stop=(f == 7)), inc=(f == 7), reads=[r_mg, r_wo], writes=[rpt_])
                fw.op("dve", lambda e, f2=f2, pt_=pt_, xc_=xc_: e.scalar_tensor_tensor(
                    out=x1T[:, f2, :], in0=pt_[:], scalar=0.5, in1=xc_[:], op0=ALU.mult, op1=ALU.add),
                    reads=[rpt_, rxc_], writes=[r_x1T])
            for tb in range(4):
                for c in range(8):
                    fw.op("pe", lambda e, c=c, tb=tb: e.matmul(
                        p_lg[:, tb, 0:36], lhsT=x1T[:, c, tb * 128:(tb + 1) * 128], rhs=wr_f[:, c, :],
                        start=(c == 0), stop=(c == 7)), inc=(c == 7), reads=[r_x1T, r_wr], writes=[r_p_lg])
            def route_block(tb, blk, rt, lg, r_rt, r_lg):
                    fw.op("act", lambda e, blk=blk: e.activation(out=junk[:], in_=x1[:, blk, :], func=AF.Square,
                                                                 accum_out=rt[:, 0:1]),
                          reads=[r_x1], writes=[r_junk, r_rt])
                    fw.op("act", lambda e: e.activation(out=rt[:, 1:2], in_=rt[:, 0:1], func=AF.Ln, bias=eps_sb[:, 0:1],
                                                        scale=1.0 / D), reads=[r_rt, r_ones], writes=[r_rt])
                    fw.op("act", lambda e: e.activation(out=rt[:, 1:2], in_=rt[:, 1:2], func=AF.Exp, scale=-0.5),
                          reads=[r_rt], writes=[r_rt])
                    fw.op("dve", lambda e, tb=tb: e.tensor_scalar(out=lg[:], in0=p_lg[:, tb, 0:36], scalar1=rt[:, 1:2],
                                                                  scalar2=None, op0=ALU.mult),
                          reads=[r_p_lg, r_rt], writes=[r_lg])
                    fw.op("dve", lambda e, blk=blk: e.scalar_tensor_tensor(
                        out=hn2k[:, blk, :], in0=x1[:, blk, :], scalar=rt[:, 1:2], in1=g2rep[:],
                        op0=ALU.mult, op1=ALU.mult), reads=[r_x1, r_rt, r_g2rep], writes=[r_hn2])
                    fw.op("dve", lambda e: e.reduce_max(out=rt[:, 2:3], in_=lg[:, 0:4], axis=mybir.AxisListType.X),
                          reads=[r_lg, r_rt], writes=[r_rt])
                    fw.op("dve", lambda e: e.tensor_scalar(out=rt[:, 3:4], in0=rt[:, 2:3], scalar1=-1.0, scalar2=None,
                                                           op0=ALU.mult), reads=[r_rt], writes=[r_rt])
                    fw.op("act", lambda e: e.activation(out=rt[:, 4:8], in_=lg[:, 0:4], func=AF.Exp, bias=rt[:, 3:4],
                                                        accum_out=rt[:, 8:9]), reads=[r_lg, r_rt], writes=[r_rt])
                    fw.op("dve", lambda e: e.reciprocal(out=rt[:, 9:10], in_=rt[:, 8:9]), reads=[r_rt], writes=[r_rt])
                    fw.op("dve", lambda e: e.tensor_scalar(out=rt[:, 10:14], in0=lg[:, 0:4], scalar1=rt[:, 2:3], scalar2=None,
                                                           op0=ALU.is_ge), reads=[r_lg, r_rt], writes=[r_rt])
                    fw.op("dve", lambda e: e.tensor_scalar(out=rt[:, 16:24], in0=lg[:, 4:12], scalar1=rt[:, 10:11], scalar2=None,
                                                           op0=ALU.mult), reads=[r_lg, r_rt], writes=[r_rt])
                    for g in range(1, 4):
                        fw.op("dve", lambda e, g=g: e.scalar_tensor_tensor(
                            out=rt[:, 16:24], in0=lg[:, 4 + 8 * g:12 + 8 * g], scalar=rt[:, 10 + g:11 + g], in1=rt[:, 16:24],
                            op0=ALU.mult, op1=ALU.add), reads=[r_lg, r_rt], writes=[r_rt])
                    fw.op("dve", lambda e: e.reduce_max(out=rt[:, 24:25], in_=rt[:, 16:24], axis=mybir.AxisListType.X),
                          reads=[r_rt], writes=[r_rt])
                    fw.op("dve", lambda e: e.tensor_scalar(out=rt[:, 32:40], in0=rt[:, 16:24], scalar1=rt[:, 24:25], scalar2=None,
                                                           op0=ALU.is_ge), reads=[r_rt], writes=[r_rt])
                    fw.op("dve", lambda e: e.scalar_tensor_tensor(out=rt[:, 40:48], in0=rt[:, 32:40], scalar=-1e30, in1=rt[:, 16:24],
                                                                  op0=ALU.mult, op1=ALU.add), reads=[r_rt], writes=[r_rt])
                    fw.op("dve", lambda e: e.reduce_max(out=rt[:, 25:26], in_=rt[:, 40:48], axis=mybir.AxisListType.X),
                          reads=[r_rt], writes=[r_rt])
                    fw.op("dve", lambda e: e.tensor_scalar(out=rt[:, 48:56], in0=rt[:, 40:48], scalar1=rt[:, 25:26], scalar2=None,
                                                           op0=ALU.is_ge), reads=[r_rt], writes=[r_rt])
                    fw.op("dve", lambda e: e.tensor_tensor(out=rt[:, 26:27], in0=rt[:, 25:26], in1=rt[:, 24:25], op=ALU.subtract),
                          reads=[r_rt], writes=[r_rt])
                    fw.op("act", lambda e: e.activation(out=rt[:, 27:28], in_=rt[:, 26:27], func=AF.Exp),
                          reads=[r_rt], writes=[r_rt])
                    fw.op("dve", lambda e: e.tensor_scalar(out=rt[:, 28:29], in0=rt[:, 27:28], scalar1=1.0, scalar2=None,
                                                           op0=ALU.add), reads=[r_rt], writes=[r_rt])
                    fw.op("dve", lambda e: e.reciprocal(out=rt[:, 28:29], in_=rt[:, 28:29]), reads=[r_rt], writes=[r_rt])
                    fw.op("dve", lambda e: e.tensor_tensor(out=rt[:, 29:30], in0=rt[:, 27:28], in1=rt[:, 28:29], op=ALU.mult),
                          reads=[r_rt], writes=[r_rt])
                    fw.op("dve", lambda e: e.tensor_tensor(out=rt[:, 28:29], in0=rt[:, 28:29], in1=rt[:, 9:10], op=ALU.mult),
                          reads=[r_rt], writes=[r_rt])
                    fw.op("dve", lambda e: e.tensor_tensor(out=rt[:, 29:30], in0=rt[:, 29:30], in1=rt[:, 9:10], op=ALU.mult),
                          reads=[r_rt], writes=[r_rt])
                    fw.op("dve", lambda e: e.tensor_scalar(out=rt[:, 56:64], in0=rt[:, 32:40], scalar1=rt[:, 28:29], scalar2=None,
                                                           op0=ALU.mult), reads=[r_rt], writes=[r_rt])
                    fw.op("dve", lambda e: e.scalar_tensor_tensor(out=rt[:, 56:64], in0=rt[:, 48:56], scalar=rt[:, 29:30],
                                                                  in1=rt[:, 56:64], op0=ALU.mult, op1=ALU.add),
                          reads=[r_rt], writes=[r_rt])
                    for g in range(4):
                        fw.op("dve", lambda e, g=g, blk=blk: e.tensor_scalar(
                            out=Cw[:, blk, 8 * g:8 * g + 8], in0=rt[:, 56:64], scalar1=rt[:, 10 + g:11 + g], scalar2=None,
                            op0=ALU.mult), reads=[r_rt], writes=[r_C])
                    fw.op("dve", lambda e: e.tensor_tensor(out=rt[:, 40:48], in0=rt[:, 32:40], in1=rt[:, 48:56], op=ALU.add),
                          reads=[r_rt], writes=[r_rt])
                    for g in range(4):
                        fw.op("dve", lambda e, g=g, blk=blk: e.tensor_scalar(
                            out=Mf[:, blk, 8 * g:8 * g + 8], in0=rt[:, 40:48], scalar1=rt[:, 10 + g:11 + g], scalar2=None,
                            op0=ALU.mult), reads=[r_rt], writes=[r_M])
                    fw.op("dve", lambda e, blk=blk: e.tensor_copy(out=Mb[:, blk, :], in_=Mf[:, blk, :]),
                          reads=[r_M], writes=[r_M])
            chains = []
            for tb in range(4):
                fw.defer = []
                route_block(tb, 4 * m + tb, rtL[tb], lgL[tb], r_rtL[tb], r_lgL[tb])
                chains.append(fw.defer)
                fw.defer = None
            for i_ in range(max(len(c_) for c_ in chains)):
                for c_ in chains:
                    if i_ < len(c_):
                        ek_, fn_, rd_, wr_, inc_ = c_[i_]
                        fw.op(ek_, fn_, reads=rd_, writes=wr_, inc=inc_)
        fw.barrier()
        es5.close()

        CAP = 256
        set_free([[104 * KB, 203 * KB]])
        es6 = ExitStack()
        cur["es"] = es6
        wgb = sb("wgb", [128, 8, 512], BF16); wub = sb("wub", [128, 8, 512], BF16); wdb = sb("wdb", [128, 4, D], BF16)
        r_wgb = Res("wgb"); r_wub = Res("wub"); r_wdb = Res("wdb")
        NSTG = 4
        stg = [sb("stg%d" % i, [128, 2, 512]) for i in range(NSTG)]
        r_stg = [Res("stg%d" % i) for i in range(NSTG)]
        iota_s = sb("iota_s", [128, CAP]); iota_t = sb("iota_t", [128, TOWN])
        ustr = sb("ustr", [128, 128], BF16); tv = sb("tv", [128, 16, 8], BF16)
        r_k5 = Res("k5")
        fw.dma("sp", iota_s[:], iota_s_d[:, :], writes=[r_k5])
        fw.dma("sp", iota_t[:], iota_t_d[:, :], writes=[r_k5])
        fw.dma("sp", ustr[:], ustrict_d[:, :], writes=[r_k5])
        fw.dma("sp", tv[:], tvals_d[:, :, :], writes=[r_k5])
        pfx = sb("pfx", [128, 16, 32]); r_pfx = Res("pfx")
        Sel = sb("Sel", [128, 16, CAP], BF16); r_Sel = Res("Sel")
        XgT = sb("XgT", [128, 8, CAP], BF16); r_XgT = Res("XgT")
        HT = sb("HT", [128, 4, CAP], BF16); r_HT = Res("HT")
        SelT = [sb("SelT%d" % i, [128, 2, TOWN], BF16) for i in range(2)]
        r_SelT = [Res("SelT%d" % i) for i in range(2)]
        Yb = [sb("Yb%d" % i, [128, 2, D], BF16) for i in range(2)]
        r_Yb = [Res("Yb%d" % i) for i in range(2)]
        tokf = sb("tokf", [128, 2]); r_tokf = Res("tokf")
        tokc = sb("tokc", [128, 2, 2]); r_tokc = Res("tokc")
        s1 = [sb("s1_%d" % i, [128, CAP]) for i in range(2)]
        r_s1 = [Res("s1_%d" % i) for i in range(2)]
        th5 = [sb("th5_%d" % i, [128, CAP]) for i in range(2)]
        r_th5 = [Res("th5_%d" % i) for i in range(2)]

        pA_ = [ps("pA5_%d" % i, [128, 512]) for i in range(2)]
        r_pA_ = [Res("pA5_%d" % i) for i in range(2)]
        pGU = [ps("pGU%d" % i, [128, 512]) for i in range(2)]
        r_pGU = [Res("pGU%d" % i) for i in range(2)]
        pTok = ps("pTok", [128, 16, 32]); r_pTok = Res("pTok")
        pC = [ps("pC%d" % i, [128, 512]) for i in range(3)]
        r_pC = [Res("pC%d" % i) for i in range(3)]

        for blk in range(16):
            for b2 in range(blk + 1):
                fw.op("pe", lambda e, blk=blk, b2=b2: e.matmul(
                    pTok[:, blk, :], lhsT=(ustr[:] if b2 == blk else ones_bf[:]), rhs=Mb[:, b2, :],
                    start=(b2 == 0), stop=(b2 == blk)), inc=(b2 == blk), reads=[r_M, r_k5, r_ones], writes=[r_pTok])
        fw.op("dve", lambda e: e.tensor_copy(out=pfx[:], in_=pTok[:]), reads=[r_pTok], writes=[r_pfx])

        cnt5 = {"stg": 0, "gu": 0, "c": 0}

        wq5 = {"todo": [], "issued": []}

        def w_begin(ex_):
            wg_v = w_gate[ex_].rearrange("(c p) n -> p c n", p=128)
            wu_v = w_up[ex_].rearrange("(c p) n -> p c n", p=128)
            wd_v = w_down[ex_].rearrange("(c p) n -> p c n", p=128)
            pcs = []
            for c in range(0, 8, 2):
                pcs.append((wg_v[:, c:c + 2, :], wgb[:, c:c + 2, :], r_wgb))
            for c in range(0, 8, 2):
                pcs.append((wu_v[:, c:c + 2, :], wub[:, c:c + 2, :], r_wub))
            for half in range(2):
                for f in range(0, 4, 2):
                    pcs.append((wd_v[:, f:f + 2, half * 512:(half + 1) * 512],
                                wdb[:, f:f + 2, half * 512:(half + 1) * 512], r_wdb))
            assert not wq5["todo"] and not wq5["issued"]
            wq5["todo"] = pcs

        def w_issue():
            if not wq5["todo"]:
                return
            src, dst, rdst = wq5["todo"].pop(0)
            sg = stg[cnt5["stg"] % NSTG]; rsg = r_stg[cnt5["stg"] % NSTG]
            cnt5["stg"] += 1
            fw.dma("sp", sg[:], src, writes=[rsg])
            wq5["issued"].append((sg, rsg, dst, rdst))

        def w_cast():
            if not wq5["issued"]:
                return
            sg, rsg, dst, rdst = wq5["issued"].pop(0)
            fw.op("act", lambda e: e.activation(out=dst, in_=sg[:], func=AF.Copy), reads=[rsg], writes=[rdst])

        def stage_a(ex_):
            sT = SelT[ex_ % 2]; rsT = r_SelT[ex_ % 2]
            yb = Yb[ex_ % 2]; ryb = r_Yb[ex_ % 2]
            for blk in range(16):
                fw.op("dve", lambda e, blk=blk: e.tensor_scalar(
                    out=Sel[:, blk, :], in0=iota_s[:], scalar1=pfx[:, blk, ex_:ex_ + 1], scalar2=Mf[:, blk, ex_:ex_ + 1],
                    op0=ALU.is_equal, op1=ALU.mult), reads=[r_k5, r_pfx, r_M], writes=[r_Sel])
            yield
            for s_ in range(2):
                for blk in range(16):
                    fw.op("pe", lambda e, s_=s_, blk=blk: e.matmul(
                        pTok[:, s_, 0:8], lhsT=Sel[:, blk, s_ * 128:(s_ + 1) * 128], rhs=tv[:, blk, :],
                        start=(blk == 0), stop=(blk == 15)), inc=(blk == 15), reads=[r_Sel, r_k5], writes=[r_pTok])
            fw.op("act", lambda e: e.activation(out=tokc[:], in_=pTok[:, 0:2, 0:2], func=AF.Copy),
                  reads=[r_pTok], writes=[r_tokc])
            for s_ in range(2):
                fw.op("dve", lambda e, s_=s_: e.scalar_tensor_tensor(
                    out=tokf[:, s_:s_ + 1], in0=tokc[:, s_, 0:1], scalar=64.0, in1=tokc[:, s_, 1:2],
                    op0=ALU.mult, op1=ALU.add), reads=[r_tokc], writes=[r_tokf])
            for s_ in range(2):
                fw.op("dve", lambda e, s_=s_: e.tensor_scalar(
                    out=sT[:, s_, :], in0=iota_t[:], scalar1=tokf[:, s_:s_ + 1], scalar2=None, op0=ALU.is_equal),
                    reads=[r_k5, r_tokf], writes=[rsT])
            yield
            for half in range(2):
                for c2 in range(4):
                    c = 4 * half + c2
                    pa = pA_[c2 // 2]; rpa = r_pA_[c2 // 2]
                    for blk in range(16):
                        fw.op("pe", lambda e, c=c, c2=c2, blk=blk, pa=pa: e.matmul(
                            pa[:, (c2 % 2) * CAP:(c2 % 2 + 1) * CAP], lhsT=hn2k[:, blk, c * 128:(c + 1) * 128], rhs=Sel[:, blk, :],
                            start=(blk == 0), stop=(blk == 15)), inc=(blk == 15), reads=[r_hn2, r_Sel], writes=[rpa])
                    fw.op("act", lambda e, c=c, c2=c2, pa=pa: e.activation(
                        out=XgT[:, c, :], in_=pa[:, (c2 % 2) * CAP:(c2 % 2 + 1) * CAP], func=AF.Copy),
                        reads=[rpa], writes=[r_XgT])
                    yield
            for f in range(4):
                b_ = cnt5["gu"] % 2
                cnt5["gu"] += 1
                for c in range(8):
                    fw.op("pe", lambda e, c=c, f=f, b_=b_: e.matmul(
                        pGU[b_][:, 0:CAP], lhsT=wgb[:, c, f * 128:(f + 1) * 128], rhs=XgT[:, c, :],
                        start=(c == 0), stop=(c == 7)), inc=(c == 7), reads=[r_wgb, r_XgT], writes=[r_pGU[b_]])
                for c in range(8):
                    fw.op("pe", lambda e, c=c, f=f, b_=b_: e.matmul(
                        pGU[b_][:, CAP:2 * CAP], lhsT=wub[:, c, f * 128:(f + 1) * 128], rhs=XgT[:, c, :],
                        start=(c == 0), stop=(c == 7)), inc=(c == 7), reads=[r_wub, r_XgT], writes=[r_pGU[b_]])
                fw.op("act", lambda e, b_=b_: e.activation(out=th5[b_][:], in_=pGU[b_][:, 0:CAP], func=AF.Tanh, scale=0.5),
                      reads=[r_pGU[b_]], writes=[r_th5[b_]])
                fw.op("dve", lambda e, b_=b_: e.scalar_tensor_tensor(out=s1[b_][:], in0=th5[b_][:], scalar=1.0,
                                                                    in1=pGU[b_][:, 0:CAP], op0=ALU.add, op1=ALU.mult),
                      reads=[r_th5[b_], r_pGU[b_]], writes=[r_s1[b_]])
                fw.op("dve", lambda e, b_=b_, f=f: e.scalar_tensor_tensor(
                    out=HT[:, f, :], in0=s1[b_][:], scalar=0.5, in1=pGU[b_][:, CAP:2 * CAP], op0=ALU.mult, op1=ALU.mult),
                    reads=[r_s1[b_], r_pGU[b_]], writes=[r_HT])
                yield
            for s_ in range(2):
                for half in range(2):
                    pa = pA_[half]; rpa = r_pA_[half]
                    for f in range(4):
                        fw.op("pe", lambda e, f=f, s_=s_, half=half, pa=pa: e.matmul(
                            pa[:], lhsT=HT[:, f, s_ * 128:(s_ + 1) * 128], rhs=wdb[:, f, half * 512:(half + 1) * 512],
                            start=(f == 0), stop=(f == 3)), inc=(f == 3), reads=[r_HT, r_wdb], writes=[rpa])
                    fw.op("act", lambda e, s_=s_, half=half, pa=pa: e.activation(
                        out=yb[:, s_, half * 512:(half + 1) * 512], in_=pa[:], func=AF.Copy),
                        reads=[rpa], writes=[ryb])
                    yield

        def stage_b_units(ex_):
            sT = SelT[ex_ % 2]; rsT = r_SelT[ex_ % 2]
            yb = Yb[ex_ % 2]; ryb = r_Yb[ex_ % 2]
            units = []
            for blk in range(16):
                for half in range(2):
                    def unit(blk=blk, half=half):
                        ci = cnt5["c"] % 3
                        cnt5["c"] += 1
                        for s_ in range(2):
                            fw.op("pe", lambda e, s_=s_: e.matmul(
                                pC[ci][:], lhsT=sT[:, s_, blk * 128:(blk + 1) * 128],
                                rhs=yb[:, s_, half * 512:(half + 1) * 512], start=(s_ == 0), stop=(s_ == 1)),
                                inc=(s_ == 1), reads=[rsT, ryb], writes=[r_pC[ci]])
                        fw.op("dve", lambda e: e.scalar_tensor_tensor(
                            out=x1[:, blk, half * 512:(half + 1) * 512], in0=pC[ci][:], scalar=Cw[:, blk, ex_:ex_ + 1],
                            in1=x1[:, blk, half * 512:(half + 1) * 512], op0=ALU.mult, op1=ALU.add),
                            reads=[r_pC[ci], r_C, r_x1], writes=[r_x1])
                    units.append(unit)
            return units

        w_begin(0)
        for _ in range(3):
            w_issue()
        for _ in stage_a(0):
            w_cast()
            w_issue()
        for ex_ in range(32):
            units = stage_b_units(ex_)
            if ex_ + 1 < 32:
                w_begin(ex_ + 1)
                for _ in range(3):
                    w_issue()
                ui = 0
                for _ in stage_a(ex_ + 1):
                    w_cast()
                    w_issue()
                    for _k in range(2):
                        if ui < len(units):
                            units[ui]()
                            ui += 1
                while wq5["todo"] or wq5["issued"]:
                    w_cast()
                    w_issue()
                while ui < len(units):
                    units[ui]()
                    ui += 1
            else:
                for u in units:
                    u()
        fw.barrier()
        es6.close()

        set_free([[104 * KB, 203 * KB]])
        es7 = ExitStack()
        cur["es"] = es7
        gF = sb("gF", [128, D]); r_gF = Res("gF")
        ob = [sb("ob%d" % i, [128, D]) for i in range(2)]
        r_ob = [Res("ob%d" % i) for i in range(2)]
        fin = sb("fin", [128, 4]); r_fin = Res("fin")
        fw.dma("sp", gF[:], g_fin[:, :], writes=[r_gF])
        out_v = out_d.rearrange("(b p) d -> p b d", p=128)
        for blk in range(16):
            o_ = ob[blk % 2]; ro_ = r_ob[blk % 2]
            fw.op("act", lambda e, blk=blk, o_=o_: e.activation(out=o_[:], in_=x1[:, blk, :], func=AF.Square,
                                                               accum_out=fin[:, 0:1]),
                  reads=[r_x1], writes=[ro_, r_fin])
            fw.op("act", lambda e: e.activation(out=fin[:, 1:2], in_=fin[:, 0:1], func=AF.Ln, bias=eps_sb[:, 0:1],
                                                scale=1.0 / D), reads=[r_fin, r_ones], writes=[r_fin])
            fw.op("act", lambda e: e.activation(out=fin[:, 1:2], in_=fin[:, 1:2], func=AF.Exp, scale=-0.5),
                  reads=[r_fin], writes=[r_fin])
            fw.op("dve", lambda e, blk=blk, o_=o_: e.scalar_tensor_tensor(
                out=o_[:], in0=x1[:, blk, :], scalar=fin[:, 1:2], in1=gF[:], op0=ALU.mult, op1=ALU.mult),
                reads=[r_x1, r_fin, r_gF], writes=[ro_])
            fw.dma("sp", out_v[:, blk, :], o_[:], reads=[ro_])
        fw.barrier()
        fw.final_wait("sp")
        es7.close()
    return nc


def _bf(a):
    return np.asarray(a, dtype=np.float32).astype(ml_dtypes.bfloat16)


def make_in_maps(inputs):
    x = np.asarray(inputs["x"], dtype=np.float32)
    f = lambda k: np.asarray(inputs[k], dtype=np.float32)
    pc = lambda v: np.ascontiguousarray(v.reshape(-1, 128).T)
    g_mix = pc(f("norm_mix_g")[0])
    w_in = np.ascontiguousarray(f("w_in")[0])
    conv_w = np.ascontiguousarray(f("conv_w")[0].reshape(4, 4, 128).transpose(2, 1, 0))
    conv_b = pc(f("conv_b")[0])

    def bd(w):
        o = np.zeros((128, 4, 128), np.float32)
        for n in range(8):
            g, hlf = n // 2, n % 2
            o[hlf * 64:(hlf + 1) * 64, g, hlf * 64:(hlf + 1) * 64] = w[n]
        return o
    wr_bd = bd(f("w_r")[0])
    wi_bd = bd(f("w_i")[0])
    tk = np.arange(S)
    kaug = _bf(np.stack([tk // 64, tk % 64, np.ones(S), np.ones(S)]).astype(np.float32))
    w_o_attn = np.ascontiguousarray(f("w_o_attn")[0])
    w_o_lru = np.ascontiguousarray(f("w_o_lru")[0])
    w_out_ = np.ascontiguousarray(f("w_out")[0])
    g_ffn = pc(f("norm_ffn_g")[0])
    w_router = np.ascontiguousarray(np.concatenate(
        [f("w_group")[0], f("w_expert_router")[0].transpose(1, 0, 2).reshape(D, 32)], axis=1))
    w_gate_ = np.ascontiguousarray(f("w_gate")[0])
    w_up_ = np.ascontiguousarray(f("w_up")[0])
    w_down_ = np.ascontiguousarray(f("w_down")[0])
    g_fin = np.ascontiguousarray(np.tile(f("final_norm_g")[None, :], (128, 1)))
    g_ffn_rep = np.ascontiguousarray(np.tile(f("norm_ffn_g")[0][None, :], (128, 1)))
    iota_s = np.ascontiguousarray(np.tile(np.arange(256, dtype=np.float32)[None, :], (128, 1)))
    iota_t = np.ascontiguousarray(np.tile(np.arange(1, TOWN + 1, dtype=np.float32)[None, :], (128, 1)))
    ustrict = _bf((np.arange(128)[:, None] < np.arange(128)[None, :]).astype(np.float32))
    code = (np.arange(16)[None, :] * 128 + np.arange(128)[:, None] + 1)
    tvals = _bf(np.stack([code // 64, code % 64] + [np.zeros_like(code)] * 6, axis=-1).astype(np.float32))
    maps = []
    for c in range(NCORES):
        b, j = c // 4, c % 4
        xn = np.ascontiguousarray(x[b].T)
        own_blocks = [4 * i + j for i in range(16)]
        tq = np.concatenate([np.arange(bl * 128, (bl + 1) * 128) for bl in own_blocks])
        xo = np.ascontiguousarray(x[b][tq].T)
        qa = np.zeros((NH, 4, TOWN), np.float32)
        for h in range(NH):
            s8 = SLOPES[h] * 8.0
            qa[h, 0] = 64.0 * s8
            qa[h, 1] = s8
            qa[h, 2] = -64.0 * s8 * (tq // 64)
            qa[h, 3] = -s8 * (tq % 64)
        cm = np.zeros((128, 4, 128), np.float32)
        for jj in range(4):
            if jj < j:
                cm[:, jj, :] = 1.0
            elif jj == j:
                cm[:, jj, :] = (np.arange(128)[:, None] <= np.arange(128)[None, :])
        sj = np.zeros((128, 4), np.float32)
        sj[:, j] = 1.0
        extra = {
            "xot": np.ascontiguousarray(x[b][tq]),
            "lam_qk": np.ascontiguousarray(f("lambda_qk")[0].reshape(4, 64).T),
            "subln": np.ascontiguousarray(f("subln_g")[0].reshape(128, 1)),
            "w_o_attn": w_o_attn, "w_o_lru": w_o_lru, "w_out": w_out_, "g_ffn": g_ffn,
            "w_router": w_router, "w_gate": w_gate_, "w_up": w_up_, "w_down": w_down_, "g_fin": g_fin, "g_ffn_rep": g_ffn_rep,
            "iota_s": iota_s, "iota_t": iota_t, "ustrict": ustrict, "tvals": tvals,
        }
        maps.append({
            "xn": xn, "xo": xo, "g_mix": g_mix, "w_in": w_in, "kaug": kaug, "qaug": _bf(qa),
            "cmask": _bf(cm), "ident": _bf(np.eye(128, dtype=np.float32)), "selj": sj, "conv_w": conv_w, "conv_b": conv_b,
            "wr_bd": wr_bd, "wi_bd": wi_bd, "b_r": pc(f("b_r")[0]), "b_i": pc(f("b_i")[0]),
            "lru_lam": pc(f("lru_lambda")[0]), **extra,
        })
    return maps


def kernel(**inputs):
    nc = build()
    in_maps = make_in_maps(inputs)
    res = run_bass_kernel_spmd(nc, in_maps, core_ids=list(range(NCORES)))
    out = np.zeros((NB, S, D), np.float32)
    for c in range(NCORES):
        b, j = c // 4, c % 4
        o = np.asarray(res.results[c]["out"])
        for i in range(16):
            bl = 4 * i + j
            out[b, bl * 128:(bl + 1) * 128, :] = o[i * 128:(i + 1) * 128, :]
    return out
```
